# Optimizing a Trainium2 kernel written in Bass

```python
import math
import jax, jax.numpy as jnp
from jax import lax
import numpy as np

D_MODEL = 1024
BATCH = 16
SEQ = 2048
DEPTH = 2

CHUNK = 64
EPS = 1e-6
NEG_INF = -1e30
RET_HEADS = 4
RET_DK = 128
RET_DV = 128
RET_W = RET_HEADS * RET_DV
ROPE_BASE = 10000.0
CONV_W = 512
CONV_K = 31
ATT_HEADS = 8
ATT_DH = 64
ATT_W = ATT_HEADS * ATT_DH
ATT_LEFT_CHUNKS = 8
ATT_BAND = (ATT_LEFT_CHUNKS + 1) * CHUNK
MAX_REL = 256
N_BRANCH = 3
IN_COLS = 4 * RET_W + 2 * CONV_W + 3 * ATT_W + N_BRANCH * D_MODEL
N_GROUPS = 4
EXPERTS_PER_GROUP = 8
TOP_K_INNER = 2
EXPERT_HIDDEN = 256
N_MOD = 6

kernel_name = 'hybrid_retention_conv_chunkattn_hmoe'


def _rmsnorm(x, g):
    xf = x.astype(jnp.float32)
    y = xf * lax.rsqrt(jnp.mean(xf * xf, axis=-1, keepdims=True) + EPS)
    return (y * g.astype(jnp.float32)).astype(x.dtype)


def _layernorm_f32(x, g, b):
    xf = x.astype(jnp.float32)
    mu = jnp.mean(xf, axis=-1, keepdims=True)
    var = jnp.mean(jnp.square(xf - mu), axis=-1, keepdims=True)
    return (xf - mu) * lax.rsqrt(var + EPS) * g + b


def _rope(x, pos):
    half = x.shape[-1] // 2
    inv = ROPE_BASE ** (-jnp.arange(half, dtype=jnp.float32) / half)
    ang = pos.astype(jnp.float32)[..., None] * inv
    cos = jnp.cos(ang)[:, :, None, :]
    sin = jnp.sin(ang)[:, :, None, :]
    x1, x2 = x[..., :half], x[..., half:]
    return jnp.concatenate([x1 * cos - x2 * sin, x1 * sin + x2 * cos], axis=-1)


def _retention(q, k, v, g, positions, gn_gain):
    B, S, _ = q.shape
    N = S // CHUNK
    f32 = jnp.float32
    q = _rope(q.astype(f32).reshape(B, S, RET_HEADS, RET_DK), positions)
    k = _rope(k.astype(f32).reshape(B, S, RET_HEADS, RET_DK), positions) * (RET_DK ** -0.5)
    v = v.astype(f32).reshape(B, S, RET_HEADS, RET_DV)

    def chunks(t):
        return t.reshape(B, N, CHUNK, RET_HEADS, -1).transpose(0, 3, 1, 2, 4)

    q, k, v = chunks(q), chunks(k), chunks(v)
    log_gamma = jnp.log1p(-jnp.exp2(-5.0 - jnp.arange(RET_HEADS, dtype=f32)))
    pos = jnp.arange(CHUNK, dtype=f32)
    diff = pos[:, None] - pos[None, :]
    intra_decay = jnp.where(diff >= 0.0,
                            jnp.exp(log_gamma[:, None, None] * jnp.maximum(diff, 0.0)), 0.0)
    scores = jnp.einsum('bhncd,bhnmd->bhncm', q, k) * intra_decay[None, :, None]
    intra = jnp.einsum('bhncm,bhnme->bhnce', scores, v)
    key_decay = jnp.exp(log_gamma[:, None] * (CHUNK - 1.0 - pos)[None, :])
    kv = jnp.einsum('bhnmd,hm,bhnme->nbhde', k, key_decay, v)
    chunk_decay = jnp.exp(log_gamma * CHUNK)[None, :, None, None]

    def step(state, kv_n):
        return chunk_decay * state + kv_n, state

    _, prev = lax.scan(step, jnp.zeros((B, RET_HEADS, RET_DK, RET_DV), f32), kv)
    query_decay = jnp.exp(log_gamma[:, None] * (pos + 1.0)[None, :])
    cross = jnp.einsum('bhncd,nbhde->bhnce', q, prev) * query_decay[None, :, None, :, None]
    o = (intra + cross).transpose(0, 2, 3, 1, 4).reshape(B, S, RET_HEADS, RET_DV)
    mu = jnp.mean(o, axis=-1, keepdims=True)
    var = jnp.mean(jnp.square(o - mu), axis=-1, keepdims=True)
    o = (o - mu) * lax.rsqrt(var + EPS) * gn_gain.astype(f32).reshape(RET_HEADS, RET_DV)
    o = o.reshape(B, S, RET_W) * jax.nn.silu(g.astype(f32))
    return o.astype(g.dtype)


def _conv_module(u, w_dw, b_dw, ln_g, ln_b):
    a, b = jnp.split(u, 2, axis=-1)
    z = (a * jax.nn.sigmoid(b)).astype(w_dw.dtype)
    z = jnp.pad(z, ((0, 0), (CONV_K - 1, 0), (0, 0)))
    z = lax.conv_general_dilated(z, w_dw, (1,), 'VALID',
                                 dimension_numbers=('NWC', 'WIO', 'NWC'),
                                 feature_group_count=CONV_W) + b_dw
    z = _layernorm_f32(z, ln_g.astype(jnp.float32), ln_b.astype(jnp.float32))
    return jax.nn.silu(z).astype(u.dtype)


def _chunk_attention(q, k, v, q_gain, k_gain, rel_table):
    B, S, _ = q.shape
    N = S // CHUNK
    f32 = jnp.float32

    def heads(t):
        return t.reshape(B, S, ATT_HEADS, ATT_DH).transpose(0, 2, 1, 3)

    qh = heads(_rmsnorm(q.reshape(B, S, ATT_HEADS, ATT_DH), q_gain).reshape(B, S, ATT_W))
    kh = heads(_rmsnorm(k.reshape(B, S, ATT_HEADS, ATT_DH), k_gain).reshape(B, S, ATT_W))
    vh = heads(v)
    pad = ((0, 0), (0, 0), (ATT_LEFT_CHUNKS * CHUNK, 0), (0, 0))
    kp = jnp.pad(kh, pad)
    vp = jnp.pad(vh, pad)
    rel = ATT_LEFT_CHUNKS * CHUNK + jnp.arange(CHUNK)[:, None] - jnp.arange(ATT_BAND)[None, :]
    bias = rel_table.astype(f32)[:, jnp.clip(rel, -MAX_REL, MAX_REL) + MAX_REL]
    scale = ATT_DH ** -0.5
    band_idx = jnp.arange(ATT_BAND)

    def one_chunk(i):
        qi = lax.dynamic_slice_in_dim(qh, i * CHUNK, CHUNK, axis=2)
        ki = lax.dynamic_slice_in_dim(kp, i * CHUNK, ATT_BAND, axis=2)
        vi = lax.dynamic_slice_in_dim(vp, i * CHUNK, ATT_BAND, axis=2)
        s = jnp.einsum('bhqd,bhkd->bhqk', qi, ki).astype(f32) * scale + bias[None]
        valid = band_idx >= (ATT_LEFT_CHUNKS - i) * CHUNK
        s = jnp.where(valid[None, None, None, :], s, NEG_INF)
        p = jax.nn.softmax(s, axis=-1).astype(vi.dtype)
        return jnp.einsum('bhqk,bhkd->bhqd', p, vi)

    o = lax.map(one_chunk, jnp.arange(N))
    return o.transpose(1, 0, 3, 2, 4).reshape(B, S, ATT_W)


def _hier_moe(h, w_group, b_group, w_inner, b_inner, w_up, w_down):
    B, S, D = h.shape
    T = B * S
    f32 = jnp.float32
    t = h.reshape(T, D)
    gl = (t @ w_group + b_group).astype(f32)
    gsel = jnp.argmax(gl, axis=-1)
    p_group = jnp.take_along_axis(jax.nn.softmax(gl, axis=-1), gsel[:, None], axis=1)[:, 0]
    il = (t @ w_inner + b_inner).astype(f32).reshape(T, N_GROUPS, EXPERTS_PER_GROUP)
    chosen = jnp.take_along_axis(il, gsel[:, None, None], axis=1)[:, 0]
    topv, topi = lax.top_k(chosen, TOP_K_INNER)
    topw = jax.nn.softmax(topv, axis=-1) * p_group[:, None]
    w_e = jnp.sum(jax.nn.one_hot(topi, EXPERTS_PER_GROUP, dtype=f32) * topw[..., None], axis=1)
    w_ge = jax.nn.one_hot(gsel, N_GROUPS, dtype=f32)[:, :, None] * w_e[:, None, :]
    y = jnp.zeros((T, D), f32)
    for gi in range(N_GROUPS):
        hid = jnp.einsum('td,edf->tef', t, w_up[gi])
        a, b = jnp.split(hid, 2, axis=-1)
        act = jax.nn.silu(a.astype(f32)) * b.astype(f32) * w_ge[:, gi, :, None]
        y = y + jnp.einsum('tef,efd->td', act.astype(t.dtype), w_down[gi]).astype(f32)
    return y.reshape(B, S, D)


def _layer(x, c, positions, w_ada, b_ada, g_mix, g_ffn, w_in, b_gate, ret_gn,
           conv_w, conv_b, conv_ln_g, conv_ln_b, att_q_gain, att_k_gain, att_rel_bias,
           w_ret_out, w_conv_out, w_att_out, w_out,
           w_group, b_group, w_inner, b_inner, w_up, w_down):
    B, S, D = x.shape
    mod = jax.nn.silu(c) @ w_ada + b_ada
    shift1, scale1, gate1, shift2, scale2, gate2 = jnp.split(mod, N_MOD, axis=-1)

    h = _rmsnorm(x, g_mix) * (1.0 + scale1[:, None, :]) + shift1[:, None, :]
    proj = h @ w_in
    cuts = [RET_W, 2 * RET_W, 3 * RET_W, 4 * RET_W,
            4 * RET_W + 2 * CONV_W,
            4 * RET_W + 2 * CONV_W + ATT_W,
            4 * RET_W + 2 * CONV_W + 2 * ATT_W,
            4 * RET_W + 2 * CONV_W + 3 * ATT_W]
    rq, rk, rv, rg, cu, aq, ak, av, gl = jnp.split(proj, cuts, axis=-1)
    y_ret = _retention(rq, rk, rv, rg, positions, ret_gn) @ w_ret_out
    y_conv = _conv_module(cu, conv_w, conv_b, conv_ln_g, conv_ln_b) @ w_conv_out
    y_att = _chunk_attention(aq, ak, av, att_q_gain, att_k_gain, att_rel_bias) @ w_att_out
    gates = jax.nn.sigmoid((gl + b_gate).reshape(B, S, N_BRANCH, D))
    merged = gates[:, :, 0] * y_ret + gates[:, :, 1] * y_conv + gates[:, :, 2] * y_att
    x = x + (gate1[:, None, :] * (merged @ w_out)).astype(x.dtype)

    h2 = _rmsnorm(x, g_ffn) * (1.0 + scale2[:, None, :]) + shift2[:, None, :]
    y_ffn = _hier_moe(h2, w_group, b_group, w_inner, b_inner, w_up, w_down)
    x = x + (gate2[:, None, :] * y_ffn).astype(x.dtype)
    return x


def setup_inputs(seed: int = 0) -> dict:
    key = jax.random.key(seed)
    ks = jax.random.split(key, 32)
    f32 = jnp.float32
    L, D = DEPTH, D_MODEL
    E, G, F = EXPERTS_PER_GROUP, N_GROUPS, EXPERT_HIDDEN

    def nrm(k, shape, scale):
        return jax.random.normal(k, shape, f32) * scale

    x = jax.random.normal(ks[0], (BATCH, SEQ, D), f32)
    c = jax.random.normal(ks[1], (BATCH, D), f32)
    offset = jax.random.randint(ks[2], (BATCH, 1), 0, 4096, dtype=jnp.int32)
    positions = (offset + jnp.arange(SEQ, dtype=jnp.int32)[None, :]).astype(jnp.int32)
    return {
        'x': x,
        'c': c,
        'positions': positions,
        'w_ada': nrm(ks[3], (L, D, N_MOD * D), 0.5 * D ** -0.5),
        'b_ada': nrm(ks[4], (L, N_MOD * D), 0.02),
        'g_mix': 1.0 + nrm(ks[5], (L, D), 0.02),
        'g_ffn': 1.0 + nrm(ks[6], (L, D), 0.02),
        'w_in': nrm(ks[7], (L, D, IN_COLS), D ** -0.5),
        'b_gate': nrm(ks[8], (L, N_BRANCH * D), 0.02),
        'ret_gn': 1.0 + nrm(ks[9], (L, RET_W), 0.02),
        'conv_w': nrm(ks[10], (L, CONV_K, 1, CONV_W), CONV_K ** -0.5),
        'conv_b': nrm(ks[11], (L, CONV_W), 0.02),
        'conv_ln_g': 1.0 + nrm(ks[12], (L, CONV_W), 0.02),
        'conv_ln_b': nrm(ks[13], (L, CONV_W), 0.02),
        'att_q_gain': 1.0 + nrm(ks[14], (L, ATT_DH), 0.02),
        'att_k_gain': 1.0 + nrm(ks[15], (L, ATT_DH), 0.02),
        'att_rel_bias': nrm(ks[16], (L, ATT_HEADS, 2 * MAX_REL + 1), 0.1),
        'w_ret_out': nrm(ks[17], (L, RET_W, D), RET_W ** -0.5),
        'w_conv_out': nrm(ks[18], (L, CONV_W, D), CONV_W ** -0.5),
        'w_att_out': nrm(ks[19], (L, ATT_W, D), ATT_W ** -0.5),
        'w_out': nrm(ks[20], (L, D, D), D ** -0.5),
        'w_group': nrm(ks[21], (L, D, G), D ** -0.5),
        'b_group': nrm(ks[22], (L, G), 0.01),
        'w_inner': nrm(ks[23], (L, D, G * E), D ** -0.5),
        'b_inner': nrm(ks[24], (L, G * E), 0.01),
        'w_up': nrm(ks[25], (L, G, E, D, 2 * F), D ** -0.5),
        'w_down': nrm(ks[26], (L, G, E, F, D), F ** -0.5),
    }


def reference(x, c, positions, w_ada, b_ada, g_mix, g_ffn, w_in, b_gate, ret_gn,
              conv_w, conv_b, conv_ln_g, conv_ln_b, att_q_gain, att_k_gain, att_rel_bias,
              w_ret_out, w_conv_out, w_att_out, w_out,
              w_group, b_group, w_inner, b_inner, w_up, w_down):
    for l in range(DEPTH):
        x = _layer(x, c, positions, w_ada[l], b_ada[l], g_mix[l], g_ffn[l], w_in[l], b_gate[l],
                   ret_gn[l], conv_w[l], conv_b[l], conv_ln_g[l], conv_ln_b[l],
                   att_q_gain[l], att_k_gain[l], att_rel_bias[l],
                   w_ret_out[l], w_conv_out[l], w_att_out[l], w_out[l],
                   w_group[l], b_group[l], w_inner[l], b_inner[l], w_up[l], w_down[l])
    return x
```

```python
import math
import numpy as np
from concourse.bass_utils import run_bass_kernel_spmd

import numpy as np
import concourse.bass as bass
import concourse.mybir as mybir

F32 = mybir.dt.float32
BF16 = mybir.dt.bfloat16
I32 = mybir.dt.int32
ALU = mybir.AluOpType
ACT = mybir.ActivationFunctionType
AX = mybir.AxisListType

SAME_ENGINE_SYNC = False
SMALL_N = 512


class Op:
    __slots__ = ("eng", "fn", "deps", "signal", "semval", "dma_inc", "idx", "small")

    def __init__(self, eng, fn, deps, dma_inc=None):
        self.eng = eng
        self.fn = fn
        self.deps = deps
        self.signal = False
        self.semval = None
        self.dma_inc = dma_inc
        self.idx = None
        self.small = False


class DmaTok:
    __slots__ = ("sem", "val", "eng")

    def __init__(self, sem, val):
        self.sem = sem
        self.val = val
        self.eng = None


class T:
    def __init__(self, h, name=""):
        self.h = h
        self.name = name
        self.last_w = None
        self.readers = []
        self.dsem = None
        self.dcount = 0

    def __getitem__(self, k):
        return self.h[k]


class Sched:
    ENGS = ("pe", "act", "dve", "pool", "sp")

    def __init__(self, nc):
        self.nc = nc
        self.ops = {e: [] for e in self.ENGS}
        self.sems = {}
        self.stack = None
        self.dma_sems = []
        self.free_dma_sems = []
        self.dma_toks = []
        self.uid = 0

    def set_stack(self, stack):
        self.stack = stack

    def sem(self, name):
        return self.stack.enter_context(self.nc.semaphore(name))

    def sb(self, name, shape, dtype, stack=None):
        st = stack or self.stack
        self.uid += 1
        name = f"{name}_{self.uid}"
        h = st.enter_context(self.nc.sbuf_tensor(name, list(shape), dtype))
        t = T(h, name)
        if st is not self.stack:
            st.callback(self._retire, t)
        return t

    def _retire(self, t):
        if t.dsem is not None:
            self.free_dma_sems.append((t.dsem, t.dcount))
            t.dsem = None

    def ps(self, name, shape, dtype=F32, stack=None):
        st = stack or self.stack
        h = st.enter_context(self.nc.psum_tensor(name, list(shape), dtype))
        return T(h, name)

    def dram(self, h, name=""):
        return T(h, name)

    def _deps(self, reads, writes):
        deps = []
        for t in reads:
            if t.last_w is not None:
                deps.append(t.last_w)
        for t in writes:
            if t.last_w is not None:
                deps.append(t.last_w)
            deps.extend(t.readers)
        return deps

    def _commit(self, tok, reads, writes):
        for t in reads:
            t.readers.append(tok)
            if len(t.readers) > 64:
                t.readers = self._compact(t.readers)
        for t in writes:
            t.last_w = tok
            t.readers = []

    @staticmethod
    def _compact(toks):
        last = {}
        for tk in toks:
            key = (tk.eng if isinstance(tk, Op) else id(tk.sem))
            last[key] = tk
        return list(last.values())

    def op(self, eng, fn, reads=(), writes=(), small=True):
        o = Op(eng, fn, self._deps(reads, writes))
        o.small = bool(small) and eng != "pe"
        o.idx = len(self.ops[eng])
        self.ops[eng].append(o)
        self._commit(o, reads, writes)
        return o

    def dma(self, q, out_t, out_ap, in_t, in_ap, sem_t=None, **kw):
        st = sem_t or out_t
        if st.dsem is None:
            if self.free_dma_sems:
                st.dsem, st.dcount = self.free_dma_sems.pop()
            else:
                st.dsem = self.sem("d_" + st.name)
                st.dcount = 0
        st.dcount += 16
        tok = DmaTok(st.dsem, st.dcount)
        deps = self._deps([in_t], [out_t])
        sem = st.dsem

        def fn(e, out_ap=out_ap, in_ap=in_ap, kw=kw):
            return e.dma_start(out=out_ap, in_=in_ap, **kw)

        o = Op(q, fn, deps, dma_inc=sem)
        o.idx = len(self.ops[q])
        self.ops[q].append(o)
        self._commit(tok, [in_t], [out_t])
        self.dma_toks.append(tok)
        return tok

    def barrier(self):
        lasts = []
        for e in self.ENGS:
            for o in reversed(self.ops[e]):
                if o.fn is not None and o.dma_inc is None:
                    lasts.append(o)
                    break
        toks = list(self.dma_toks)
        self.dma_toks = []
        for e in self.ENGS:
            deps = [o for o in lasts if o.eng != e] + toks
            if not deps:
                continue
            o = Op(e, None, deps)
            o.idx = len(self.ops[e])
            self.ops[e].append(o)

    def finalize(self):
        for e in self.ENGS:
            for o in self.ops[e]:
                for d in o.deps:
                    if isinstance(d, Op):
                        if d.eng != o.eng or SAME_ENGINE_SYNC or d.small:
                            d.signal = True
        self.counts = {}
        for e in self.ENGS:
            c = 0
            for o in self.ops[e]:
                if o.signal:
                    c += 1
                    o.semval = c
            self.counts[e] = c
            if c > 0 or True:
                self.sems[e] = self.sem("eng_" + e)

    def replay(self, e, handle):
        seen = {}
        nc = self.nc
        for o in self.ops[e]:
            waits = {}
            for d in o.deps:
                if isinstance(d, Op):
                    if d.eng == e and not (SAME_ENGINE_SYNC or d.small):
                        continue
                    key = ("e", d.eng)
                    sem = self.sems[d.eng]
                    val = d.semval
                else:
                    key = ("d", id(d.sem))
                    sem = d.sem
                    val = d.val
                if seen.get(key, 0) >= val:
                    continue
                if key not in waits or waits[key][1] < val:
                    waits[key] = (sem, val)
            for key, (sem, val) in waits.items():
                handle.wait_ge(sem, val)
                seen[key] = val
            if o.fn is None:
                continue
            ins = o.fn(handle)
            if o.dma_inc is not None:
                ins.then_inc(o.dma_inc, 16)
            elif o.signal:
                ins.then_inc(self.sems[e], 1)

    def emit(self, final_toks=()):
        nc = self.nc
        self.finalize()
        with nc.Block() as block:
            @block.tensor
            def _(h):
                self.replay("pe", h)

            @block.scalar
            def _(h):
                self.replay("act", h)

            @block.vector
            def _(h):
                self.replay("dve", h)

            @block.gpsimd
            def _(h):
                self.replay("pool", h)

            @block.sync
            def _(h):
                self.replay("sp", h)
                for tk in final_toks:
                    h.wait_ge(tk.sem, tk.val)

from contextlib import ExitStack

P = 128
SEQ = 2048
D = 1024
KC = 8
NT = 16
NQ = 4
L = 2
NSEQ = 2
IN_COLS = 7680
EPS = 1e-6
NEG = -1e30
C_RQ, C_RK, C_RV, C_RG = 0, 512, 1024, 1536
C_CU = 2048
C_AQ, C_AK, C_AV = 3072, 3584, 4096
C_GL = 4608
NG, NE = 4, 8
PP_CW = 0
PP_CB = 124
PP_LG = 128
PP_LB = 132
PP_QG = 136
PP_KG = 137
PP_BG = 138
NPP = 162
CS_ID = 0
CS_MASK = 128
CS_QDEC = 640
CS_KDEC = 1152
CS_INV = 1156
CS_SGN = 1157
CS_MLO = 1158
CS_MHI = 1159
NCST = 1160
RET_CD = [float((1.0 - 2.0 ** (-5 - h)) ** 128) for h in range(4)]


def build_program(n_layers=L, n_seq=NSEQ, n_experts=NG * NE, dbg=None, dump=False):
    nc = bass.Bass("TRN2", target_bir_lowering=False)
    dump_toks = []

    def din(name, shape, dt=F32):
        return nc.dram_tensor(name, list(shape), dt, kind="ExternalInput")

    x_h = din("x", [NSEQ, SEQ, D])
    pos_h = din("pos", [NSEQ, SEQ], I32)
    cT_h = din("cT", [P, KC, NSEQ])
    w_ada_h = din("w_ada", [L, D, 6 * D])
    b_ada_h = din("b_ada", [L, 6 * D])
    g_mix_h = din("g_mix", [L, D])
    g_ffn_h = din("g_ffn", [L, D])
    w_in_h = din("w_in", [L, D, IN_COLS])
    ret_gn_h = din("ret_gn", [L, 512])
    w_ro_h = din("w_ret_out", [L, 512, D])
    w_co_h = din("w_conv_out", [L, 512, D])
    w_ao_h = din("w_att_out", [L, 512, D])
    w_out_h = din("w_out", [L, D, D])
    w_up_h = din("w_up", [L, NG, NE, D, 512])
    w_dn_h = din("w_down", [L, NG, NE, 256, D])
    pp_h = din("pp", [L, P, NPP])
    wr_h = din("wr", [L, P, KC, 36])
    brow_h = din("brow", [L, 36])
    biasx_h = din("biasx", [L, P, 5, 8, P])
    cst_h = din("cst", [P, NCST])
    out_h = nc.dram_tensor("out", [NSEQ, SEQ, D], F32, kind="ExternalOutput")
    modd_h = nc.dram_tensor("modd", [L, NSEQ, 6 * D], F32)
    dbg_h = nc.dram_tensor("dbg", [P, 6, 8192], F32, kind="ExternalOutput") if dump else None

    top = ExitStack()
    with top:
        S = Sched(nc)
        S.set_stack(top)

        def MM(out_t, out_ap, lt, lap, rt, rap, start=True, stop=True):
            S.op("pe", lambda e: e.matmul(out_ap, lhsT=lap, rhs=rap, start=start, stop=stop),
                 [lt, rt], [out_t])

        def TR(out_t, out_ap, in_t, in_ap, id_t, id_ap):
            S.op("pe", lambda e: e.transpose(out=out_ap, in_=in_ap, identity=id_ap), [in_t, id_t], [out_t])

        def ACTF(out_t, out_ap, in_t, in_ap, func, bias=None, scale=None, accum=None, reads=()):
            kw = {}
            if bias is not None:
                kw["bias"] = bias
            if scale is not None:
                kw["scale"] = scale
            wr = [out_t]
            if accum is not None:
                kw["accum_out"] = accum[1]
                wr.append(accum[0])
            S.op("act", lambda e: e.activation(out=out_ap, in_=in_ap, func=func, **kw),
                 [in_t] + list(reads), wr, small=(accum is not None or out_ap.free_size() < SMALL_N))

        def TT(eng, out_t, out_ap, a_t, a_ap, b_t, b_ap, op):
            S.op(eng, lambda e: e.tensor_tensor(out=out_ap, in0=a_ap, in1=b_ap, op=op), [a_t, b_t], [out_t],
                 small=out_ap.free_size() < SMALL_N)

        def TS(eng, out_t, out_ap, a_t, a_ap, s1, s2, op0, op1=None, reads=()):
            if op1 is None:
                S.op(eng, lambda e: e.tensor_scalar(out=out_ap, in0=a_ap, scalar1=s1, scalar2=None, op0=op0),
                     [a_t] + list(reads), [out_t], small=out_ap.free_size() < SMALL_N)
            else:
                S.op(eng, lambda e: e.tensor_scalar(out=out_ap, in0=a_ap, scalar1=s1, scalar2=s2, op0=op0, op1=op1),
                     [a_t] + list(reads), [out_t], small=out_ap.free_size() < SMALL_N)

        def STT(eng, out_t, out_ap, a_t, a_ap, sc, b_t, b_ap, op0, op1, reads=()):
            S.op(eng, lambda e: e.scalar_tensor_tensor(out=out_ap, in0=a_ap, scalar=sc, in1=b_ap, op0=op0, op1=op1),
                 [a_t, b_t] + list(reads), [out_t], small=out_ap.free_size() < SMALL_N)

        def CP(eng, out_t, out_ap, in_t, in_ap):
            if eng == "act":
                S.op("act", lambda e: e.copy(out=out_ap, in_=in_ap), [in_t], [out_t], small=out_ap.free_size() < SMALL_N)
            else:
                S.op(eng, lambda e: e.tensor_copy(out=out_ap, in_=in_ap), [in_t], [out_t], small=out_ap.free_size() < SMALL_N)

        def MSET(eng, t, ap, val):
            S.op(eng, lambda e: e.memset(ap, val), [], [t], small=ap.free_size() < SMALL_N)

        def RSTD(t, ap, n_is_one_col=True):
            S.op("dve", lambda e: e.reciprocal(out=ap, in_=ap), [t], [t])
            ACTF(t, ap, t, ap, ACT.Sqrt)

        class Pool:
            def __init__(self, name, shape, dt, n, stack):
                self.tiles = [S.sb(f"{name}{i}", shape, dt, stack) for i in range(n)]
                self.i = 0

            def get(self):
                t = self.tiles[self.i % len(self.tiles)]
                self.i += 1
                return t

        def wcols(ap2d, c0, n):
            return ap2d.rearrange("(kc p) n -> p kc n", p=P)[:, :, c0:c0 + n]

        DR = lambda h, name: S.dram(h, name)
        dbg_d = DR(dbg_h, "dbg") if dump else None

        def DUMP(slot, t, ap, ncols):
            if dump:
                dump_toks.append(S.dma("pool", dbg_d, dbg_h[:, slot, 0:ncols], t, ap, sem_t=t))
        x_d = DR(x_h, "x")
        pos_d = DR(pos_h, "pos")
        wts_d = DR(w_in_h, "weights")
        modd_d = DR(modd_h, "modd")
        xo_d = [[DR(out_h, f"xo{s}_{j}") for j in range(NT)] for s in range(NSEQ)]

        cst = S.sb("cst", [P, NCST], F32)
        S.dma("sp", cst, cst[:], wts_d, cst_h[:, :])
        ident = (cst, cst[:, CS_ID:CS_ID + P])
        ps = [S.ps(f"ps{i}", [P, 512]) for i in range(8)]
        blk64 = S.sb("blk64", [P, P], BF16)
        MSET("dve", blk64, blk64[:], 0.0)
        MSET("dve", blk64, blk64[0:64, 0:64], 1.0 / 64)
        MSET("dve", blk64, blk64[64:128, 64:128], 1.0 / 64)
        onesln = S.sb("onesln", [P, P], F32)
        MSET("dve", onesln, onesln[:], 1.0 / 512)

        with ExitStack() as st:
          if dbg != "pro0":
              cT = S.sb("cT", [P, KC, NSEQ], F32, st)
              scT = S.sb("scT", [P, KC, NSEQ], BF16, st)
              S.dma("sp", cT, cT[:], wts_d, cT_h[:, :, :])
              ACTF(scT, scT[:], cT, cT[:], ACT.Silu)
              modrow = S.sb("modrow", [NSEQ, 6 * D], F32, st)
              brow2 = S.sb("brow2", [NSEQ, 6 * D], F32, st)
              grow = S.sb("grow", [NSEQ, 2, D], F32, st)
              wpool = Pool("wada", [P, KC, 512], BF16, 2, st)
              for l in range(n_layers):
                  S.dma("sp", brow2, brow2[:], wts_d, b_ada_h[l:l + 1, :].partition_broadcast(NSEQ))
                  S.dma("sp", grow, grow[:, 0, :], wts_d, g_mix_h[l:l + 1, :].partition_broadcast(NSEQ))
                  S.dma("sp", grow, grow[:, 1, :], wts_d, g_ffn_h[l:l + 1, :].partition_broadcast(NSEQ))
                  if dbg == "pro1":
                      break
                  for cb in range(12):
                      wb = wpool.get()
                      S.dma("pool", wb, wb[:], wts_d, wcols(w_ada_h[l], cb * 512, 512))
                      pt = ps[cb % 2]
                      for kc in range(KC):
                          MM(pt, pt[0:NSEQ, :], scT, scT[:, kc, :], wb, wb[:, kc, :], kc == 0, kc == KC - 1)
                      TT("dve", modrow, modrow[:, cb * 512:(cb + 1) * 512], pt, pt[0:NSEQ, :],
                         brow2, brow2[:, cb * 512:(cb + 1) * 512], ALU.add)
                  if dbg == "pro2":
                      break
                  for i, sl in ((0, 1), (1, 4)):
                      TS("dve", modrow, modrow[:, sl * D:(sl + 1) * D], modrow, modrow[:, sl * D:(sl + 1) * D], 1.0, None, ALU.add)
                      TT("dve", modrow, modrow[:, sl * D:(sl + 1) * D], modrow, modrow[:, sl * D:(sl + 1) * D],
                         grow, grow[:, i, :], ALU.mult)
                  if dbg == "pro3":
                      break
                  S.dma("sp", modd_d, modd_h[l], modrow, modrow[:], sem_t=modrow)
        S.barrier()

        class _Stop(Exception):
            pass

        def load_mod_bc(st, l, s, idx, name):
            t = S.sb(name, [P, D], F32, st)
            S.dma("sp", t, t[:], modd_d, modd_h[l, s:s + 1, idx * D:(idx + 1) * D].partition_broadcast(P))
            return t

        def norm_and_transpose(st, l, s, first_layer_input, gmod, shift, hT, h2f_cb=None):
            xs_pool = Pool("xs", [P, D], F32, 2, st)
            hf_pool = Pool("hf", [P, D], F32, 2, st)
            junk = S.sb("junk", [P, D], BF16, st)
            ssq = Pool("ssq", [P, 8], F32, 2, st)
            for j in range(NT):
                xs = xs_pool.get()
                if first_layer_input:
                    S.dma("sp", xs, xs[:], x_d, x_h[s, j * P:(j + 1) * P, :])
                else:
                    S.dma("sp", xs, xs[:], xo_d[s][j], out_h[s, j * P:(j + 1) * P, :])
                ss = ssq.get()
                MSET("dve", ss, ss[:], 0.0)
                ACTF(junk, junk[:], xs, xs[:], ACT.Square, accum=(ss, ss[:, 0:1]))
                TS("dve", ss, ss[:, 0:1], ss, ss[:, 0:1], 1.0 / D, EPS, ALU.mult, ALU.add)
                S.op("dve", lambda e, ss=ss: e.reciprocal(out=ss[:, 0:1], in_=ss[:, 0:1]), [ss], [ss])
                ACTF(ss, ss[:, 0:1], ss, ss[:, 0:1], ACT.Sqrt)
                hf = hf_pool.get()
                STT("dve", hf, hf[:], xs, xs[:], ss[:, 0:1], gmod, gmod[:], ALU.mult, ALU.mult, reads=[ss])
                TT("pool", hf, hf[:], hf, hf[:], shift, shift[:], ALU.add)
                if j == 0 and first_layer_input:
                    for o_, (t_, n_) in enumerate(((hf, D), (gmod, D), (shift, D), (xs, D), (ss, 8))):
                        if dump:
                            dump_toks.append(S.dma("pool", dbg_d, dbg_h[:, 5, o_ * 1024:o_ * 1024 + n_], t_, t_[:, 0:n_], sem_t=t_))
                q, jj = divmod(j, 4)
                pa, pb = ps[(2 * j) % 4], ps[(2 * j) % 4 + 1]
                for kc in range(KC):
                    pt = pa if kc < 4 else pb
                    TR(pt, pt[:, (kc % 4) * P:(kc % 4 + 1) * P], hf, hf[:, kc * P:(kc + 1) * P], *ident)
                CP("act", hT[q], hT[q][:, 0:4, jj * P:(jj + 1) * P], pa, pa[:].rearrange("p (k t) -> p k t", k=4))
                CP("act", hT[q], hT[q][:, 4:8, jj * P:(jj + 1) * P], pb, pb[:].rearrange("p (k t) -> p k t", k=4))
                if h2f_cb is not None:
                    h2f_cb(j, pa, pb)

        def mixer(s, l):
            first = (l == 0)
            w_in = w_in_h[l]
            with ExitStack() as st:
                hT = [S.sb(f"hT{q}", [P, KC, 512], BF16, st) for q in range(NQ)]
                oT = {k: S.sb(f"oT_{k}", [P, 4, SEQ], BF16, st) for k in ("ret", "conv", "att")}
                pp = S.sb("pp", [P, NPP], F32, st)
                S.dma("sp", pp, pp[:], wts_d, pp_h[l])
                with ExitStack() as st1:
                    gmod = load_mod_bc(st1, l, s, 1, "gmod1")
                    shift = load_mod_bc(st1, l, s, 0, "shift1")
                    norm_and_transpose(st1, l, s, first, gmod, shift, hT)
                DUMP(0, hT[0], hT[0][:].rearrange("p k t -> p (k t)"), 4096)
                S.barrier()
                if dbg == "s1":
                    return "stop"
                fpool = Pool("fblk", [P, KC, P], BF16, 4, st)

                def fproj(c0, q, pt):
                    raise NotImplementedError

                def load_f(c0, swap=False):
                    wb = fpool.get()
                    if not swap:
                        S.dma("pool", wb, wb[:], wts_d, wcols(w_in, c0, P))
                    else:
                        S.dma("pool", wb, wb[:, :, 0:64], wts_d, wcols(w_in, c0 + 64, 64))
                        S.dma("pool", wb, wb[:, :, 64:128], wts_d, wcols(w_in, c0, 64))
                    return wb

                def f_mm(wb, q, pt):
                    for kc in range(KC):
                        MM(pt, pt[:], wb, wb[:, kc, :], hT[q], hT[q][:, kc, :], kc == 0, kc == KC - 1)

                with ExitStack() as st2:
                    cosT = S.sb("cosT", [P, SEQ], BF16, st2)
                    sinT = S.sb("sinT", [P, SEQ], BF16, st2)
                    with ExitStack() as st3:
                        posi = S.sb("posi", [P, SEQ], I32, st3)
                        u = S.sb("rope_u", [P, SEQ], F32, st3)
                        f = S.sb("rope_f", [P, SEQ], F32, st3)
                        ki = S.sb("rope_ki", [P, SEQ], I32, st3)
                        S.dma("sp", posi, posi[:], pos_d, pos_h[s:s + 1, :].partition_broadcast(P))
                        CP("dve", u, u[:], posi, posi[:])
                        TS("dve", u, u[:], u, u[:], cst[:, CS_INV:CS_INV + 1], 1.0 / (2.0 * math.pi), ALU.mult, ALU.mult, reads=[cst])
                        for tab, off, sgn in ((sinT, 0.0, True), (cosT, 0.25, False)):
                            if off != 0.0:
                                TS("dve", f, f[:], u, u[:], off, None, ALU.add)
                                src = f
                            else:
                                src = u
                            CP("dve", ki, ki[:], src, src[:])
                            fk = S.sb("rope_fk" + ("s" if sgn else "c"), [P, SEQ], F32, st3)
                            CP("dve", fk, fk[:], ki, ki[:])
                            TT("dve", fk, fk[:], src, src[:], fk, fk[:], ALU.subtract)
                            TS("dve", fk, fk[:], fk, fk[:], 0.49999, -0.49999, ALU.min, ALU.max)
                            if sgn:
                                ACTF(fk, fk[:], fk, fk[:], ACT.Sin, scale=2.0 * math.pi)
                                TS("dve", tab, tab[:], fk, fk[:], cst[:, CS_SGN:CS_SGN + 1], None, ALU.mult, reads=[cst])
                            else:
                                ACTF(tab, tab[:], fk, fk[:], ACT.Sin, scale=2.0 * math.pi)
                    S.barrier()
                    state_f = S.sb("state_f", [P, 4, P], F32, st2)
                    state_b = S.sb("state_b", [P, 4, P], BF16, st2)
                    gn_bc = S.sb("gn_bc", [P, 512], F32, st2)
                    S.dma("sp", gn_bc, gn_bc[:], wts_d, ret_gn_h[l:l + 1, :].partition_broadcast(P))
                    HS = SEQ // 2
                    qT = S.sb("r_qT", [P, 4, HS], BF16, st2)
                    kT = S.sb("r_kT", [P, 4, HS], BF16, st2)
                    v_tok = S.sb("r_v", [P, 8, 512], BF16, st2)
                    gs = S.sb("r_gs", [P, 8, 512], BF16, st2)
                    tpool = Pool("tblk", [P, KC, 512], BF16, 2, st2)
                    t1p = Pool("r_t1", [P, 512], F32, 2, st2)
                    t2p = Pool("r_t2", [P, 512], F32, 2, st2)
                    sTm_p = Pool("r_sTm", [P, 512], BF16, 2, st2)
                    qd_p = Pool("r_qd", [P, 4, P], BF16, 2, st2)
                    kt_p = Pool("r_kt", [P, 512], BF16, 2, st2)
                    on_p = Pool("r_on", [P, 512], F32, 2, st2)
                    og_p = Pool("r_og", [P, 512], F32, 2, st2)
                    st_p = Pool("r_stats", [P, 16], F32, 2, st2)
                    sgt_p = Pool("r_sg", [P, 512], F32, 2, st2)
                    junk = S.sb("r_junk", [P, P], BF16, st2)
                    for seg in range(2):
                        for which, cbase, dst in (("q", C_RQ, qT), ("k", C_RK, kT)):
                            for h in range(4):
                                wb = load_f(cbase + h * P)
                                wsw = load_f(cbase + h * P, swap=True)
                                for qq in range(2):
                                    q = seg * 2 + qq
                                    pa, pb = ps[2 * (qq % 2)], ps[2 * (qq % 2) + 1]
                                    f_mm(wb, q, pa)
                                    f_mm(wsw, q, pb)
                                    t1, t2 = t1p.get(), t2p.get()
                                    TT("dve", t1, t1[:], pa, pa[:], cosT, cosT[:, q * 512:(q + 1) * 512], ALU.mult)
                                    TT("dve", t2, t2[:], pb, pb[:], sinT, sinT[:, q * 512:(q + 1) * 512], ALU.mult)
                                    TT("pool", dst, dst[:, h, qq * 512:(qq + 1) * 512], t1, t1[:], t2, t2[:], ALU.add)
                        for which, cbase in (("v", C_RV), ("g", C_RG)):
                            wb = tpool.get()
                            S.dma("pool", wb, wb[:], wts_d, wcols(w_in, cbase, 512))
                            for jl in range(8):
                                j = seg * 8 + jl
                                q, jj = divmod(j, 4)
                                pt = ps[4 + jl % 2]
                                for kc in range(KC):
                                    MM(pt, pt[:], hT[q], hT[q][:, kc, jj * P:(jj + 1) * P], wb, wb[:, kc, :], kc == 0, kc == KC - 1)
                                if which == "v":
                                    CP("act", v_tok, v_tok[:, jl, :], pt, pt[:])
                                else:
                                    sg = sgt_p.get()
                                    ACTF(sg, sg[:], pt, pt[:], ACT.Silu)
                                    TT("pool", gs, gs[:, jl, :], sg, sg[:], gn_bc, gn_bc[:], ALU.mult)
                        for jl in range(8):
                            j = seg * 8 + jl
                            tsl = slice(jl * P, (jl + 1) * P)
                            pS, pO, pK, pT_ = ps[0 + (jl % 2)], ps[2 + (jl % 2)], ps[6], ps[7]
                            for h in range(4):
                                MM(pS, pS[:, h * P:(h + 1) * P], kT, kT[:, h, tsl], qT, qT[:, h, tsl])
                            sTm = sTm_p.get()
                            TT("dve", sTm, sTm[:], pS, pS[:], cst, cst[:, CS_MASK:CS_MASK + 512], ALU.mult)
                            qd = qd_p.get()
                            TT("pool", qd, qd[:], qT, qT[:, :, tsl], cst,
                               cst[:, CS_QDEC:CS_QDEC + 512].rearrange("p (h c) -> p h c", h=4), ALU.mult)
                            pTb = pT_[:, 0:256].bitcast(BF16)
                            for h in range(4):
                                S.op("pe", lambda e, h=h, tsl=tsl, pTb=pTb: e.transpose(
                                    out=pTb[:, h * P:(h + 1) * P], in_=kT[:, h, tsl], identity=identb[:]),
                                    [kT, identb], [pT_])
                            kt = kt_p.get()
                            for h in range(4):
                                TS("dve", kt, kt[:, h * P:(h + 1) * P], pT_, pTb[:, h * P:(h + 1) * P],
                                   cst[:, CS_KDEC + h:CS_KDEC + h + 1], None, ALU.mult, reads=[cst])
                            for h in range(4):
                                hs = slice(h * P, (h + 1) * P)
                                MM(pO, pO[:, hs], sTm, sTm[:, hs], v_tok, v_tok[:, jl, hs], True, j == 0)
                                if j > 0:
                                    MM(pO, pO[:, hs], qd, qd[:, h, :], state_b, state_b[:, h, :], False, True)
                            if j < NT - 1:
                                for h in range(4):
                                    hs = slice(h * P, (h + 1) * P)
                                    MM(pK, pK[:, hs], kt, kt[:, hs], v_tok, v_tok[:, jl, hs])
                                if j == 0:
                                    CP("dve", state_f, state_f[:], pK, pK[:].rearrange("p (h e) -> p h e", h=4))
                                else:
                                    for h in range(4):
                                        STT("dve", state_f, state_f[:, h, :], state_f, state_f[:, h, :], RET_CD[h],
                                            pK, pK[:, h * P:(h + 1) * P], ALU.mult, ALU.add)
                                CP("act", state_b, state_b[:], state_f, state_f[:])
                            stt = st_p.get()
                            MSET("dve", stt, stt[:], 0.0)
                            for h in range(4):
                                hs = slice(h * P, (h + 1) * P)
                                ACTF(junk, junk[:], pO, pO[:, hs], ACT.Copy, accum=(stt, stt[:, h:h + 1]))
                                ACTF(junk, junk[:], pO, pO[:, hs], ACT.Square, accum=(stt, stt[:, 4 + h:5 + h]))
                            TS("dve", stt, stt[:, 0:8], stt, stt[:, 0:8], 1.0 / P, None, ALU.mult)
                            TT("dve", stt, stt[:, 8:12], stt, stt[:, 0:4], stt, stt[:, 0:4], ALU.mult)
                            TT("dve", stt, stt[:, 12:16], stt, stt[:, 4:8], stt, stt[:, 8:12], ALU.subtract)
                            TS("dve", stt, stt[:, 12:16], stt, stt[:, 12:16], EPS, None, ALU.add)
                            RSTD(stt, stt[:, 12:16])
                            on = on_p.get()
                            for h in range(4):
                                hs = slice(h * P, (h + 1) * P)
                                TS("dve", on, on[:, hs], pO, pO[:, hs], stt[:, h:h + 1], stt[:, 12 + h:13 + h],
                                   ALU.subtract, ALU.mult, reads=[stt])
                            og = og_p.get()
                            TT("pool", og, og[:], on, on[:], gs, gs[:, jl, :], ALU.mult)
                            pX = ps[4 + jl % 2]
                            for h in range(4):
                                TR(pX, pX[:, h * P:(h + 1) * P], og, og[:, h * P:(h + 1) * P], *ident)
                            CP("act", oT["ret"], oT["ret"][:, :, j * P:(j + 1) * P], pX, pX[:].rearrange("p (h c) -> p h c", h=4))
                S.barrier()
                if dbg == "ret":
                    return "stop"
                with ExitStack() as st2:
                    zT = S.sb("c_zT", [P, 4, 30 + SEQ], F32, st2)
                    acc = S.sb("c_acc", [P, 4, SEQ], F32, st2)
                    sgp = Pool("c_sg", [P, 512], F32, 2, st2)
                    ctmp = S.sb("c_tmp", [P, SEQ // 2], F32, st2)
                    MSET("pool", zT, zT[:, :, 0:30], 0.0)
                    for cc in range(4):
                        wa = load_f(C_CU + cc * P)
                        wb = load_f(C_CU + 512 + cc * P)
                        for q in range(NQ):
                            pa, pb = ps[2 * (q % 2)], ps[2 * (q % 2) + 1]
                            f_mm(wa, q, pa)
                            f_mm(wb, q, pb)
                            sg = sgp.get()
                            ACTF(sg, sg[:], pb, pb[:], ACT.Sigmoid)
                            TT("dve", zT, zT[:, cc, 30 + q * 512:30 + (q + 1) * 512], pa, pa[:], sg, sg[:], ALU.mult)
                        for half in range(2):
                            o0 = half * (SEQ // 2)
                            n = SEQ // 2
                            eng = "pool" if (cc * 2 + half) % 2 == 0 else "dve"
                            TS(eng, acc, acc[:, cc, o0:o0 + n], zT, zT[:, cc, o0:o0 + n],
                               pp[:, PP_CW + cc * 31:PP_CW + cc * 31 + 1], pp[:, PP_CB + cc:PP_CB + cc + 1],
                               ALU.mult, ALU.add, reads=[pp])
                            for k in range(1, 31):
                                wk = pp[:, PP_CW + cc * 31 + k:PP_CW + cc * 31 + k + 1]
                                if eng == "dve":
                                    STT(eng, acc, acc[:, cc, o0:o0 + n], zT, zT[:, cc, o0 + k:o0 + k + n], wk,
                                        acc, acc[:, cc, o0:o0 + n], ALU.mult, ALU.add, reads=[pp])
                                else:
                                    TS(eng, ctmp, ctmp[:], zT, zT[:, cc, o0 + k:o0 + k + n], wk, None, ALU.mult, reads=[pp])
                                    TT(eng, acc, acc[:, cc, o0:o0 + n], acc, acc[:, cc, o0:o0 + n], ctmp, ctmp[:], ALU.add)
                    sqp = Pool("c_sq", [P, 512], F32, 2, st2)
                    m2p = Pool("c_m2", [P, 512], F32, 2, st2)
                    rsp = Pool("c_rs", [P, 512], F32, 2, st2)
                    tp = Pool("c_t", [P, 512], F32, 2, st2)
                    for q in range(NQ):
                        qs = slice(q * 512, (q + 1) * 512)
                        pm, pe2 = ps[4 + 2 * (q % 2)], ps[5 + 2 * (q % 2)]
                        for cc in range(4):
                            MM(pm, pm[:], onesln, onesln[:], acc, acc[:, cc, qs], cc == 0, cc == 3)
                        for cc in range(4):
                            sq = sqp.get()
                            ACTF(sq, sq[:], acc, acc[:, cc, qs], ACT.Square)
                            MM(pe2, pe2[:], onesln, onesln[:], sq, sq[:], cc == 0, cc == 3)
                        m2 = m2p.get()
                        ACTF(m2, m2[:], pm, pm[:], ACT.Square)
                        rs = rsp.get()
                        TT("dve", rs, rs[:], pe2, pe2[:], m2, m2[:], ALU.subtract)
                        TS("dve", rs, rs[:], rs, rs[:], EPS, None, ALU.add)
                        RSTD(rs, rs[:])
                        for cc in range(4):
                            t = tp.get()
                            TT("dve", t, t[:], acc, acc[:, cc, qs], pm, pm[:], ALU.subtract)
                            TT("pool", t, t[:], t, t[:], rs, rs[:], ALU.mult)
                            ACTF(oT["conv"], oT["conv"][:, cc, qs], t, t[:], ACT.Silu,
                                 bias=pp[:, PP_LB + cc:PP_LB + cc + 1], scale=pp[:, PP_LG + cc:PP_LG + cc + 1], reads=[pp])
                S.barrier()
                if dbg == "conv":
                    return "stop"
                with ExitStack() as st2:
                    aqM = [S.sb(f"a_qM{i}", [P, 4, SEQ], BF16, st2) for i in range(2)]
                    MSET("pool", aqM[0], aqM[0][:], 0.0)
                    MSET("pool", aqM[1], aqM[1][:], 0.0)
                    akT = S.sb("a_kT", [P, 4, SEQ], BF16, st2)
                    vaug = S.sb("a_v", [P, NT, 8, 66], BF16, st2)
                    biasT = S.sb("a_bias", [P, 5, 8, P], F32, st2)
                    S.dma("sp", biasT, biasT[:], wts_d, biasx_h[l])
                    MSET("pool", biasT, biasT[0:64, 0, :, 64:128], NEG)
                    MSET("pool", biasT, biasT[64:128, 4, :, 0:64], NEG)
                    MSET("pool", vaug, vaug[:], 1.0)
                    with ExitStack() as st3:
                        sqp = Pool("a_sq", [P, 512], BF16, 2, st3)
                        rsp = Pool("a_rs", [P, 512], F32, 2, st3)
                        for dst, cbase, gcol in ((None, C_AQ, PP_QG), (akT, C_AK, PP_KG)):
                            for c in range(4):
                                wb = load_f(cbase + c * P)
                                for q in range(NQ):
                                    qs = slice(q * 512, (q + 1) * 512)
                                    pa, pb = ps[2 * (q % 2)], ps[2 * (q % 2) + 1]
                                    f_mm(wb, q, pa)
                                    sq = sqp.get()
                                    ACTF(sq, sq[:], pa, pa[:], ACT.Square)
                                    MM(pb, pb[:], blk64, blk64[:], sq, sq[:])
                                    rs = rsp.get()
                                    TS("dve", rs, rs[:], pb, pb[:], EPS, None, ALU.add)
                                    RSTD(rs, rs[:])
                                    if dst is None:
                                        for i in range(2):
                                            hp = slice(64 * i, 64 * i + 64)
                                            STT("dve", aqM[i], aqM[i][hp, c, qs], pa, pa[hp, :], pp[hp, gcol:gcol + 1], rs, rs[hp, :],
                                                ALU.mult, ALU.mult, reads=[pp])
                                    else:
                                        STT("dve", dst, dst[:, c, qs], pa, pa[:], pp[:, gcol:gcol + 1], rs, rs[:],
                                            ALU.mult, ALU.mult, reads=[pp])
                        tpool = Pool("tblk", [P, KC, 512], BF16, 1, st3)
                        wb = tpool.get()
                        S.dma("pool", wb, wb[:], wts_d, wcols(w_in, C_AV, 512))
                        for j in range(NT if dbg != "att0" else 0):
                            q, jj = divmod(j, 4)
                            pt = ps[4 + j % 2]
                            for kc in range(KC):
                                MM(pt, pt[:], hT[q], hT[q][:, kc, jj * P:(jj + 1) * P], wb, wb[:, kc, :], kc == 0, kc == KC - 1)
                            CP("act", vaug, vaug[:, j, :, 0:64], pt, pt[:].rearrange("p (h d) -> p h d", h=8))
                    S.barrier()
                    ep = Pool("a_e", [P, 512], F32, 2, st2)
                    pTp = Pool("a_pT", [P, 5, 512], BF16, 2, st2)
                    recp = Pool("a_rec", [P, 8], F32, 2, st2)
                    oap = Pool("a_o", [P, 512], F32, 2, st2)
                    for j in range(NT if dbg not in ("att0", "att1") else 0):
                        tsl = slice(j * P, (j + 1) * P)
                        kbs = [kb for kb in range(5) if j - 4 + kb >= 0]
                        oa = oap.get()
                        rec = recp.get()
                        for g in range(2):
                            pTt = pTp.get()
                            for kb in kbs:
                                jk = j - 4 + kb
                                ksl = slice(jk * P, (jk + 1) * P)
                                pq = ps[kb % 2 + 2 * g]
                                for hh in range(4):
                                    h = g * 4 + hh
                                    c, r0 = h // 2, 64 * (h % 2)
                                    MM(pq, pq[:, hh * P:(hh + 1) * P], akT, akT[:, c, ksl], aqM[h % 2], aqM[h % 2][:, c, tsl])
                                e_ = ep.get()
                                STT("dve", e_, e_[:].rearrange("p (h q) -> p h q", h=4), pq,
                                    pq[:].rearrange("p (h q) -> p h q", h=4), 0.125,
                                    biasT, biasT[:, kb, g * 4:(g + 1) * 4, :], ALU.mult, ALU.add)
                                ACTF(pTt, pTt[:, kb, :], e_, e_[:], ACT.Exp)
                            po = ps[4 + g]
                            if dbg == "att2":
                                continue
                            for hh in range(4):
                                h = g * 4 + hh
                                for i, kb in enumerate(kbs):
                                    jk = j - 4 + kb
                                    MM(po, po[:, hh * 66:(hh + 1) * 66], pTt, pTt[:, kb, hh * P:(hh + 1) * P],
                                       vaug, vaug[:, jk, h, :], i == 0, i == len(kbs) - 1)
                            pov = po[:, 0:264].rearrange("p (h d) -> p h d", h=4)
                            S.op("dve", lambda e, rec=rec, pov=pov, g=g: e.reciprocal(
                                out=rec[:, g * 4:(g + 1) * 4].unsqueeze(2), in_=pov[:, :, 64:65]), [po], [rec])
                            for hh in range(4):
                                h = g * 4 + hh
                                TS("dve", oa, oa[:, h * 64:(h + 1) * 64], po, po[:, hh * 66:hh * 66 + 64],
                                   rec[:, h:h + 1], None, ALU.mult, reads=[rec])
                        if dbg in ("att2", "att3"):
                            continue
                        pX = ps[6 + j % 2]
                        for c in range(4):
                            TR(pX, pX[:, c * P:(c + 1) * P], oa, oa[:, c * P:(c + 1) * P], *ident)
                        CP("act", oT["att"], oT["att"][:, :, tsl], pX, pX[:].rearrange("p (c t) -> p c t", c=4))
                for i_, k_ in enumerate(("ret", "conv", "att")):
                    DUMP(1 + i_, oT[k_], oT[k_][:].rearrange("p k t -> p (k t)"), 8192)
                S.barrier()
                if dbg and dbg.startswith("att"):
                    return "stop"
                with ExitStack() as st2:
                    mT = [S.sb(f"mT{q}", [P, KC, 512], BF16, st2) for q in range(NQ)]
                    glp = Pool("s4_gl", [P, KC, 3, P], BF16, 2, st2)
                    wop = Pool("s4_wo", [P, 4, 3, P], BF16, 2, st2)
                    gtp = Pool("s4_g", [P, 512], F32, 3, st2)
                    tmp = Pool("s4_t", [P, 512], F32, 2, st2)
                    macc = Pool("s4_m", [P, 512], F32, 2, st2)
                    outs_w = (w_ro_h[l], w_co_h[l], w_ao_h[l])
                    keys = ("ret", "conv", "att")
                    for c in range(KC):
                        gw = glp.get()
                        ow = wop.get()
                        for b in range(3):
                            S.dma("pool", gw, gw[:, :, b, :], wts_d, wcols(w_in, C_GL + b * D + c * P, P))
                            S.dma("pool", ow, ow[:, :, b, :], wts_d, wcols(outs_w[b], c * P, P))
                        for q in range(NQ):
                            qs = slice(q * 512, (q + 1) * 512)
                            m = macc.get()
                            for b in range(3):
                                pg, py = ps[2 * (b % 2)], ps[2 * (b % 2) + 1]
                                for kc in range(KC):
                                    MM(pg, pg[:], gw, gw[:, kc, b, :], hT[q], hT[q][:, kc, :], kc == 0, kc == KC - 1)
                                for kc in range(4):
                                    MM(py, py[:], ow, ow[:, kc, b, :], oT[keys[b]], oT[keys[b]][:, kc, qs], kc == 0, kc == 3)
                                gt = gtp.get()
                                ACTF(gt, gt[:], pg, pg[:], ACT.Sigmoid,
                                     bias=pp[:, PP_BG + b * 8 + c:PP_BG + b * 8 + c + 1], reads=[pp])
                                if b == 0:
                                    TT("dve", m, m[:], py, py[:], gt, gt[:], ALU.mult)
                                elif b == 1:
                                    t = tmp.get()
                                    TT("dve", t, t[:], py, py[:], gt, gt[:], ALU.mult)
                                    TT("pool", m, m[:], m, m[:], t, t[:], ALU.add)
                                else:
                                    t = tmp.get()
                                    TT("dve", t, t[:], py, py[:], gt, gt[:], ALU.mult)
                                    TT("pool", mT[q], mT[q][:, c, :], m, m[:], t, t[:], ALU.add)
                    DUMP(4, mT[0], mT[0][:].rearrange("p k t -> p (k t)"), 4096)
                    wo = S.sb("s4_wout", [P, KC, D], BF16, st2)
                    S.dma("pool", wo, wo[:], wts_d, wcols(w_out_h[l], 0, D))
                    gate1 = load_mod_bc(st2, l, s, 2, "gate1")
                    xs_pool = Pool("s4_xs", [P, D], F32, 2, st2)
                    for j in range(NT):
                        q, jj = divmod(j, 4)
                        xs = xs_pool.get()
                        if first:
                            S.dma("sp", xs, xs[:], x_d, x_h[s, j * P:(j + 1) * P, :])
                        else:
                            S.dma("sp", xs, xs[:], xo_d[s][j], out_h[s, j * P:(j + 1) * P, :])
                        for half in range(2):
                            pt = ps[4 + (2 * j + half) % 4]
                            hsl = slice(half * 512, (half + 1) * 512)
                            for kc in range(KC):
                                MM(pt, pt[:], mT[q], mT[q][:, kc, jj * P:(jj + 1) * P], wo, wo[:, kc, hsl], kc == 0, kc == KC - 1)
                            t = tmp.get()
                            TT("dve", t, t[:], pt, pt[:], gate1, gate1[:, hsl], ALU.mult)
                            TT("pool", xs, xs[:, hsl], xs, xs[:, hsl], t, t[:], ALU.add)
                        S.dma("sp", xo_d[s][j], out_h[s, j * P:(j + 1) * P, :], xs, xs[:], sem_t=xs)
                S.barrier()
            S.barrier()

        def moe(s, l):
            with ExitStack() as st:
                hT = [S.sb(f"hT{q}", [P, KC, 512], BF16, st) for q in range(NQ)]
                acc = S.sb("m_acc", [P, NT, D], F32, st)
                wge = S.sb("m_wge", [P, NT, 32], F32, st)
                with ExitStack() as st1:
                    gmod = load_mod_bc(st1, l, s, 4, "gmod2")
                    shift = load_mod_bc(st1, l, s, 3, "shift2")
                    wr = S.sb("m_wr", [P, KC, 36], F32, st1)
                    S.dma("sp", wr, wr[:], wts_d, wr_h[l])
                    brow = S.sb("m_brow", [P, 36], F32, st1)
                    S.dma("sp", brow, brow[:], wts_d, brow_h[l:l + 1, :].partition_broadcast(P))
                    whi = S.sb("m_whi", [P, KC, 36], BF16, st1)
                    wlo = S.sb("m_wlo", [P, KC, 36], BF16, st1)
                    CP("dve", whi, whi[:], wr, wr[:])
                    TT("dve", wlo, wlo[:], wr, wr[:], whi, whi[:], ALU.subtract)
                    h2p = Pool("m_h2lo", [P, KC, P], BF16, 2, st1)
                    rp = Pool("m_r", [P, 96], F32, 2, st1)

                    def router(j, pa, pb):
                        q, jj = divmod(j, 4)
                        tsl = slice(jj * P, (jj + 1) * P)
                        h2 = h2p.get()
                        TT("dve", h2, h2[:, 0:4, :], pa, pa[:].rearrange("p (k t) -> p k t", k=4),
                           hT[q], hT[q][:, 0:4, tsl], ALU.subtract)
                        TT("dve", h2, h2[:, 4:8, :], pb, pb[:].rearrange("p (k t) -> p k t", k=4),
                           hT[q], hT[q][:, 4:8, tsl], ALU.subtract)
                        pl = ps[4 + j % 2]
                        for kc in range(KC):
                            MM(pl, pl[:, 0:36], hT[q], hT[q][:, kc, tsl], whi, whi[:, kc, :], kc == 0, False)
                            MM(pl, pl[:, 0:36], hT[q], hT[q][:, kc, tsl], wlo, wlo[:, kc, :], False, False)
                            MM(pl, pl[:, 0:36], h2, h2[:, kc, :], whi, whi[:, kc, :], False, kc == KC - 1)
                        r = rp.get()
                        MSET("dve", r, r[:], 0.0)
                        LG, GMX, OHG, NGM, GS, CH, M1, OH1, CH2, M2, OH2, DD, W1, W2, WE, EX = (
                            slice(0, 36), slice(36, 37), slice(37, 41), slice(41, 42), slice(42, 43), slice(43, 51),
                            slice(51, 52), slice(52, 60), slice(60, 68), slice(68, 69), slice(69, 77), slice(77, 78),
                            slice(78, 79), slice(79, 80), slice(80, 88), slice(88, 92))
                        R = lambda sl: r[:, sl]
                        TT("dve", r, R(LG), pl, pl[:, 0:36], brow, brow[:], ALU.add)
                        if dbg == "m_r1":
                            return
                        S.op("dve", lambda e: e.tensor_reduce(out=R(GMX), in_=r[:, 0:4], axis=AX.X, op=ALU.max), [r], [r])
                        TS("dve", r, R(OHG), r, r[:, 0:4], R(GMX), None, ALU.is_ge)
                        TS("dve", r, R(NGM), r, R(GMX), -1.0, None, ALU.mult)
                        ACTF(r, R(EX), r, r[:, 0:4], ACT.Exp, bias=R(NGM), accum=(r, R(GS)))
                        S.op("dve", lambda e: e.reciprocal(out=R(GS), in_=R(GS)), [r], [r])
                        if dbg == "m_r2":
                            return
                        TS("dve", r, R(CH), r, r[:, 4:12], r[:, 37:38], None, ALU.mult)
                        for g in range(1, 4):
                            STT("dve", r, R(CH), r, r[:, 4 + g * 8:12 + g * 8], r[:, 37 + g:38 + g], r, R(CH), ALU.mult, ALU.add)
                        S.op("dve", lambda e: e.tensor_reduce(out=R(M1), in_=R(CH), axis=AX.X, op=ALU.max), [r], [r])
                        TS("dve", r, R(OH1), r, R(CH), R(M1), None, ALU.is_ge)
                        STT("dve", r, R(CH2), r, R(OH1), NEG, r, R(CH), ALU.mult, ALU.add)
                        S.op("dve", lambda e: e.tensor_reduce(out=R(M2), in_=R(CH2), axis=AX.X, op=ALU.max), [r], [r])
                        TS("dve", r, R(OH2), r, R(CH2), R(M2), None, ALU.is_ge)
                        TT("dve", r, R(DD), r, R(M2), r, R(M1), ALU.subtract)
                        ACTF(r, R(W2), r, R(DD), ACT.Sigmoid)
                        TS("dve", r, R(W1), r, R(W2), -1.0, 1.0, ALU.mult, ALU.add)
                        TT("dve", r, R(W1), r, R(W1), r, R(GS), ALU.mult)
                        TT("dve", r, R(W2), r, R(W2), r, R(GS), ALU.mult)
                        TS("dve", r, R(WE), r, R(OH1), R(W1), None, ALU.mult)
                        STT("dve", r, R(WE), r, R(OH2), R(W2), r, R(WE), ALU.mult, ALU.add)
                        if dbg == "m_r3":
                            return
                        for g in range(4):
                            TS("dve", wge, wge[:, j, g * 8:(g + 1) * 8], r, R(WE), r[:, 37 + g:38 + g], None, ALU.mult, reads=[r])

                    norm_and_transpose(st1, l, s, False, gmod, shift, hT, h2f_cb=(None if dbg == "m_norouter" else router))
                S.barrier()
                with ExitStack() as st2:
                    upp = Pool("m_wup", [P, KC, 512], BF16, 2, st2)
                    dnp = Pool("m_wdn", [P, 2, D], BF16, 2, st2)
                    sap = Pool("m_sa", [P, 512], F32, 2, st2)
                    actp = Pool("m_act", [P, 2, 512], BF16, 2, st2)
                    dcount = 0
                    for ge in range(n_experts):
                        g, e_ = divmod(ge, NE)
                        wu = upp.get()
                        wd = dnp.get()
                        S.dma("pool", wu, wu[:], wts_d, wcols(w_up_h[l, g, e_], 0, 512))
                        S.dma("pool", wd, wd[:], wts_d, w_dn_h[l, g, e_].rearrange("(fc p) n -> p fc n", p=P))
                        for q in range(NQ):
                            for fc in range(4):
                                pu = ps[fc]
                                for kc in range(KC):
                                    MM(pu, pu[:], wu, wu[:, kc, fc * P:(fc + 1) * P], hT[q], hT[q][:, kc, :], kc == 0, kc == KC - 1)
                            at = actp.get()
                            for fc in range(2):
                                sa = sap.get()
                                ACTF(sa, sa[:], ps[fc], ps[fc][:], ACT.Silu)
                                TT("dve", at, at[:, fc, :], sa, sa[:], ps[fc + 2], ps[fc + 2][:], ALU.mult)
                            for jj in range(4):
                                j = q * 4 + jj
                                for half in range(2):
                                    pd = ps[4 + dcount % 4]
                                    dcount += 1
                                    hsl = slice(half * 512, (half + 1) * 512)
                                    for fc in range(2):
                                        MM(pd, pd[:], at, at[:, fc, jj * P:(jj + 1) * P], wd, wd[:, fc, hsl], fc == 0, fc == 1)
                                    if ge == 0:
                                        TS("dve", acc, acc[:, j, hsl], pd, pd[:], wge[:, j, ge:ge + 1], None, ALU.mult, reads=[wge])
                                    else:
                                        STT("dve", acc, acc[:, j, hsl], pd, pd[:], wge[:, j, ge:ge + 1], acc, acc[:, j, hsl],
                                            ALU.mult, ALU.add, reads=[wge])
                    gate2 = load_mod_bc(st2, l, s, 5, "gate2")
                    xs_pool = Pool("m_xs", [P, D], F32, 2, st2)
                    toks = []
                    for j in range(NT):
                        xs = xs_pool.get()
                        S.dma("sp", xs, xs[:], xo_d[s][j], out_h[s, j * P:(j + 1) * P, :])
                        TT("pool", acc, acc[:, j, :], acc, acc[:, j, :], gate2, gate2[:], ALU.mult)
                        TT("pool", xs, xs[:], xs, xs[:], acc, acc[:, j, :], ALU.add)
                        toks.append(S.dma("sp", xo_d[s][j], out_h[s, j * P:(j + 1) * P, :], xs, xs[:], sem_t=xs))
                S.barrier()
                return toks

        identb = S.sb("identb", [P, P], BF16)
        CP("dve", identb, identb[:], cst, cst[:, CS_ID:CS_ID + P])

        final = []
        try:
            if dbg and dbg.startswith("pro"):
                raise _Stop()
            for s in range(n_seq):
                for l in range(n_layers):
                    if mixer(s, l) == "stop":
                        raise _Stop()
                    final = moe(s, l) if n_experts > 0 else []
        except _Stop:
            S.barrier()
        S.emit(final_toks=S.dma_toks + final + dump_toks)
    return nc


def _constants():
    cst = np.zeros((P, NCST), np.float32)
    cst[:, CS_ID:CS_ID + P] = np.eye(P, dtype=np.float32)
    m = np.arange(P)[:, None].astype(np.float64)
    c = np.arange(P)[None, :].astype(np.float64)
    for h in range(4):
        gamma = 1.0 - 2.0 ** (-5 - h)
        mask = np.where(c >= m, gamma ** np.maximum(c - m, 0.0), 0.0) * (128.0 ** -0.5)
        cst[:, CS_MASK + h * P:CS_MASK + (h + 1) * P] = mask
        cst[:, CS_QDEC + h * P:CS_QDEC + (h + 1) * P] = (gamma ** (c + 1.0))
        cst[:, CS_KDEC + h] = (gamma ** (127.0 - m[:, 0])) * (128.0 ** -0.5)
    half = 64
    inv = (np.float32(10000.0) ** (-np.arange(half, dtype=np.float32) / np.float32(half))).astype(np.float32)
    cst[:, CS_INV] = np.tile(inv, 2)
    cst[0:64, CS_SGN] = -1.0
    cst[64:128, CS_SGN] = 1.0
    cst[0:64, CS_MLO] = 1.0
    cst[64:128, CS_MHI] = 1.0
    return cst


def _layout_params(inp):
    f32 = np.float32
    pp = np.zeros((L, P, NPP), f32)
    cw = np.asarray(inp["conv_w"], f32)[:, :, 0, :]
    pp[:, :, PP_CW:PP_CW + 124] = cw.reshape(L, 31, 4, P).transpose(0, 3, 2, 1).reshape(L, P, 124)
    pp[:, :, PP_CB:PP_CB + 4] = np.asarray(inp["conv_b"], f32).reshape(L, 4, P).transpose(0, 2, 1)
    pp[:, :, PP_LG:PP_LG + 4] = np.asarray(inp["conv_ln_g"], f32).reshape(L, 4, P).transpose(0, 2, 1)
    pp[:, :, PP_LB:PP_LB + 4] = np.asarray(inp["conv_ln_b"], f32).reshape(L, 4, P).transpose(0, 2, 1)
    pp[:, :, PP_QG] = np.tile(np.asarray(inp["att_q_gain"], f32), (1, 2))
    pp[:, :, PP_KG] = np.tile(np.asarray(inp["att_k_gain"], f32), (1, 2))
    pp[:, :, PP_BG:PP_BG + 24] = np.asarray(inp["b_gate"], f32).reshape(L, 24, P).transpose(0, 2, 1)
    wr = np.concatenate([np.asarray(inp["w_group"], f32), np.asarray(inp["w_inner"], f32)], axis=-1)
    wr = np.ascontiguousarray(wr.reshape(L, KC, P, 36).transpose(0, 2, 1, 3))
    brow = np.ascontiguousarray(np.concatenate([np.asarray(inp["b_group"], f32), np.asarray(inp["b_inner"], f32)], axis=-1))
    k = np.arange(P)[:, None, None]
    kb = np.arange(5)[None, :, None]
    q = np.arange(P)[None, None, :]
    idx = np.clip(q - k + 128 * (4 - kb), -256, 256) + 256
    tab = np.asarray(inp["att_rel_bias"], f32)
    bx = tab[:, :, idx]
    biasx = np.ascontiguousarray(bx.transpose(0, 2, 3, 1, 4))
    return pp, wr, brow, biasx


_PROG = {}


def kernel(**inp):
    n = 8
    f32 = np.float32
    x = np.asarray(inp["x"], f32)
    c = np.asarray(inp["c"], f32)
    pos = np.asarray(inp["positions"], np.int32)
    pp, wr, brow, biasx = _layout_params(inp)
    cst = _constants()
    shared = dict(
        w_ada=np.asarray(inp["w_ada"], f32), b_ada=np.asarray(inp["b_ada"], f32),
        g_mix=np.asarray(inp["g_mix"], f32), g_ffn=np.asarray(inp["g_ffn"], f32),
        w_in=np.asarray(inp["w_in"], f32), ret_gn=np.asarray(inp["ret_gn"], f32),
        w_ret_out=np.asarray(inp["w_ret_out"], f32), w_conv_out=np.asarray(inp["w_conv_out"], f32),
        w_att_out=np.asarray(inp["w_att_out"], f32), w_out=np.asarray(inp["w_out"], f32),
        w_up=np.asarray(inp["w_up"], f32), w_down=np.asarray(inp["w_down"], f32),
        pp=pp, wr=wr, brow=brow, biasx=biasx, cst=cst)
    if "nc" not in _PROG:
        _PROG["nc"] = build_program()
    nc = _PROG["nc"]
    in_maps = []
    for i in range(n):
        sl = slice(NSEQ * i, NSEQ * (i + 1))
        m = dict(shared)
        m["x"] = np.ascontiguousarray(x[sl])
        m["pos"] = np.ascontiguousarray(pos[sl])
        m["cT"] = np.ascontiguousarray(c[sl].reshape(NSEQ, KC, P).transpose(2, 1, 0))
        in_maps.append(m)
    res = run_bass_kernel_spmd(nc, in_maps, core_ids=list(range(n)))
    return np.concatenate([np.asarray(r["out"], f32) for r in res.results], axis=0)
```

```python
import math
import numpy as np
from concourse.bass_utils import run_bass_kernel_spmd

import numpy as np
import concourse.bass as bass
import concourse.mybir as mybir

F32 = mybir.dt.float32
BF16 = mybir.dt.bfloat16
I32 = mybir.dt.int32
ALU = mybir.AluOpType
ACT = mybir.ActivationFunctionType
AX = mybir.AxisListType

SAME_ENGINE_SYNC = False
SMALL_N = 512


class Op:
    __slots__ = ("eng", "fn", "deps", "signal", "semval", "dma_inc", "idx", "small")

    def __init__(self, eng, fn, deps, dma_inc=None):
        self.eng = eng
        self.fn = fn
        self.deps = deps
        self.signal = False
        self.semval = None
        self.dma_inc = dma_inc
        self.idx = None
        self.small = False


class DmaTok:
    __slots__ = ("sem", "val", "eng")

    def __init__(self, sem, val):
        self.sem = sem
        self.val = val
        self.eng = None


class T:
    def __init__(self, h, name=""):
        self.h = h
        self.name = name
        self.last_w = None
        self.readers = []
        self.dsem = None
        self.dcount = 0

    def __getitem__(self, k):
        return self.h[k]


class Sched:
    ENGS = ("pe", "act", "dve", "pool", "sp")

    def __init__(self, nc):
        self.nc = nc
        self.ops = {e: [] for e in self.ENGS}
        self.sems = {}
        self.stack = None
        self.dma_sems = []
        self.free_dma_sems = []
        self.dma_toks = []
        self.uid = 0

    def set_stack(self, stack):
        self.stack = stack

    def sem(self, name):
        return self.stack.enter_context(self.nc.semaphore(name))

    def sb(self, name, shape, dtype, stack=None):
        st = stack or self.stack
        self.uid += 1
        name = f"{name}_{self.uid}"
        h = st.enter_context(self.nc.sbuf_tensor(name, list(shape), dtype))
        t = T(h, name)
        if st is not self.stack:
            st.callback(self._retire, t)
        return t

    def _retire(self, t):
        if t.dsem is not None:
            self.free_dma_sems.append((t.dsem, t.dcount))
            t.dsem = None

    def ps(self, name, shape, dtype=F32, stack=None):
        st = stack or self.stack
        h = st.enter_context(self.nc.psum_tensor(name, list(shape), dtype))
        return T(h, name)

    def dram(self, h, name=""):
        return T(h, name)

    def _deps(self, reads, writes):
        deps = []
        for t in reads:
            if t.last_w is not None:
                deps.append(t.last_w)
        for t in writes:
            if t.last_w is not None:
                deps.append(t.last_w)
            deps.extend(t.readers)
        return deps

    def _commit(self, tok, reads, writes):
        for t in reads:
            t.readers.append(tok)
            if len(t.readers) > 64:
                t.readers = self._compact(t.readers)
        for t in writes:
            t.last_w = tok
            t.readers = []

    @staticmethod
    def _compact(toks):
        last = {}
        for tk in toks:
            key = (tk.eng if isinstance(tk, Op) else id(tk.sem))
            last[key] = tk
        return list(last.values())

    def op(self, eng, fn, reads=(), writes=(), small=True):
        o = Op(eng, fn, self._deps(reads, writes))
        o.small = bool(small) and eng != "pe"
        o.idx = len(self.ops[eng])
        self.ops[eng].append(o)
        self._commit(o, reads, writes)
        return o

    def dma(self, q, out_t, out_ap, in_t, in_ap, sem_t=None, **kw):
        st = sem_t or out_t
        if st.dsem is None:
            if self.free_dma_sems:
                st.dsem, st.dcount = self.free_dma_sems.pop()
            else:
                st.dsem = self.sem("d_" + st.name)
                st.dcount = 0
        st.dcount += 16
        tok = DmaTok(st.dsem, st.dcount)
        deps = self._deps([in_t], [out_t])
        sem = st.dsem

        def fn(e, out_ap=out_ap, in_ap=in_ap, kw=kw):
            return e.dma_start(out=out_ap, in_=in_ap, **kw)

        o = Op(q, fn, deps, dma_inc=sem)
        o.idx = len(self.ops[q])
        self.ops[q].append(o)
        self._commit(tok, [in_t], [out_t])
        self.dma_toks.append(tok)
        return tok

    def barrier(self):
        lasts = []
        for e in self.ENGS:
            for o in reversed(self.ops[e]):
                if o.fn is not None and o.dma_inc is None:
                    lasts.append(o)
                    break
        toks = list(self.dma_toks)
        self.dma_toks = []
        for e in self.ENGS:
            deps = [o for o in lasts if o.eng != e] + toks
            if not deps:
                continue
            o = Op(e, None, deps)
            o.idx = len(self.ops[e])
            self.ops[e].append(o)

    def finalize(self):
        for e in self.ENGS:
            for o in self.ops[e]:
                for d in o.deps:
                    if isinstance(d, Op):
                        if d.eng != o.eng or SAME_ENGINE_SYNC or d.small:
                            d.signal = True
        self.counts = {}
        for e in self.ENGS:
            c = 0
            for o in self.ops[e]:
                if o.signal:
                    c += 1
                    o.semval = c
            self.counts[e] = c
            if c > 0 or True:
                self.sems[e] = self.sem("eng_" + e)

    def replay(self, e, handle):
        seen = {}
        nc = self.nc
        for o in self.ops[e]:
            waits = {}
            for d in o.deps:
                if isinstance(d, Op):
                    if d.eng == e and not (SAME_ENGINE_SYNC or d.small):
                        continue
                    key = ("e", d.eng)
                    sem = self.sems[d.eng]
                    val = d.semval
                else:
                    key = ("d", id(d.sem))
                    sem = d.sem
                    val = d.val
                if seen.get(key, 0) >= val:
                    continue
                if key not in waits or waits[key][1] < val:
                    waits[key] = (sem, val)
            for key, (sem, val) in waits.items():
                handle.wait_ge(sem, val)
                seen[key] = val
            if o.fn is None:
                continue
            ins = o.fn(handle)
            if o.dma_inc is not None:
                ins.then_inc(o.dma_inc, 16)
            elif o.signal:
                ins.then_inc(self.sems[e], 1)

    def emit(self, final_toks=()):
        nc = self.nc
        self.finalize()
        with nc.Block() as block:
            @block.tensor
            def _(h):
                self.replay("pe", h)

            @block.scalar
            def _(h):
                self.replay("act", h)

            @block.vector
            def _(h):
                self.replay("dve", h)

            @block.gpsimd
            def _(h):
                self.replay("pool", h)

            @block.sync
            def _(h):
                self.replay("sp", h)
                for tk in final_toks:
                    h.wait_ge(tk.sem, tk.val)

from contextlib import ExitStack

P = 128
SEQ = 2048
D = 1024
KC = 8
NT = 16
NQ = 4
L = 2
NSEQ = 2
IN_COLS = 7680
EPS = 1e-6
NEG = -1e30
C_RQ, C_RK, C_RV, C_RG = 0, 512, 1024, 1536
C_CU = 2048
C_AQ, C_AK, C_AV = 3072, 3584, 4096
C_GL = 4608
NG, NE = 4, 8
PP_CW = 0
PP_CB = 124
PP_LG = 128
PP_LB = 132
PP_QG = 136
PP_KG = 137
PP_BG = 138
NPP = 162
CS_ID = 0
CS_MASK = 128
CS_QDEC = 640
CS_KDEC = 1152
CS_INV = 1156
CS_SGN = 1157
CS_MLO = 1158
CS_MHI = 1159
NCST = 1160
RET_CD = [float((1.0 - 2.0 ** (-5 - h)) ** 128) for h in range(4)]


def build_program(n_layers=L, n_seq=NSEQ, n_experts=NG * NE, dbg=None, dump=False):
    nc = bass.Bass("TRN2", target_bir_lowering=False)
    dump_toks = []

    def din(name, shape, dt=F32):
        return nc.dram_tensor(name, list(shape), dt, kind="ExternalInput")

    x_h = din("x", [NSEQ, SEQ, D])
    pos_h = din("pos", [NSEQ, SEQ], I32)
    cT_h = din("cT", [P, KC, NSEQ])
    w_ada_h = din("w_ada", [L, D, 6 * D])
    b_ada_h = din("b_ada", [L, 6 * D])
    g_mix_h = din("g_mix", [L, D])
    g_ffn_h = din("g_ffn", [L, D])
    w_in_h = din("w_in", [L, D, IN_COLS])
    ret_gn_h = din("ret_gn", [L, 512])
    w_ro_h = din("w_ret_out", [L, 512, D])
    w_co_h = din("w_conv_out", [L, 512, D])
    w_ao_h = din("w_att_out", [L, 512, D])
    w_out_h = din("w_out", [L, D, D])
    w_up_h = din("w_up", [L, NG, NE, D, 512])
    w_dn_h = din("w_down", [L, NG, NE, 256, D])
    pp_h = din("pp", [L, P, NPP])
    wr_h = din("wr", [L, P, KC, 36])
    brow_h = din("brow", [L, 36])
    biasx_h = din("biasx", [L, P, 5, 8, P])
    cst_h = din("cst", [P, NCST])
    out_h = nc.dram_tensor("out", [NSEQ, SEQ, D], F32, kind="ExternalOutput")
    modd_h = nc.dram_tensor("modd", [L, NSEQ, 6 * D], F32)
    dbg_h = nc.dram_tensor("dbg", [P, 6, 8192], F32, kind="ExternalOutput") if dump else None

    top = ExitStack()
    with top:
        S = Sched(nc)
        S.set_stack(top)

        def MM(out_t, out_ap, lt, lap, rt, rap, start=True, stop=True):
            S.op("pe", lambda e: e.matmul(out_ap, lhsT=lap, rhs=rap, start=start, stop=stop),
                 [lt, rt], [out_t])

        def TR(out_t, out_ap, in_t, in_ap, id_t, id_ap):
            S.op("pe", lambda e: e.transpose(out=out_ap, in_=in_ap, identity=id_ap), [in_t, id_t], [out_t])

        def ACTF(out_t, out_ap, in_t, in_ap, func, bias=None, scale=None, accum=None, reads=()):
            kw = {}
            if bias is not None:
                kw["bias"] = bias
            if scale is not None:
                kw["scale"] = scale
            wr = [out_t]
            if accum is not None:
                kw["accum_out"] = accum[1]
                wr.append(accum[0])
            S.op("act", lambda e: e.activation(out=out_ap, in_=in_ap, func=func, **kw),
                 [in_t] + list(reads), wr, small=(accum is not None or out_ap.free_size() < SMALL_N))

        def TT(eng, out_t, out_ap, a_t, a_ap, b_t, b_ap, op):
            S.op(eng, lambda e: e.tensor_tensor(out=out_ap, in0=a_ap, in1=b_ap, op=op), [a_t, b_t], [out_t],
                 small=out_ap.free_size() < SMALL_N)

        def TS(eng, out_t, out_ap, a_t, a_ap, s1, s2, op0, op1=None, reads=()):
            if op1 is None:
                S.op(eng, lambda e: e.tensor_scalar(out=out_ap, in0=a_ap, scalar1=s1, scalar2=None, op0=op0),
                     [a_t] + list(reads), [out_t], small=out_ap.free_size() < SMALL_N)
            else:
                S.op(eng, lambda e: e.tensor_scalar(out=out_ap, in0=a_ap, scalar1=s1, scalar2=s2, op0=op0, op1=op1),
                     [a_t] + list(reads), [out_t], small=out_ap.free_size() < SMALL_N)

        def STT(eng, out_t, out_ap, a_t, a_ap, sc, b_t, b_ap, op0, op1, reads=()):
            S.op(eng, lambda e: e.scalar_tensor_tensor(out=out_ap, in0=a_ap, scalar=sc, in1=b_ap, op0=op0, op1=op1),
                 [a_t, b_t] + list(reads), [out_t], small=out_ap.free_size() < SMALL_N)

        def CP(eng, out_t, out_ap, in_t, in_ap):
            if eng == "act":
                S.op("act", lambda e: e.copy(out=out_ap, in_=in_ap), [in_t], [out_t], small=out_ap.free_size() < SMALL_N)
            else:
                S.op(eng, lambda e: e.tensor_copy(out=out_ap, in_=in_ap), [in_t], [out_t], small=out_ap.free_size() < SMALL_N)

        def MSET(eng, t, ap, val):
            S.op(eng, lambda e: e.memset(ap, val), [], [t], small=ap.free_size() < SMALL_N)

        def RSTD(t, ap, n_is_one_col=True):
            S.op("dve", lambda e: e.reciprocal(out=ap, in_=ap), [t], [t])
            ACTF(t, ap, t, ap, ACT.Sqrt)

        class Pool:
            def __init__(self, name, shape, dt, n, stack):
                self.tiles = [S.sb(f"{name}{i}", shape, dt, stack) for i in range(n)]
                self.i = 0

            def get(self):
                t = self.tiles[self.i % len(self.tiles)]
                self.i += 1
                return t

        def wcols(ap2d, c0, n):
            return ap2d.rearrange("(kc p) n -> p kc n", p=P)[:, :, c0:c0 + n]

        DR = lambda h, name: S.dram(h, name)
        dbg_d = DR(dbg_h, "dbg") if dump else None

        def DUMP(slot, t, ap, ncols):
            if dump:
                dump_toks.append(S.dma("pool", dbg_d, dbg_h[:, slot, 0:ncols], t, ap, sem_t=t))
        x_d = DR(x_h, "x")
        pos_d = DR(pos_h, "pos")
        wts_d = DR(w_in_h, "weights")
        modd_d = DR(modd_h, "modd")
        xo_d = [[DR(out_h, f"xo{s}_{j}") for j in range(NT)] for s in range(NSEQ)]

        cst = S.sb("cst", [P, NCST], F32)
        S.dma("sp", cst, cst[:], wts_d, cst_h[:, :])
        ident = (cst, cst[:, CS_ID:CS_ID + P])
        ps = [S.ps(f"ps{i}", [P, 512]) for i in range(8)]
        blk64 = S.sb("blk64", [P, P], BF16)
        MSET("dve", blk64, blk64[:], 0.0)
        MSET("dve", blk64, blk64[0:64, 0:64], 1.0 / 64)
        MSET("dve", blk64, blk64[64:128, 64:128], 1.0 / 64)
        onesln = S.sb("onesln", [P, P], F32)
        MSET("dve", onesln, onesln[:], 1.0 / 512)

        with ExitStack() as st:
          if dbg != "pro0":
              cT = S.sb("cT", [P, KC, NSEQ], F32, st)
              scT = S.sb("scT", [P, KC, NSEQ], BF16, st)
              S.dma("sp", cT, cT[:], wts_d, cT_h[:, :, :])
              ACTF(scT, scT[:], cT, cT[:], ACT.Silu)
              modrow = S.sb("modrow", [NSEQ, 6 * D], F32, st)
              brow2 = S.sb("brow2", [NSEQ, 6 * D], F32, st)
              grow = S.sb("grow", [NSEQ, 2, D], F32, st)
              wpool = Pool("wada", [P, KC, 512], BF16, 2, st)
              for l in range(n_layers):
                  S.dma("sp", brow2, brow2[:], wts_d, b_ada_h[l:l + 1, :].partition_broadcast(NSEQ))
                  S.dma("sp", grow, grow[:, 0, :], wts_d, g_mix_h[l:l + 1, :].partition_broadcast(NSEQ))
                  S.dma("sp", grow, grow[:, 1, :], wts_d, g_ffn_h[l:l + 1, :].partition_broadcast(NSEQ))
                  if dbg == "pro1":
                      break
                  for cb in range(12):
                      wb = wpool.get()
                      S.dma("pool", wb, wb[:], wts_d, wcols(w_ada_h[l], cb * 512, 512))
                      pt = ps[cb % 2]
                      for kc in range(KC):
                          MM(pt, pt[0:NSEQ, :], scT, scT[:, kc, :], wb, wb[:, kc, :], kc == 0, kc == KC - 1)
                      TT("dve", modrow, modrow[:, cb * 512:(cb + 1) * 512], pt, pt[0:NSEQ, :],
                         brow2, brow2[:, cb * 512:(cb + 1) * 512], ALU.add)
                  if dbg == "pro2":
                      break
                  for i, sl in ((0, 1), (1, 4)):
                      TS("dve", modrow, modrow[:, sl * D:(sl + 1) * D], modrow, modrow[:, sl * D:(sl + 1) * D], 1.0, None, ALU.add)
                      TT("dve", modrow, modrow[:, sl * D:(sl + 1) * D], modrow, modrow[:, sl * D:(sl + 1) * D],
                         grow, grow[:, i, :], ALU.mult)
                  if dbg == "pro3":
                      break
                  S.dma("sp", modd_d, modd_h[l], modrow, modrow[:], sem_t=modrow)
        S.barrier()

        class _Stop(Exception):
            pass

        def load_mod_bc(st, l, s, idx, name):
            t = S.sb(name, [P, D], F32, st)
            S.dma("sp", t, t[:], modd_d, modd_h[l, s:s + 1, idx * D:(idx + 1) * D].partition_broadcast(P))
            return t

        def norm_and_transpose(st, l, s, first_layer_input, gmod, shift, hT, h2f_cb=None):
            xs_pool = Pool("xs", [P, D], F32, 2, st)
            hf_pool = Pool("hf", [P, D], F32, 2, st)
            junk = S.sb("junk", [P, D], BF16, st)
            ssq = Pool("ssq", [P, 8], F32, 2, st)
            for j in range(NT):
                xs = xs_pool.get()
                if first_layer_input:
                    S.dma("sp", xs, xs[:], x_d, x_h[s, j * P:(j + 1) * P, :])
                else:
                    S.dma("sp", xs, xs[:], xo_d[s][j], out_h[s, j * P:(j + 1) * P, :])
                ss = ssq.get()
                MSET("dve", ss, ss[:], 0.0)
                ACTF(junk, junk[:], xs, xs[:], ACT.Square, accum=(ss, ss[:, 0:1]))
                TS("dve", ss, ss[:, 0:1], ss, ss[:, 0:1], 1.0 / D, EPS, ALU.mult, ALU.add)
                S.op("dve", lambda e, ss=ss: e.reciprocal(out=ss[:, 0:1], in_=ss[:, 0:1]), [ss], [ss])
                ACTF(ss, ss[:, 0:1], ss, ss[:, 0:1], ACT.Sqrt)
                hf = hf_pool.get()
                STT("dve", hf, hf[:], xs, xs[:], ss[:, 0:1], gmod, gmod[:], ALU.mult, ALU.mult, reads=[ss])
                TT("pool", hf, hf[:], hf, hf[:], shift, shift[:], ALU.add)
                if j == 0 and first_layer_input:
                    for o_, (t_, n_) in enumerate(((hf, D), (gmod, D), (shift, D), (xs, D), (ss, 8))):
                        if dump:
                            dump_toks.append(S.dma("pool", dbg_d, dbg_h[:, 5, o_ * 1024:o_ * 1024 + n_], t_, t_[:, 0:n_], sem_t=t_))
                q, jj = divmod(j, 4)
                pa, pb = ps[(2 * j) % 4], ps[(2 * j) % 4 + 1]
                for kc in range(KC):
                    pt = pa if kc < 4 else pb
                    TR(pt, pt[:, (kc % 4) * P:(kc % 4 + 1) * P], hf, hf[:, kc * P:(kc + 1) * P], *ident)
                CP("act", hT[q], hT[q][:, 0:4, jj * P:(jj + 1) * P], pa, pa[:].rearrange("p (k t) -> p k t", k=4))
                CP("act", hT[q], hT[q][:, 4:8, jj * P:(jj + 1) * P], pb, pb[:].rearrange("p (k t) -> p k t", k=4))
                if h2f_cb is not None:
                    h2f_cb(j, pa, pb)

        def mixer(s, l):
            first = (l == 0)
            w_in = w_in_h[l]
            with ExitStack() as st:
                hT = [S.sb(f"hT{q}", [P, KC, 512], BF16, st) for q in range(NQ)]
                oT = {k: S.sb(f"oT_{k}", [P, 4, SEQ], BF16, st) for k in ("ret", "conv", "att")}
                pp = S.sb("pp", [P, NPP], F32, st)
                S.dma("sp", pp, pp[:], wts_d, pp_h[l])
                with ExitStack() as st1:
                    gmod = load_mod_bc(st1, l, s, 1, "gmod1")
                    shift = load_mod_bc(st1, l, s, 0, "shift1")
                    norm_and_transpose(st1, l, s, first, gmod, shift, hT)
                DUMP(0, hT[0], hT[0][:].rearrange("p k t -> p (k t)"), 4096)
                S.barrier()
                if dbg == "s1":
                    return "stop"
                fpool = Pool("fblk", [P, KC, P], BF16, 4, st)

                def fproj(c0, q, pt):
                    raise NotImplementedError

                def load_f(c0, swap=False):
                    wb = fpool.get()
                    if not swap:
                        S.dma("pool", wb, wb[:], wts_d, wcols(w_in, c0, P))
                    else:
                        S.dma("pool", wb, wb[:, :, 0:64], wts_d, wcols(w_in, c0 + 64, 64))
                        S.dma("pool", wb, wb[:, :, 64:128], wts_d, wcols(w_in, c0, 64))
                    return wb

                def f_mm(wb, q, pt):
                    for kc in range(KC):
                        MM(pt, pt[:], wb, wb[:, kc, :], hT[q], hT[q][:, kc, :], kc == 0, kc == KC - 1)

                with ExitStack() as st2:
                    cosT = S.sb("cosT", [P, SEQ], BF16, st2)
                    sinT = S.sb("sinT", [P, SEQ], BF16, st2)
                    with ExitStack() as st3:
                        posi = S.sb("posi", [P, SEQ], I32, st3)
                        u = S.sb("rope_u", [P, SEQ], F32, st3)
                        f = S.sb("rope_f", [P, SEQ], F32, st3)
                        ki = S.sb("rope_ki", [P, SEQ], I32, st3)
                        S.dma("sp", posi, posi[:], pos_d, pos_h[s:s + 1, :].partition_broadcast(P))
                        CP("dve", u, u[:], posi, posi[:])
                        TS("dve", u, u[:], u, u[:], cst[:, CS_INV:CS_INV + 1], 1.0 / (2.0 * math.pi), ALU.mult, ALU.mult, reads=[cst])
                        for tab, off, sgn in ((sinT, 0.0, True), (cosT, 0.25, False)):
                            if off != 0.0:
                                TS("dve", f, f[:], u, u[:], off, None, ALU.add)
                                src = f
                            else:
                                src = u
                            CP("dve", ki, ki[:], src, src[:])
                            fk = S.sb("rope_fk" + ("s" if sgn else "c"), [P, SEQ], F32, st3)
                            CP("dve", fk, fk[:], ki, ki[:])
                            TT("dve", fk, fk[:], src, src[:], fk, fk[:], ALU.subtract)
                            TS("dve", fk, fk[:], fk, fk[:], 0.49999, -0.49999, ALU.min, ALU.max)
                            if sgn:
                                ACTF(fk, fk[:], fk, fk[:], ACT.Sin, scale=2.0 * math.pi)
                                TS("dve", tab, tab[:], fk, fk[:], cst[:, CS_SGN:CS_SGN + 1], None, ALU.mult, reads=[cst])
                            else:
                                ACTF(tab, tab[:], fk, fk[:], ACT.Sin, scale=2.0 * math.pi)
                    S.barrier()
                    state_f = S.sb("state_f", [P, 4, P], F32, st2)
                    state_b = S.sb("state_b", [P, 4, P], BF16, st2)
                    gn_bc = S.sb("gn_bc", [P, 512], F32, st2)
                    S.dma("sp", gn_bc, gn_bc[:], wts_d, ret_gn_h[l:l + 1, :].partition_broadcast(P))
                    HS = SEQ // 2
                    qT = S.sb("r_qT", [P, 4, HS], BF16, st2)
                    kT = S.sb("r_kT", [P, 4, HS], BF16, st2)
                    v_tok = S.sb("r_v", [P, 8, 512], BF16, st2)
                    gs = S.sb("r_gs", [P, 8, 512], BF16, st2)
                    tpool = Pool("tblk", [P, KC, 512], BF16, 2, st2)
                    t1p = Pool("r_t1", [P, 512], F32, 2, st2)
                    t2p = Pool("r_t2", [P, 512], F32, 2, st2)
                    sTm_p = Pool("r_sTm", [P, 512], BF16, 2, st2)
                    qd_p = Pool("r_qd", [P, 4, P], BF16, 2, st2)
                    kt_p = Pool("r_kt", [P, 512], BF16, 2, st2)
                    on_p = Pool("r_on", [P, 512], F32, 2, st2)
                    og_p = Pool("r_og", [P, 512], F32, 2, st2)
                    st_p = Pool("r_stats", [P, 16], F32, 2, st2)
                    sgt_p = Pool("r_sg", [P, 512], F32, 2, st2)
                    junk = S.sb("r_junk", [P, P], BF16, st2)
                    for seg in range(2):
                        for which, cbase, dst in (("q", C_RQ, qT), ("k", C_RK, kT)):
                            for h in range(4):
                                wb = load_f(cbase + h * P)
                                wsw = load_f(cbase + h * P, swap=True)
                                for qq in range(2):
                                    q = seg * 2 + qq
                                    pa, pb = ps[2 * (qq % 2)], ps[2 * (qq % 2) + 1]
                                    f_mm(wb, q, pa)
                                    f_mm(wsw, q, pb)
                                    t1, t2 = t1p.get(), t2p.get()
                                    TT("dve", t1, t1[:], pa, pa[:], cosT, cosT[:, q * 512:(q + 1) * 512], ALU.mult)
                                    TT("dve", t2, t2[:], pb, pb[:], sinT, sinT[:, q * 512:(q + 1) * 512], ALU.mult)
                                    TT("pool", dst, dst[:, h, qq * 512:(qq + 1) * 512], t1, t1[:], t2, t2[:], ALU.add)
                        for which, cbase in (("v", C_RV), ("g", C_RG)):
                            wb = tpool.get()
                            S.dma("pool", wb, wb[:], wts_d, wcols(w_in, cbase, 512))
                            for jl in range(8):
                                j = seg * 8 + jl
                                q, jj = divmod(j, 4)
                                pt = ps[4 + jl % 2]
                                for kc in range(KC):
                                    MM(pt, pt[:], hT[q], hT[q][:, kc, jj * P:(jj + 1) * P], wb, wb[:, kc, :], kc == 0, kc == KC - 1)
                                if which == "v":
                                    CP("act", v_tok, v_tok[:, jl, :], pt, pt[:])
                                else:
                                    sg = sgt_p.get()
                                    ACTF(sg, sg[:], pt, pt[:], ACT.Silu)
                                    TT("pool", gs, gs[:, jl, :], sg, sg[:], gn_bc, gn_bc[:], ALU.mult)
                        for jl in range(8):
                            j = seg * 8 + jl
                            tsl = slice(jl * P, (jl + 1) * P)
                            pS, pO, pK, pT_ = ps[0 + (jl % 2)], ps[2 + (jl % 2)], ps[6], ps[7]
                            for h in range(4):
                                MM(pS, pS[:, h * P:(h + 1) * P], kT, kT[:, h, tsl], qT, qT[:, h, tsl])
                            sTm = sTm_p.get()
                            TT("dve", sTm, sTm[:], pS, pS[:], cst, cst[:, CS_MASK:CS_MASK + 512], ALU.mult)
                            qd = qd_p.get()
                            TT("pool", qd, qd[:], qT, qT[:, :, tsl], cst,
                               cst[:, CS_QDEC:CS_QDEC + 512].rearrange("p (h c) -> p h c", h=4), ALU.mult)
                            pTb = pT_[:, 0:256].bitcast(BF16)
                            for h in range(4):
                                S.op("pe", lambda e, h=h, tsl=tsl, pTb=pTb: e.transpose(
                                    out=pTb[:, h * P:(h + 1) * P], in_=kT[:, h, tsl], identity=identb[:]),
                                    [kT, identb], [pT_])
                            kt = kt_p.get()
                            for h in range(4):
                                TS("dve", kt, kt[:, h * P:(h + 1) * P], pT_, pTb[:, h * P:(h + 1) * P],
                                   cst[:, CS_KDEC + h:CS_KDEC + h + 1], None, ALU.mult, reads=[cst])
                            for h in range(4):
                                hs = slice(h * P, (h + 1) * P)
                                MM(pO, pO[:, hs], sTm, sTm[:, hs], v_tok, v_tok[:, jl, hs], True, j == 0)
                                if j > 0:
                                    MM(pO, pO[:, hs], qd, qd[:, h, :], state_b, state_b[:, h, :], False, True)
                            if j < NT - 1:
                                for h in range(4):
                                    hs = slice(h * P, (h + 1) * P)
                                    MM(pK, pK[:, hs], kt, kt[:, hs], v_tok, v_tok[:, jl, hs])
                                if j == 0:
                                    CP("dve", state_f, state_f[:], pK, pK[:].rearrange("p (h e) -> p h e", h=4))
                                else:
                                    for h in range(4):
                                        STT("dve", state_f, state_f[:, h, :], state_f, state_f[:, h, :], RET_CD[h],
                                            pK, pK[:, h * P:(h + 1) * P], ALU.mult, ALU.add)
                                CP("act", state_b, state_b[:], state_f, state_f[:])
                            stt = st_p.get()
                            MSET("dve", stt, stt[:], 0.0)
                            for h in range(4):
                                hs = slice(h * P, (h + 1) * P)
                                ACTF(junk, junk[:], pO, pO[:, hs], ACT.Copy, accum=(stt, stt[:, h:h + 1]))
                                ACTF(junk, junk[:], pO, pO[:, hs], ACT.Square, accum=(stt, stt[:, 4 + h:5 + h]))
                            TS("dve", stt, stt[:, 0:8], stt, stt[:, 0:8], 1.0 / P, None, ALU.mult)
                            TT("dve", stt, stt[:, 8:12], stt, stt[:, 0:4], stt, stt[:, 0:4], ALU.mult)
                            TT("dve", stt, stt[:, 12:16], stt, stt[:, 4:8], stt, stt[:, 8:12], ALU.subtract)
                            TS("dve", stt, stt[:, 12:16], stt, stt[:, 12:16], EPS, None, ALU.add)
                            RSTD(stt, stt[:, 12:16])
                            on = on_p.get()
                            for h in range(4):
                                hs = slice(h * P, (h + 1) * P)
                                TS("dve", on, on[:, hs], pO, pO[:, hs], stt[:, h:h + 1], stt[:, 12 + h:13 + h],
                                   ALU.subtract, ALU.mult, reads=[stt])
                            og = og_p.get()
                            TT("pool", og, og[:], on, on[:], gs, gs[:, jl, :], ALU.mult)
                            pX = ps[4 + jl % 2]
                            for h in range(4):
                                TR(pX, pX[:, h * P:(h + 1) * P], og, og[:, h * P:(h + 1) * P], *ident)
                            CP("act", oT["ret"], oT["ret"][:, :, j * P:(j + 1) * P], pX, pX[:].rearrange("p (h c) -> p h c", h=4))
                S.barrier()
                if dbg == "ret":
                    return "stop"
                with ExitStack() as st2:
                    zT = S.sb("c_zT", [P, 4, 30 + SEQ], BF16, st2)
                    acc = S.sb("c_acc", [P, 4, SEQ], F32, st2)
                    sgp = Pool("c_sg", [P, 512], F32, 2, st2)
                    dgp = Pool("c_dg", [P, P], BF16, 6, st2)
                    MSET("pool", zT, zT[:, :, 0:30], 0.0)
                    for cc in range(4):
                        wa = load_f(C_CU + cc * P)
                        wb = load_f(C_CU + 512 + cc * P)
                        for q in range(NQ):
                            pa, pb = ps[2 * (q % 2)], ps[2 * (q % 2) + 1]
                            f_mm(wa, q, pa)
                            f_mm(wb, q, pb)
                            sg = sgp.get()
                            ACTF(sg, sg[:], pb, pb[:], ACT.Sigmoid)
                            TT("dve", zT, zT[:, cc, 30 + q * 512:30 + (q + 1) * 512], pa, pa[:], sg, sg[:], ALU.mult)
                        for k in range(31):
                            dg = dgp.get()
                            wk = pp[:, PP_CW + cc * 31 + k:PP_CW + cc * 31 + k + 1]
                            TS("pool" if k % 2 else "dve", dg, dg[:], identb, identb[:], wk, None, ALU.mult, reads=[pp])
                            for q in range(NQ):
                                pc = ps[4 + q]
                                MM(pc, pc[:], dg, dg[:], zT, zT[:, cc, q * 512 + k:q * 512 + k + 512], k == 0, k == 30)
                        for q in range(NQ):
                            pc = ps[4 + q]
                            TS("dve", acc, acc[:, cc, q * 512:(q + 1) * 512], pc, pc[:], pp[:, PP_CB + cc:PP_CB + cc + 1], None,
                               ALU.add, reads=[pp])
                    sqp = Pool("c_sq", [P, 512], F32, 2, st2)
                    m2p = Pool("c_m2", [P, 512], F32, 2, st2)
                    rsp = Pool("c_rs", [P, 512], F32, 2, st2)
                    tp = Pool("c_t", [P, 512], F32, 2, st2)
                    for q in range(NQ):
                        qs = slice(q * 512, (q + 1) * 512)
                        pm, pe2 = ps[4 + 2 * (q % 2)], ps[5 + 2 * (q % 2)]
                        for cc in range(4):
                            MM(pm, pm[:], onesln, onesln[:], acc, acc[:, cc, qs], cc == 0, cc == 3)
                        for cc in range(4):
                            sq = sqp.get()
                            ACTF(sq, sq[:], acc, acc[:, cc, qs], ACT.Square)
                            MM(pe2, pe2[:], onesln, onesln[:], sq, sq[:], cc == 0, cc == 3)
                        m2 = m2p.get()
                        ACTF(m2, m2[:], pm, pm[:], ACT.Square)
                        rs = rsp.get()
                        TT("dve", rs, rs[:], pe2, pe2[:], m2, m2[:], ALU.subtract)
                        TS("dve", rs, rs[:], rs, rs[:], EPS, None, ALU.add)
                        RSTD(rs, rs[:])
                        for cc in range(4):
                            t = tp.get()
                            TT("dve", t, t[:], acc, acc[:, cc, qs], pm, pm[:], ALU.subtract)
                            TT("pool", t, t[:], t, t[:], rs, rs[:], ALU.mult)
                            ACTF(oT["conv"], oT["conv"][:, cc, qs], t, t[:], ACT.Silu,
                                 bias=pp[:, PP_LB + cc:PP_LB + cc + 1], scale=pp[:, PP_LG + cc:PP_LG + cc + 1], reads=[pp])
                S.barrier()
                if dbg == "conv":
                    return "stop"
                with ExitStack() as st2:
                    aqM = [S.sb(f"a_qM{i}", [P, 4, SEQ], BF16, st2) for i in range(2)]
                    MSET("pool", aqM[0], aqM[0][:], 0.0)
                    MSET("pool", aqM[1], aqM[1][:], 0.0)
                    akT = S.sb("a_kT", [P, 4, SEQ], BF16, st2)
                    vaug = S.sb("a_v", [P, NT, 8, 66], BF16, st2)
                    biasT = S.sb("a_bias", [P, 5, 8, P], F32, st2)
                    S.dma("sp", biasT, biasT[:], wts_d, biasx_h[l])
                    MSET("pool", biasT, biasT[0:64, 0, :, 64:128], NEG)
                    MSET("pool", biasT, biasT[64:128, 4, :, 0:64], NEG)
                    MSET("pool", vaug, vaug[:], 1.0)
                    with ExitStack() as st3:
                        sqp = Pool("a_sq", [P, 512], BF16, 2, st3)
                        rsp = Pool("a_rs", [P, 512], F32, 2, st3)
                        for dst, cbase, gcol in ((None, C_AQ, PP_QG), (akT, C_AK, PP_KG)):
                            for c in range(4):
                                wb = load_f(cbase + c * P)
                                for q in range(NQ):
                                    qs = slice(q * 512, (q + 1) * 512)
                                    pa, pb = ps[2 * (q % 2)], ps[2 * (q % 2) + 1]
                                    f_mm(wb, q, pa)
                                    sq = sqp.get()
                                    ACTF(sq, sq[:], pa, pa[:], ACT.Square)
                                    MM(pb, pb[:], blk64, blk64[:], sq, sq[:])
                                    rs = rsp.get()
                                    TS("dve", rs, rs[:], pb, pb[:], EPS, None, ALU.add)
                                    RSTD(rs, rs[:])
                                    if dst is None:
                                        for i in range(2):
                                            hp = slice(64 * i, 64 * i + 64)
                                            STT("dve", aqM[i], aqM[i][hp, c, qs], pa, pa[hp, :], pp[hp, gcol:gcol + 1], rs, rs[hp, :],
                                                ALU.mult, ALU.mult, reads=[pp])
                                    else:
                                        STT("dve", dst, dst[:, c, qs], pa, pa[:], pp[:, gcol:gcol + 1], rs, rs[:],
                                            ALU.mult, ALU.mult, reads=[pp])
                        tpool = Pool("tblk", [P, KC, 512], BF16, 1, st3)
                        wb = tpool.get()
                        S.dma("pool", wb, wb[:], wts_d, wcols(w_in, C_AV, 512))
                        for j in range(NT if dbg != "att0" else 0):
                            q, jj = divmod(j, 4)
                            pt = ps[4 + j % 2]
                            for kc in range(KC):
                                MM(pt, pt[:], hT[q], hT[q][:, kc, jj * P:(jj + 1) * P], wb, wb[:, kc, :], kc == 0, kc == KC - 1)
                            CP("act", vaug, vaug[:, j, :, 0:64], pt, pt[:].rearrange("p (h d) -> p h d", h=8))
                    S.barrier()
                    ep = Pool("a_e", [P, 512], F32, 2, st2)
                    pTp = Pool("a_pT", [P, 5, 512], BF16, 2, st2)
                    recp = Pool("a_rec", [P, 8], F32, 2, st2)
                    oap = Pool("a_o", [P, 512], F32, 2, st2)
                    for j in range(NT if dbg not in ("att0", "att1") else 0):
                        tsl = slice(j * P, (j + 1) * P)
                        kbs = [kb for kb in range(5) if j - 4 + kb >= 0]
                        oa = oap.get()
                        rec = recp.get()
                        for g in range(2):
                            pTt = pTp.get()
                            for kb in kbs:
                                jk = j - 4 + kb
                                ksl = slice(jk * P, (jk + 1) * P)
                                pq = ps[kb % 2 + 2 * g]
                                for hh in range(4):
                                    h = g * 4 + hh
                                    c, r0 = h // 2, 64 * (h % 2)
                                    MM(pq, pq[:, hh * P:(hh + 1) * P], akT, akT[:, c, ksl], aqM[h % 2], aqM[h % 2][:, c, tsl])
                                e_ = ep.get()
                                STT("dve", e_, e_[:].rearrange("p (h q) -> p h q", h=4), pq,
                                    pq[:].rearrange("p (h q) -> p h q", h=4), 0.125,
                                    biasT, biasT[:, kb, g * 4:(g + 1) * 4, :], ALU.mult, ALU.add)
                                ACTF(pTt, pTt[:, kb, :], e_, e_[:], ACT.Exp)
                            po = ps[4 + g]
                            if dbg == "att2":
                                continue
                            for hh in range(4):
                                h = g * 4 + hh
                                for i, kb in enumerate(kbs):
                                    jk = j - 4 + kb
                                    MM(po, po[:, hh * 66:(hh + 1) * 66], pTt, pTt[:, kb, hh * P:(hh + 1) * P],
                                       vaug, vaug[:, jk, h, :], i == 0, i == len(kbs) - 1)
                            pov = po[:, 0:264].rearrange("p (h d) -> p h d", h=4)
                            S.op("dve", lambda e, rec=rec, pov=pov, g=g: e.reciprocal(
                                out=rec[:, g * 4:(g + 1) * 4].unsqueeze(2), in_=pov[:, :, 64:65]), [po], [rec])
                            for hh in range(4):
                                h = g * 4 + hh
                                TS("dve", oa, oa[:, h * 64:(h + 1) * 64], po, po[:, hh * 66:hh * 66 + 64],
                                   rec[:, h:h + 1], None, ALU.mult, reads=[rec])
                        if dbg in ("att2", "att3"):
                            continue
                        pX = ps[6 + j % 2]
                        for c in range(4):
                            TR(pX, pX[:, c * P:(c + 1) * P], oa, oa[:, c * P:(c + 1) * P], *ident)
                        CP("act", oT["att"], oT["att"][:, :, tsl], pX, pX[:].rearrange("p (c t) -> p c t", c=4))
                for i_, k_ in enumerate(("ret", "conv", "att")):
                    DUMP(1 + i_, oT[k_], oT[k_][:].rearrange("p k t -> p (k t)"), 8192)
                S.barrier()
                if dbg and dbg.startswith("att"):
                    return "stop"
                with ExitStack() as st2:
                    mT = [S.sb(f"mT{q}", [P, KC, 512], BF16, st2) for q in range(NQ)]
                    glp = Pool("s4_gl", [P, KC, 3, P], BF16, 2, st2)
                    wop = Pool("s4_wo", [P, 4, 3, P], BF16, 2, st2)
                    gtp = Pool("s4_g", [P, 512], F32, 3, st2)
                    tmp = Pool("s4_t", [P, 512], F32, 2, st2)
                    macc = Pool("s4_m", [P, 512], F32, 2, st2)
                    outs_w = (w_ro_h[l], w_co_h[l], w_ao_h[l])
                    keys = ("ret", "conv", "att")
                    for c in range(KC):
                        gw = glp.get()
                        ow = wop.get()
                        for b in range(3):
                            S.dma("pool", gw, gw[:, :, b, :], wts_d, wcols(w_in, C_GL + b * D + c * P, P))
                            S.dma("pool", ow, ow[:, :, b, :], wts_d, wcols(outs_w[b], c * P, P))
                        for q in range(NQ):
                            qs = slice(q * 512, (q + 1) * 512)
                            m = macc.get()
                            for b in range(3):
                                pg, py = ps[2 * (b % 2)], ps[2 * (b % 2) + 1]
                                for kc in range(KC):
                                    MM(pg, pg[:], gw, gw[:, kc, b, :], hT[q], hT[q][:, kc, :], kc == 0, kc == KC - 1)
                                for kc in range(4):
                                    MM(py, py[:], ow, ow[:, kc, b, :], oT[keys[b]], oT[keys[b]][:, kc, qs], kc == 0, kc == 3)
                                gt = gtp.get()
                                ACTF(gt, gt[:], pg, pg[:], ACT.Sigmoid,
                                     bias=pp[:, PP_BG + b * 8 + c:PP_BG + b * 8 + c + 1], reads=[pp])
                                if b == 0:
                                    TT("dve", m, m[:], py, py[:], gt, gt[:], ALU.mult)
                                elif b == 1:
                                    t = tmp.get()
                                    TT("dve", t, t[:], py, py[:], gt, gt[:], ALU.mult)
                                    TT("pool", m, m[:], m, m[:], t, t[:], ALU.add)
                                else:
                                    t = tmp.get()
                                    TT("dve", t, t[:], py, py[:], gt, gt[:], ALU.mult)
                                    TT("pool", mT[q], mT[q][:, c, :], m, m[:], t, t[:], ALU.add)
                    DUMP(4, mT[0], mT[0][:].rearrange("p k t -> p (k t)"), 4096)
                    wo = S.sb("s4_wout", [P, KC, D], BF16, st2)
                    S.dma("pool", wo, wo[:], wts_d, wcols(w_out_h[l], 0, D))
                    gate1 = load_mod_bc(st2, l, s, 2, "gate1")
                    xs_pool = Pool("s4_xs", [P, D], F32, 2, st2)
                    for j in range(NT):
                        q, jj = divmod(j, 4)
                        xs = xs_pool.get()
                        if first:
                            S.dma("sp", xs, xs[:], x_d, x_h[s, j * P:(j + 1) * P, :])
                        else:
                            S.dma("sp", xs, xs[:], xo_d[s][j], out_h[s, j * P:(j + 1) * P, :])
                        for half in range(2):
                            pt = ps[4 + (2 * j + half) % 4]
                            hsl = slice(half * 512, (half + 1) * 512)
                            for kc in range(KC):
                                MM(pt, pt[:], mT[q], mT[q][:, kc, jj * P:(jj + 1) * P], wo, wo[:, kc, hsl], kc == 0, kc == KC - 1)
                            t = tmp.get()
                            TT("dve", t, t[:], pt, pt[:], gate1, gate1[:, hsl], ALU.mult)
                            TT("pool", xs, xs[:, hsl], xs, xs[:, hsl], t, t[:], ALU.add)
                        S.dma("sp", xo_d[s][j], out_h[s, j * P:(j + 1) * P, :], xs, xs[:], sem_t=xs)
                S.barrier()
            S.barrier()

        def moe(s, l):
            with ExitStack() as st:
                hT = [S.sb(f"hT{q}", [P, KC, 512], BF16, st) for q in range(NQ)]
                acc = S.sb("m_acc", [P, NT, D], F32, st)
                wge = S.sb("m_wge", [P, NT, 32], F32, st)
                with ExitStack() as st1:
                    gmod = load_mod_bc(st1, l, s, 4, "gmod2")
                    shift = load_mod_bc(st1, l, s, 3, "shift2")
                    wr = S.sb("m_wr", [P, KC, 36], F32, st1)
                    S.dma("sp", wr, wr[:], wts_d, wr_h[l])
                    brow = S.sb("m_brow", [P, 36], F32, st1)
                    S.dma("sp", brow, brow[:], wts_d, brow_h[l:l + 1, :].partition_broadcast(P))
                    whi = S.sb("m_whi", [P, KC, 36], BF16, st1)
                    wlo = S.sb("m_wlo", [P, KC, 36], BF16, st1)
                    CP("dve", whi, whi[:], wr, wr[:])
                    TT("dve", wlo, wlo[:], wr, wr[:], whi, whi[:], ALU.subtract)
                    h2p = Pool("m_h2lo", [P, KC, P], BF16, 2, st1)
                    rp = Pool("m_r", [P, 96], F32, 2, st1)

                    def router(j, pa, pb):
                        q, jj = divmod(j, 4)
                        tsl = slice(jj * P, (jj + 1) * P)
                        h2 = h2p.get()
                        TT("dve", h2, h2[:, 0:4, :], pa, pa[:].rearrange("p (k t) -> p k t", k=4),
                           hT[q], hT[q][:, 0:4, tsl], ALU.subtract)
                        TT("dve", h2, h2[:, 4:8, :], pb, pb[:].rearrange("p (k t) -> p k t", k=4),
                           hT[q], hT[q][:, 4:8, tsl], ALU.subtract)
                        pl = ps[4 + j % 2]
                        for kc in range(KC):
                            MM(pl, pl[:, 0:36], hT[q], hT[q][:, kc, tsl], whi, whi[:, kc, :], kc == 0, False)
                            MM(pl, pl[:, 0:36], hT[q], hT[q][:, kc, tsl], wlo, wlo[:, kc, :], False, False)
                            MM(pl, pl[:, 0:36], h2, h2[:, kc, :], whi, whi[:, kc, :], False, kc == KC - 1)
                        r = rp.get()
                        MSET("dve", r, r[:], 0.0)
                        LG, GMX, OHG, NGM, GS, CH, M1, OH1, CH2, M2, OH2, DD, W1, W2, WE, EX = (
                            slice(0, 36), slice(36, 37), slice(37, 41), slice(41, 42), slice(42, 43), slice(43, 51),
                            slice(51, 52), slice(52, 60), slice(60, 68), slice(68, 69), slice(69, 77), slice(77, 78),
                            slice(78, 79), slice(79, 80), slice(80, 88), slice(88, 92))
                        R = lambda sl: r[:, sl]
                        TT("dve", r, R(LG), pl, pl[:, 0:36], brow, brow[:], ALU.add)
                        if dbg == "m_r1":
                            return
                        S.op("dve", lambda e: e.tensor_reduce(out=R(GMX), in_=r[:, 0:4], axis=AX.X, op=ALU.max), [r], [r])
                        TS("dve", r, R(OHG), r, r[:, 0:4], R(GMX), None, ALU.is_ge)
                        TS("dve", r, R(NGM), r, R(GMX), -1.0, None, ALU.mult)
                        ACTF(r, R(EX), r, r[:, 0:4], ACT.Exp, bias=R(NGM), accum=(r, R(GS)))
                        S.op("dve", lambda e: e.reciprocal(out=R(GS), in_=R(GS)), [r], [r])
                        if dbg == "m_r2":
                            return
                        TS("dve", r, R(CH), r, r[:, 4:12], r[:, 37:38], None, ALU.mult)
                        for g in range(1, 4):
                            STT("dve", r, R(CH), r, r[:, 4 + g * 8:12 + g * 8], r[:, 37 + g:38 + g], r, R(CH), ALU.mult, ALU.add)
                        S.op("dve", lambda e: e.tensor_reduce(out=R(M1), in_=R(CH), axis=AX.X, op=ALU.max), [r], [r])
                        TS("dve", r, R(OH1), r, R(CH), R(M1), None, ALU.is_ge)
                        STT("dve", r, R(CH2), r, R(OH1), NEG, r, R(CH), ALU.mult, ALU.add)
                        S.op("dve", lambda e: e.tensor_reduce(out=R(M2), in_=R(CH2), axis=AX.X, op=ALU.max), [r], [r])
                        TS("dve", r, R(OH2), r, R(CH2), R(M2), None, ALU.is_ge)
                        TT("dve", r, R(DD), r, R(M2), r, R(M1), ALU.subtract)
                        ACTF(r, R(W2), r, R(DD), ACT.Sigmoid)
                        TS("dve", r, R(W1), r, R(W2), -1.0, 1.0, ALU.mult, ALU.add)
                        TT("dve", r, R(W1), r, R(W1), r, R(GS), ALU.mult)
                        TT("dve", r, R(W2), r, R(W2), r, R(GS), ALU.mult)
                        TS("dve", r, R(WE), r, R(OH1), R(W1), None, ALU.mult)
                        STT("dve", r, R(WE), r, R(OH2), R(W2), r, R(WE), ALU.mult, ALU.add)
                        if dbg == "m_r3":
                            return
                        for g in range(4):
                            TS("dve", wge, wge[:, j, g * 8:(g + 1) * 8], r, R(WE), r[:, 37 + g:38 + g], None, ALU.mult, reads=[r])

                    norm_and_transpose(st1, l, s, False, gmod, shift, hT, h2f_cb=(None if dbg == "m_norouter" else router))
                S.barrier()
                with ExitStack() as st2:
                    upp = Pool("m_wup", [P, KC, 512], BF16, 2, st2)
                    dnp = Pool("m_wdn", [P, 2, D], BF16, 2, st2)
                    sap = Pool("m_sa", [P, 512], F32, 2, st2)
                    actp = Pool("m_act", [P, 2, 512], BF16, 2, st2)
                    dcount = 0
                    for ge in range(n_experts):
                        g, e_ = divmod(ge, NE)
                        wu = upp.get()
                        wd = dnp.get()
                        S.dma("pool", wu, wu[:], wts_d, wcols(w_up_h[l, g, e_], 0, 512))
                        S.dma("pool", wd, wd[:], wts_d, w_dn_h[l, g, e_].rearrange("(fc p) n -> p fc n", p=P))
                        for q in range(NQ):
                            for fc in range(4):
                                pu = ps[fc]
                                for kc in range(KC):
                                    MM(pu, pu[:], wu, wu[:, kc, fc * P:(fc + 1) * P], hT[q], hT[q][:, kc, :], kc == 0, kc == KC - 1)
                            at = actp.get()
                            for fc in range(2):
                                sa = sap.get()
                                ACTF(sa, sa[:], ps[fc], ps[fc][:], ACT.Silu)
                                TT("dve", at, at[:, fc, :], sa, sa[:], ps[fc + 2], ps[fc + 2][:], ALU.mult)
                            for jj in range(4):
                                j = q * 4 + jj
                                for half in range(2):
                                    pd = ps[4 + dcount % 4]
                                    dcount += 1
                                    hsl = slice(half * 512, (half + 1) * 512)
                                    for fc in range(2):
                                        MM(pd, pd[:], at, at[:, fc, jj * P:(jj + 1) * P], wd, wd[:, fc, hsl], fc == 0, fc == 1)
                                    if ge == 0:
                                        TS("dve", acc, acc[:, j, hsl], pd, pd[:], wge[:, j, ge:ge + 1], None, ALU.mult, reads=[wge])
                                    else:
                                        STT("dve", acc, acc[:, j, hsl], pd, pd[:], wge[:, j, ge:ge + 1], acc, acc[:, j, hsl],
                                            ALU.mult, ALU.add, reads=[wge])
                    gate2 = load_mod_bc(st2, l, s, 5, "gate2")
                    xs_pool = Pool("m_xs", [P, D], F32, 2, st2)
                    toks = []
                    for j in range(NT):
                        xs = xs_pool.get()
                        S.dma("sp", xs, xs[:], xo_d[s][j], out_h[s, j * P:(j + 1) * P, :])
                        TT("pool", acc, acc[:, j, :], acc, acc[:, j, :], gate2, gate2[:], ALU.mult)
                        TT("pool", xs, xs[:], xs, xs[:], acc, acc[:, j, :], ALU.add)
                        toks.append(S.dma("sp", xo_d[s][j], out_h[s, j * P:(j + 1) * P, :], xs, xs[:], sem_t=xs))
                S.barrier()
                return toks

        identb = S.sb("identb", [P, P], BF16)
        CP("dve", identb, identb[:], cst, cst[:, CS_ID:CS_ID + P])

        final = []
        try:
            if dbg and dbg.startswith("pro"):
                raise _Stop()
            for s in range(n_seq):
                for l in range(n_layers):
                    if mixer(s, l) == "stop":
                        raise _Stop()
                    final = moe(s, l) if n_experts > 0 else []
        except _Stop:
            S.barrier()
        S.emit(final_toks=S.dma_toks + final + dump_toks)
    return nc


def _constants():
    cst = np.zeros((P, NCST), np.float32)
    cst[:, CS_ID:CS_ID + P] = np.eye(P, dtype=np.float32)
    m = np.arange(P)[:, None].astype(np.float64)
    c = np.arange(P)[None, :].astype(np.float64)
    for h in range(4):
        gamma = 1.0 - 2.0 ** (-5 - h)
        mask = np.where(c >= m, gamma ** np.maximum(c - m, 0.0), 0.0) * (128.0 ** -0.5)
        cst[:, CS_MASK + h * P:CS_MASK + (h + 1) * P] = mask
        cst[:, CS_QDEC + h * P:CS_QDEC + (h + 1) * P] = (gamma ** (c + 1.0))
        cst[:, CS_KDEC + h] = (gamma ** (127.0 - m[:, 0])) * (128.0 ** -0.5)
    half = 64
    inv = (np.float32(10000.0) ** (-np.arange(half, dtype=np.float32) / np.float32(half))).astype(np.float32)
    cst[:, CS_INV] = np.tile(inv, 2)
    cst[0:64, CS_SGN] = -1.0
    cst[64:128, CS_SGN] = 1.0
    cst[0:64, CS_MLO] = 1.0
    cst[64:128, CS_MHI] = 1.0
    return cst


def _layout_params(inp):
    f32 = np.float32
    pp = np.zeros((L, P, NPP), f32)
    cw = np.asarray(inp["conv_w"], f32)[:, :, 0, :]
    pp[:, :, PP_CW:PP_CW + 124] = cw.reshape(L, 31, 4, P).transpose(0, 3, 2, 1).reshape(L, P, 124)
    pp[:, :, PP_CB:PP_CB + 4] = np.asarray(inp["conv_b"], f32).reshape(L, 4, P).transpose(0, 2, 1)
    pp[:, :, PP_LG:PP_LG + 4] = np.asarray(inp["conv_ln_g"], f32).reshape(L, 4, P).transpose(0, 2, 1)
    pp[:, :, PP_LB:PP_LB + 4] = np.asarray(inp["conv_ln_b"], f32).reshape(L, 4, P).transpose(0, 2, 1)
    pp[:, :, PP_QG] = np.tile(np.asarray(inp["att_q_gain"], f32), (1, 2))
    pp[:, :, PP_KG] = np.tile(np.asarray(inp["att_k_gain"], f32), (1, 2))
    pp[:, :, PP_BG:PP_BG + 24] = np.asarray(inp["b_gate"], f32).reshape(L, 24, P).transpose(0, 2, 1)
    wr = np.concatenate([np.asarray(inp["w_group"], f32), np.asarray(inp["w_inner"], f32)], axis=-1)
    wr = np.ascontiguousarray(wr.reshape(L, KC, P, 36).transpose(0, 2, 1, 3))
    brow = np.ascontiguousarray(np.concatenate([np.asarray(inp["b_group"], f32), np.asarray(inp["b_inner"], f32)], axis=-1))
    k = np.arange(P)[:, None, None]
    kb = np.arange(5)[None, :, None]
    q = np.arange(P)[None, None, :]
    idx = np.clip(q - k + 128 * (4 - kb), -256, 256) + 256
    tab = np.asarray(inp["att_rel_bias"], f32)
    bx = tab[:, :, idx]
    biasx = np.ascontiguousarray(bx.transpose(0, 2, 3, 1, 4))
    return pp, wr, brow, biasx


_PROG = {}


def kernel(**inp):
    n = 8
    f32 = np.float32
    x = np.asarray(inp["x"], f32)
    c = np.asarray(inp["c"], f32)
    pos = np.asarray(inp["positions"], np.int32)
    pp, wr, brow, biasx = _layout_params(inp)
    cst = _constants()
    shared = dict(
        w_ada=np.asarray(inp["w_ada"], f32), b_ada=np.asarray(inp["b_ada"], f32),
        g_mix=np.asarray(inp["g_mix"], f32), g_ffn=np.asarray(inp["g_ffn"], f32),
        w_in=np.asarray(inp["w_in"], f32), ret_gn=np.asarray(inp["ret_gn"], f32),
        w_ret_out=np.asarray(inp["w_ret_out"], f32), w_conv_out=np.asarray(inp["w_conv_out"], f32),
        w_att_out=np.asarray(inp["w_att_out"], f32), w_out=np.asarray(inp["w_out"], f32),
        w_up=np.asarray(inp["w_up"], f32), w_down=np.asarray(inp["w_down"], f32),
        pp=pp, wr=wr, brow=brow, biasx=biasx, cst=cst)
    if "nc" not in _PROG:
        _PROG["nc"] = build_program()
    nc = _PROG["nc"]
    in_maps = []
    for i in range(n):
        sl = slice(NSEQ * i, NSEQ * (i + 1))
        m = dict(shared)
        m["x"] = np.ascontiguousarray(x[sl])
        m["pos"] = np.ascontiguousarray(pos[sl])
        m["cT"] = np.ascontiguousarray(c[sl].reshape(NSEQ, KC, P).transpose(2, 1, 0))
        in_maps.append(m)
    res = run_bass_kernel_spmd(nc, in_maps, core_ids=list(range(n)))
    return np.concatenate([np.asarray(r["out"], f32) for r in res.results], axis=0)
```

```python
import math
import numpy as np
from concourse.bass_utils import run_bass_kernel_spmd

import numpy as np
import concourse.bass as bass
import concourse.mybir as mybir

F32 = mybir.dt.float32
BF16 = mybir.dt.bfloat16
I32 = mybir.dt.int32
ALU = mybir.AluOpType
ACT = mybir.ActivationFunctionType
AX = mybir.AxisListType

SAME_ENGINE_SYNC = False
SMALL_N = 512


class Op:
    __slots__ = ("eng", "fn", "deps", "signal", "semval", "dma_inc", "idx", "small")

    def __init__(self, eng, fn, deps, dma_inc=None):
        self.eng = eng
        self.fn = fn
        self.deps = deps
        self.signal = False
        self.semval = None
        self.dma_inc = dma_inc
        self.idx = None
        self.small = False


class DmaTok:
    __slots__ = ("sem", "val", "eng")

    def __init__(self, sem, val):
        self.sem = sem
        self.val = val
        self.eng = None


class T:
    def __init__(self, h, name=""):
        self.h = h
        self.name = name
        self.last_w = None
        self.readers = []
        self.dsem = None
        self.dcount = 0

    def __getitem__(self, k):
        return self.h[k]


class TView:
    def __init__(self, base, ap):
        self.__dict__["base"] = base
        self.__dict__["ap"] = ap

    def __getitem__(self, k):
        return self.ap[k]

    def __getattr__(self, k):
        return getattr(self.base, k)

    def __setattr__(self, k, v):
        setattr(self.base, k, v)


class Sched:
    ENGS = ("pe", "act", "dve", "pool", "sp")

    def __init__(self, nc):
        self.nc = nc
        self.ops = {e: [] for e in self.ENGS}
        self.sems = {}
        self.stack = None
        self.dma_sems = []
        self.free_dma_sems = []
        self.dma_toks = []
        self.uid = 0

    def set_stack(self, stack):
        self.stack = stack

    def sem(self, name):
        return self.stack.enter_context(self.nc.semaphore(name))

    def sb(self, name, shape, dtype, stack=None):
        st = stack or self.stack
        self.uid += 1
        name = f"{name}_{self.uid}"
        h = st.enter_context(self.nc.sbuf_tensor(name, list(shape), dtype))
        t = T(h, name)
        if st is not self.stack:
            st.callback(self._retire, t)
        return t

    def _retire(self, t):
        if t.dsem is not None:
            self.free_dma_sems.append((t.dsem, t.dcount))
            t.dsem = None

    def ps(self, name, shape, dtype=F32, stack=None):
        st = stack or self.stack
        h = st.enter_context(self.nc.psum_tensor(name, list(shape), dtype))
        return T(h, name)

    def dram(self, h, name=""):
        return T(h, name)

    def _deps(self, reads, writes):
        deps = []
        for t in reads:
            if t.last_w is not None:
                deps.append(t.last_w)
        for t in writes:
            if t.last_w is not None:
                deps.append(t.last_w)
            deps.extend(t.readers)
        return deps

    def _commit(self, tok, reads, writes):
        for t in reads:
            t.readers.append(tok)
            if len(t.readers) > 64:
                t.readers = self._compact(t.readers)
        for t in writes:
            t.last_w = tok
            t.readers = []

    @staticmethod
    def _compact(toks):
        last = {}
        for tk in toks:
            key = (tk.eng if isinstance(tk, Op) else id(tk.sem))
            last[key] = tk
        return list(last.values())

    def op(self, eng, fn, reads=(), writes=(), small=True):
        o = Op(eng, fn, self._deps(reads, writes))
        o.small = bool(small) and eng != "pe"
        o.idx = len(self.ops[eng])
        self.ops[eng].append(o)
        self._commit(o, reads, writes)
        return o

    def dma(self, q, out_t, out_ap, in_t, in_ap, sem_t=None, **kw):
        st = sem_t or out_t
        if st.dsem is None:
            if self.free_dma_sems:
                st.dsem, st.dcount = self.free_dma_sems.pop()
            else:
                st.dsem = self.sem("d_" + st.name)
                st.dcount = 0
        st.dcount += 16
        tok = DmaTok(st.dsem, st.dcount)
        deps = self._deps([in_t], [out_t])
        sem = st.dsem

        def fn(e, out_ap=out_ap, in_ap=in_ap, kw=kw):
            return e.dma_start(out=out_ap, in_=in_ap, **kw)

        o = Op(q, fn, deps, dma_inc=sem)
        o.idx = len(self.ops[q])
        self.ops[q].append(o)
        self._commit(tok, [in_t], [out_t])
        self.dma_toks.append(tok)
        return tok

    def barrier(self):
        lasts = []
        for e in self.ENGS:
            for o in reversed(self.ops[e]):
                if o.fn is not None and o.dma_inc is None:
                    lasts.append(o)
                    break
        toks = list(self.dma_toks)
        self.dma_toks = []
        for e in self.ENGS:
            deps = [o for o in lasts if o.eng != e] + toks
            if not deps:
                continue
            o = Op(e, None, deps)
            o.idx = len(self.ops[e])
            self.ops[e].append(o)

    def finalize(self):
        for e in self.ENGS:
            for o in self.ops[e]:
                for d in o.deps:
                    if isinstance(d, Op):
                        if d.eng != o.eng or SAME_ENGINE_SYNC or d.small:
                            d.signal = True
        self.counts = {}
        for e in self.ENGS:
            c = 0
            for o in self.ops[e]:
                if o.signal:
                    c += 1
                    o.semval = c
            self.counts[e] = c
            if c > 0 or True:
                self.sems[e] = self.sem("eng_" + e)

    def replay(self, e, handle):
        seen = {}
        nc = self.nc
        for o in self.ops[e]:
            waits = {}
            for d in o.deps:
                if isinstance(d, Op):
                    if d.eng == e and not (SAME_ENGINE_SYNC or d.small):
                        continue
                    key = ("e", d.eng)
                    sem = self.sems[d.eng]
                    val = d.semval
                else:
                    key = ("d", id(d.sem))
                    sem = d.sem
                    val = d.val
                if seen.get(key, 0) >= val:
                    continue
                if key not in waits or waits[key][1] < val:
                    waits[key] = (sem, val)
            for key, (sem, val) in waits.items():
                handle.wait_ge(sem, val)
                seen[key] = val
            if o.fn is None:
                continue
            ins = o.fn(handle)
            if o.dma_inc is not None:
                ins.then_inc(o.dma_inc, 16)
            elif o.signal:
                ins.then_inc(self.sems[e], 1)

    def emit(self, final_toks=()):
        nc = self.nc
        self.finalize()
        with nc.Block() as block:
            @block.tensor
            def _(h):
                self.replay("pe", h)

            @block.scalar
            def _(h):
                self.replay("act", h)

            @block.vector
            def _(h):
                self.replay("dve", h)

            @block.gpsimd
            def _(h):
                self.replay("pool", h)

            @block.sync
            def _(h):
                self.replay("sp", h)
                for tk in final_toks:
                    h.wait_ge(tk.sem, tk.val)

from contextlib import ExitStack

P = 128
SEQ = 2048
D = 1024
KC = 8
NT = 16
NQ = 4
L = 2
NSEQ = 2
IN_COLS = 7680
EPS = 1e-6
NEG = -1e30
C_RQ, C_RK, C_RV, C_RG = 0, 512, 1024, 1536
C_CU = 2048
C_AQ, C_AK, C_AV = 3072, 3584, 4096
C_GL = 4608
NG, NE = 4, 8
PP_CW = 0
PP_CB = 124
PP_LG = 128
PP_LB = 132
PP_QG = 136
PP_KG = 137
PP_BG = 138
NPP = 162
CS_ID = 0
CS_MASK = 128
CS_QDEC = 640
CS_KDEC = 1152
CS_INV = 1156
CS_SGN = 1157
CS_MLO = 1158
CS_MHI = 1159
NCST = 1160
RET_CD = [float((1.0 - 2.0 ** (-5 - h)) ** 128) for h in range(4)]


def build_program(n_layers=L, n_seq=NSEQ, n_experts=NG * NE, dbg=None, dump=False):
    nc = bass.Bass("TRN2", target_bir_lowering=False)
    dump_toks = []

    def din(name, shape, dt=F32):
        return nc.dram_tensor(name, list(shape), dt, kind="ExternalInput")

    x_h = din("x", [NSEQ, SEQ, D])
    pos_h = din("pos", [NSEQ, SEQ], I32)
    cT_h = din("cT", [P, KC, NSEQ])
    w_ada_h = din("w_ada", [L, D, 6 * D])
    b_ada_h = din("b_ada", [L, 6 * D])
    g_mix_h = din("g_mix", [L, D])
    g_ffn_h = din("g_ffn", [L, D])
    w_in_h = din("w_in", [L, D, IN_COLS])
    ret_gn_h = din("ret_gn", [L, 512])
    w_ro_h = din("w_ret_out", [L, 512, D])
    w_co_h = din("w_conv_out", [L, 512, D])
    w_ao_h = din("w_att_out", [L, 512, D])
    w_out_h = din("w_out", [L, D, D])
    w_up_h = din("w_up", [L, NG, NE, D, 512])
    w_dn_h = din("w_down", [L, NG, NE, 256, D])
    pp_h = din("pp", [L, P, NPP])
    wr_h = din("wr", [L, P, KC, 36])
    brow_h = din("brow", [L, 36])
    biasx_h = din("biasx", [L, P, 5, 8, P])
    cst_h = din("cst", [P, NCST])
    out_h = nc.dram_tensor("out", [NSEQ, SEQ, D], F32, kind="ExternalOutput")
    modd_h = nc.dram_tensor("modd", [L, NSEQ, 6 * D], F32)
    dbg_h = nc.dram_tensor("dbg", [P, 6, 8192], F32, kind="ExternalOutput") if dump else None

    top = ExitStack()
    with top:
        S = Sched(nc)
        S.set_stack(top)

        def MM(out_t, out_ap, lt, lap, rt, rap, start=True, stop=True):
            S.op("pe", lambda e: e.matmul(out_ap, lhsT=lap, rhs=rap, start=start, stop=stop),
                 [lt, rt], [out_t])

        def TR(out_t, out_ap, in_t, in_ap, id_t, id_ap):
            S.op("pe", lambda e: e.transpose(out=out_ap, in_=in_ap, identity=id_ap), [in_t, id_t], [out_t])

        def ACTF(out_t, out_ap, in_t, in_ap, func, bias=None, scale=None, accum=None, reads=()):
            kw = {}
            if bias is not None:
                kw["bias"] = bias
            if scale is not None:
                kw["scale"] = scale
            wr = [out_t]
            if accum is not None:
                kw["accum_out"] = accum[1]
                wr.append(accum[0])
            S.op("act", lambda e: e.activation(out=out_ap, in_=in_ap, func=func, **kw),
                 [in_t] + list(reads), wr, small=(accum is not None or out_ap.free_size() < SMALL_N))

        def TT(eng, out_t, out_ap, a_t, a_ap, b_t, b_ap, op):
            S.op(eng, lambda e: e.tensor_tensor(out=out_ap, in0=a_ap, in1=b_ap, op=op), [a_t, b_t], [out_t],
                 small=out_ap.free_size() < SMALL_N)

        def TS(eng, out_t, out_ap, a_t, a_ap, s1, s2, op0, op1=None, reads=()):
            if op1 is None:
                S.op(eng, lambda e: e.tensor_scalar(out=out_ap, in0=a_ap, scalar1=s1, scalar2=None, op0=op0),
                     [a_t] + list(reads), [out_t], small=out_ap.free_size() < SMALL_N)
            else:
                S.op(eng, lambda e: e.tensor_scalar(out=out_ap, in0=a_ap, scalar1=s1, scalar2=s2, op0=op0, op1=op1),
                     [a_t] + list(reads), [out_t], small=out_ap.free_size() < SMALL_N)

        def STT(eng, out_t, out_ap, a_t, a_ap, sc, b_t, b_ap, op0, op1, reads=()):
            S.op(eng, lambda e: e.scalar_tensor_tensor(out=out_ap, in0=a_ap, scalar=sc, in1=b_ap, op0=op0, op1=op1),
                 [a_t, b_t] + list(reads), [out_t], small=out_ap.free_size() < SMALL_N)

        def CP(eng, out_t, out_ap, in_t, in_ap):
            if eng == "act":
                S.op("act", lambda e: e.copy(out=out_ap, in_=in_ap), [in_t], [out_t], small=out_ap.free_size() < SMALL_N)
            else:
                S.op(eng, lambda e: e.tensor_copy(out=out_ap, in_=in_ap), [in_t], [out_t], small=out_ap.free_size() < SMALL_N)

        def MSET(eng, t, ap, val):
            S.op(eng, lambda e: e.memset(ap, val), [], [t], small=ap.free_size() < SMALL_N)

        def RSTD(t, ap, n_is_one_col=True):
            S.op("dve", lambda e: e.reciprocal(out=ap, in_=ap), [t], [t])
            ACTF(t, ap, t, ap, ACT.Sqrt)

        class Pool:
            def __init__(self, name, shape, dt, n, stack):
                self.tiles = [S.sb(f"{name}{i}", shape, dt, stack) for i in range(n)]
                self.i = 0

            def get(self):
                t = self.tiles[self.i % len(self.tiles)]
                self.i += 1
                return t

        def run_units(units):
            nxt = units[0][0]() if units[0][0] is not None else None
            for i, (ld, cp) in enumerate(units):
                cur, nxt = nxt, None
                if i + 1 < len(units) and units[i + 1][0] is not None:
                    nxt = units[i + 1][0]()
                cp(cur)

        def wcols(ap2d, c0, n):
            return ap2d.rearrange("(kc p) n -> p kc n", p=P)[:, :, c0:c0 + n]

        DR = lambda h, name: S.dram(h, name)
        dbg_d = DR(dbg_h, "dbg") if dump else None

        def DUMP(slot, t, ap, ncols):
            if dump:
                dump_toks.append(S.dma("pool", dbg_d, dbg_h[:, slot, 0:ncols], t, ap, sem_t=t))
        x_d = DR(x_h, "x")
        pos_d = DR(pos_h, "pos")
        wts_d = DR(w_in_h, "weights")
        modd_d = DR(modd_h, "modd")
        xo_d = [[DR(out_h, f"xo{s}_{j}") for j in range(NT)] for s in range(NSEQ)]

        cst = S.sb("cst", [P, NCST], F32)
        S.dma("sp", cst, cst[:], wts_d, cst_h[:, :])
        ident = (cst, cst[:, CS_ID:CS_ID + P])
        ps = [S.ps(f"ps{i}", [P, 512]) for i in range(8)]
        blk64 = S.sb("blk64", [P, P], BF16)
        MSET("dve", blk64, blk64[:], 0.0)
        MSET("dve", blk64, blk64[0:64, 0:64], 1.0 / 64)
        MSET("dve", blk64, blk64[64:128, 64:128], 1.0 / 64)
        onesln = S.sb("onesln", [P, P], F32)
        MSET("dve", onesln, onesln[:], 1.0 / 512)

        with ExitStack() as st:
          if dbg != "pro0":
              cT = S.sb("cT", [P, KC, NSEQ], F32, st)
              scT = S.sb("scT", [P, KC, NSEQ], BF16, st)
              S.dma("sp", cT, cT[:], wts_d, cT_h[:, :, :])
              ACTF(scT, scT[:], cT, cT[:], ACT.Silu)
              modrow = S.sb("modrow", [NSEQ, 6 * D], F32, st)
              brow2 = S.sb("brow2", [NSEQ, 6 * D], F32, st)
              grow = S.sb("grow", [NSEQ, 2, D], F32, st)
              wpool = Pool("wada", [P, KC, 512], BF16, 2, st)
              for l in range(n_layers):
                  S.dma("sp", brow2, brow2[:], wts_d, b_ada_h[l:l + 1, :].partition_broadcast(NSEQ))
                  S.dma("sp", grow, grow[:, 0, :], wts_d, g_mix_h[l:l + 1, :].partition_broadcast(NSEQ))
                  S.dma("sp", grow, grow[:, 1, :], wts_d, g_ffn_h[l:l + 1, :].partition_broadcast(NSEQ))
                  if dbg == "pro1":
                      break
                  for cb in range(12):
                      wb = wpool.get()
                      S.dma("pool", wb, wb[:], wts_d, wcols(w_ada_h[l], cb * 512, 512))
                      pt = ps[cb % 2]
                      for kc in range(KC):
                          MM(pt, pt[0:NSEQ, :], scT, scT[:, kc, :], wb, wb[:, kc, :], kc == 0, kc == KC - 1)
                      TT("dve", modrow, modrow[:, cb * 512:(cb + 1) * 512], pt, pt[0:NSEQ, :],
                         brow2, brow2[:, cb * 512:(cb + 1) * 512], ALU.add)
                  if dbg == "pro2":
                      break
                  for i, sl in ((0, 1), (1, 4)):
                      TS("dve", modrow, modrow[:, sl * D:(sl + 1) * D], modrow, modrow[:, sl * D:(sl + 1) * D], 1.0, None, ALU.add)
                      TT("dve", modrow, modrow[:, sl * D:(sl + 1) * D], modrow, modrow[:, sl * D:(sl + 1) * D],
                         grow, grow[:, i, :], ALU.mult)
                  if dbg == "pro3":
                      break
                  S.dma("sp", modd_d, modd_h[l], modrow, modrow[:], sem_t=modrow)
        S.barrier()

        class _Stop(Exception):
            pass

        def load_mod_bc(st, l, s, idx, name):
            t = S.sb(name, [P, D], F32, st)
            S.dma("sp", t, t[:], modd_d, modd_h[l, s:s + 1, idx * D:(idx + 1) * D].partition_broadcast(P))
            return t

        def norm_and_transpose(st, l, s, first_layer_input, gmod, shift, hT, h2f_cb=None):
            xs_pool = Pool("xs", [P, D], F32, 2, st)
            hf_pool = Pool("hf", [P, D], F32, 2, st)
            junk = S.sb("junk", [P, D], BF16, st)
            ssq = Pool("ssq", [P, 8], F32, 2, st)
            for j in range(NT):
                xs = xs_pool.get()
                if first_layer_input:
                    S.dma("sp", xs, xs[:], x_d, x_h[s, j * P:(j + 1) * P, :])
                else:
                    S.dma("sp", xs, xs[:], xo_d[s][j], out_h[s, j * P:(j + 1) * P, :])
                ss = ssq.get()
                MSET("dve", ss, ss[:], 0.0)
                ACTF(junk, junk[:], xs, xs[:], ACT.Square, accum=(ss, ss[:, 0:1]))
                TS("dve", ss, ss[:, 0:1], ss, ss[:, 0:1], 1.0 / D, EPS, ALU.mult, ALU.add)
                S.op("dve", lambda e, ss=ss: e.reciprocal(out=ss[:, 0:1], in_=ss[:, 0:1]), [ss], [ss])
                ACTF(ss, ss[:, 0:1], ss, ss[:, 0:1], ACT.Sqrt)
                hf = hf_pool.get()
                STT("dve", hf, hf[:], xs, xs[:], ss[:, 0:1], gmod, gmod[:], ALU.mult, ALU.mult, reads=[ss])
                TT("pool", hf, hf[:], hf, hf[:], shift, shift[:], ALU.add)
                if j == 0 and first_layer_input:
                    for o_, (t_, n_) in enumerate(((hf, D), (gmod, D), (shift, D), (xs, D), (ss, 8))):
                        if dump:
                            dump_toks.append(S.dma("pool", dbg_d, dbg_h[:, 5, o_ * 1024:o_ * 1024 + n_], t_, t_[:, 0:n_], sem_t=t_))
                q, jj = divmod(j, 4)
                pa, pb = ps[(2 * j) % 4], ps[(2 * j) % 4 + 1]
                for kc in range(KC):
                    pt = pa if kc < 4 else pb
                    TR(pt, pt[:, (kc % 4) * P:(kc % 4 + 1) * P], hf, hf[:, kc * P:(kc + 1) * P], *ident)
                CP("act", hT[q], hT[q][:, 0:4, jj * P:(jj + 1) * P], pa, pa[:].rearrange("p (k t) -> p k t", k=4))
                CP("act", hT[q], hT[q][:, 4:8, jj * P:(jj + 1) * P], pb, pb[:].rearrange("p (k t) -> p k t", k=4))
                if h2f_cb is not None:
                    h2f_cb(j, pa, pb)

        def mixer(s, l):
            first = (l == 0)
            w_in = w_in_h[l]
            with ExitStack() as st:
                hT = [S.sb(f"hT{q}", [P, KC, 512], BF16, st) for q in range(NQ)]
                oT = {k: S.sb(f"oT_{k}", [P, 4, SEQ], BF16, st) for k in ("ret", "conv", "att")}
                pp = S.sb("pp", [P, NPP], F32, st)
                S.dma("sp", pp, pp[:], wts_d, pp_h[l])
                with ExitStack() as st1:
                    gmod = load_mod_bc(st1, l, s, 1, "gmod1")
                    shift = load_mod_bc(st1, l, s, 0, "shift1")
                    norm_and_transpose(st1, l, s, first, gmod, shift, hT)
                DUMP(0, hT[0], hT[0][:].rearrange("p k t -> p (k t)"), 4096)
                S.barrier()
                if dbg == "s1":
                    return "stop"
                fpool = Pool("fblk", [P, KC, P], BF16, 4, st)

                def fproj(c0, q, pt):
                    raise NotImplementedError

                def load_f(c0, swap=False):
                    wb = fpool.get()
                    if not swap:
                        S.dma("pool", wb, wb[:], wts_d, wcols(w_in, c0, P))
                    else:
                        S.dma("pool", wb, wb[:, :, 0:64], wts_d, wcols(w_in, c0 + 64, 64))
                        S.dma("pool", wb, wb[:, :, 64:128], wts_d, wcols(w_in, c0, 64))
                    return wb

                def f_mm(wb, q, pt):
                    for kc in range(KC):
                        MM(pt, pt[:], wb, wb[:, kc, :], hT[q], hT[q][:, kc, :], kc == 0, kc == KC - 1)

                with ExitStack() as st2:
                    cosT = S.sb("cosT", [P, SEQ], BF16, st2)
                    sinT = S.sb("sinT", [P, SEQ], BF16, st2)
                    with ExitStack() as st3:
                        posi = S.sb("posi", [P, SEQ], I32, st3)
                        u = S.sb("rope_u", [P, SEQ], F32, st3)
                        f = S.sb("rope_f", [P, SEQ], F32, st3)
                        ki = S.sb("rope_ki", [P, SEQ], I32, st3)
                        S.dma("sp", posi, posi[:], pos_d, pos_h[s:s + 1, :].partition_broadcast(P))
                        CP("dve", u, u[:], posi, posi[:])
                        TS("dve", u, u[:], u, u[:], cst[:, CS_INV:CS_INV + 1], 1.0 / (2.0 * math.pi), ALU.mult, ALU.mult, reads=[cst])
                        for tab, off, sgn in ((sinT, 0.0, True), (cosT, 0.25, False)):
                            if off != 0.0:
                                TS("dve", f, f[:], u, u[:], off, None, ALU.add)
                                src = f
                            else:
                                src = u
                            CP("dve", ki, ki[:], src, src[:])
                            fk = S.sb("rope_fk" + ("s" if sgn else "c"), [P, SEQ], F32, st3)
                            CP("dve", fk, fk[:], ki, ki[:])
                            TT("dve", fk, fk[:], src, src[:], fk, fk[:], ALU.subtract)
                            TS("dve", fk, fk[:], fk, fk[:], 0.49999, -0.49999, ALU.min, ALU.max)
                            if sgn:
                                ACTF(fk, fk[:], fk, fk[:], ACT.Sin, scale=2.0 * math.pi)
                                TS("dve", tab, tab[:], fk, fk[:], cst[:, CS_SGN:CS_SGN + 1], None, ALU.mult, reads=[cst])
                            else:
                                ACTF(tab, tab[:], fk, fk[:], ACT.Sin, scale=2.0 * math.pi)
                    S.barrier()
                    state_f = S.sb("state_f", [P, 4, P], F32, st2)
                    state_b = S.sb("state_b", [P, 4, P], BF16, st2)
                    gn_bc = S.sb("gn_bc", [P, 512], F32, st2)
                    S.dma("sp", gn_bc, gn_bc[:], wts_d, ret_gn_h[l:l + 1, :].partition_broadcast(P))
                    HS = SEQ // 2
                    qT = [S.sb("r_qT0", [P, 4, HS], BF16, st2), TView(oT["conv"], oT["conv"][:, 0:2, :].rearrange("p a (b t) -> p (a b) t", b=2))]
                    kT = [S.sb("r_kT0", [P, 4, HS], BF16, st2), TView(oT["conv"], oT["conv"][:, 2:4, :].rearrange("p a (b t) -> p (a b) t", b=2))]
                    v_tok = [S.sb("r_v0", [P, 8, 512], BF16, st2), TView(oT["att"], oT["att"][:, 0:2, :].rearrange("p a (b t) -> p (a b) t", b=4))]
                    gs = [S.sb("r_gs0", [P, 8, 512], BF16, st2), TView(oT["att"], oT["att"][:, 2:4, :].rearrange("p a (b t) -> p (a b) t", b=4))]
                    tpool = Pool("tblk", [P, KC, 512], BF16, 2, st2)
                    t1p = Pool("r_t1", [P, 512], F32, 2, st2)
                    t2p = Pool("r_t2", [P, 512], F32, 2, st2)
                    sTm_p = Pool("r_sTm", [P, 512], BF16, 2, st2)
                    qd_p = Pool("r_qd", [P, 4, P], BF16, 2, st2)
                    kt_p = Pool("r_kt", [P, 512], BF16, 2, st2)
                    on_p = Pool("r_on", [P, 512], F32, 2, st2)
                    og_p = Pool("r_og", [P, 512], F32, 2, st2)
                    st_p = Pool("r_stats", [P, 16], F32, 2, st2)
                    sgt_p = Pool("r_sg", [P, 512], F32, 2, st2)
                    junk = S.sb("r_junk", [P, P], BF16, st2)
                    def r_units(seg):
                        us = []
                        for which, cbase, dst in (("q", C_RQ, qT[seg]), ("k", C_RK, kT[seg])):
                            for h in range(4):
                                def ld(cbase=cbase, h=h):
                                    return (load_f(cbase + h * P), load_f(cbase + h * P, swap=True))

                                def cp(w, dst=dst, h=h, seg=seg):
                                    wb, wsw = w
                                    for qq in range(2):
                                        q = seg * 2 + qq
                                        pa, pb = ps[2 * (qq % 2)], ps[2 * (qq % 2) + 1]
                                        f_mm(wb, q, pa)
                                        f_mm(wsw, q, pb)
                                        t1, t2 = t1p.get(), t2p.get()
                                        TT("dve", t1, t1[:], pa, pa[:], cosT, cosT[:, q * 512:(q + 1) * 512], ALU.mult)
                                        TT("dve", t2, t2[:], pb, pb[:], sinT, sinT[:, q * 512:(q + 1) * 512], ALU.mult)
                                        TT("pool", dst, dst[:, h, qq * 512:(qq + 1) * 512], t1, t1[:], t2, t2[:], ALU.add)
                                us.append((ld, cp))
                        for which, cbase in (("v", C_RV), ("g", C_RG)):
                            def ld(cbase=cbase):
                                wb = tpool.get()
                                S.dma("pool", wb, wb[:], wts_d, wcols(w_in, cbase, 512))
                                return wb

                            def cp(wb, which=which, seg=seg):
                                for jl in range(8):
                                    j = seg * 8 + jl
                                    q, jj = divmod(j, 4)
                                    pt = ps[4 + jl % 2]
                                    for kc in range(KC):
                                        MM(pt, pt[:], hT[q], hT[q][:, kc, jj * P:(jj + 1) * P], wb, wb[:, kc, :], kc == 0, kc == KC - 1)
                                    if which == "v":
                                        CP("act", v_tok[seg], v_tok[seg][:, jl, :], pt, pt[:])
                                    else:
                                        sg = sgt_p.get()
                                        ACTF(sg, sg[:], pt, pt[:], ACT.Silu)
                                        TT("pool", gs[seg], gs[seg][:, jl, :], sg, sg[:], gn_bc, gn_bc[:], ALU.mult)
                            us.append((ld, cp))
                        return us

                    def r_rec(seg, jl):
                        qT_, kT_, v_tok_, gs_ = qT[seg], kT[seg], v_tok[seg], gs[seg]
                        if True:
                            j = seg * 8 + jl
                            tsl = slice(jl * P, (jl + 1) * P)
                            pS, pO, pK, pT_ = ps[0 + (jl % 2)], ps[2 + (jl % 2)], ps[6], ps[7]
                            for h in range(4):
                                MM(pS, pS[:, h * P:(h + 1) * P], kT_, kT_[:, h, tsl], qT_, qT_[:, h, tsl])
                            sTm = sTm_p.get()
                            TT("dve", sTm, sTm[:], pS, pS[:], cst, cst[:, CS_MASK:CS_MASK + 512], ALU.mult)
                            qd = qd_p.get()
                            TT("pool", qd, qd[:], qT_, qT_[:, :, tsl], cst,
                               cst[:, CS_QDEC:CS_QDEC + 512].rearrange("p (h c) -> p h c", h=4), ALU.mult)
                            pTb = pT_[:, 0:256].bitcast(BF16)
                            for h in range(4):
                                S.op("pe", lambda e, h=h, tsl=tsl, pTb=pTb: e.transpose(
                                    out=pTb[:, h * P:(h + 1) * P], in_=kT_[:, h, tsl], identity=identb[:]),
                                    [kT_, identb], [pT_])
                            kt = kt_p.get()
                            for h in range(4):
                                TS("dve", kt, kt[:, h * P:(h + 1) * P], pT_, pTb[:, h * P:(h + 1) * P],
                                   cst[:, CS_KDEC + h:CS_KDEC + h + 1], None, ALU.mult, reads=[cst])
                            for h in range(4):
                                hs = slice(h * P, (h + 1) * P)
                                MM(pO, pO[:, hs], sTm, sTm[:, hs], v_tok_, v_tok_[:, jl, hs], True, j == 0)
                                if j > 0:
                                    MM(pO, pO[:, hs], qd, qd[:, h, :], state_b, state_b[:, h, :], False, True)
                            if j < NT - 1:
                                for h in range(4):
                                    hs = slice(h * P, (h + 1) * P)
                                    MM(pK, pK[:, hs], kt, kt[:, hs], v_tok_, v_tok_[:, jl, hs])
                                if j == 0:
                                    CP("dve", state_f, state_f[:], pK, pK[:].rearrange("p (h e) -> p h e", h=4))
                                else:
                                    for h in range(4):
                                        STT("dve", state_f, state_f[:, h, :], state_f, state_f[:, h, :], RET_CD[h],
                                            pK, pK[:, h * P:(h + 1) * P], ALU.mult, ALU.add)
                                CP("act", state_b, state_b[:], state_f, state_f[:])
                            stt = st_p.get()
                            MSET("dve", stt, stt[:], 0.0)
                            for h in range(4):
                                hs = slice(h * P, (h + 1) * P)
                                ACTF(junk, junk[:], pO, pO[:, hs], ACT.Copy, accum=(stt, stt[:, h:h + 1]))
                                ACTF(junk, junk[:], pO, pO[:, hs], ACT.Square, accum=(stt, stt[:, 4 + h:5 + h]))
                            TS("dve", stt, stt[:, 0:8], stt, stt[:, 0:8], 1.0 / P, None, ALU.mult)
                            TT("dve", stt, stt[:, 8:12], stt, stt[:, 0:4], stt, stt[:, 0:4], ALU.mult)
                            TT("dve", stt, stt[:, 12:16], stt, stt[:, 4:8], stt, stt[:, 8:12], ALU.subtract)
                            TS("dve", stt, stt[:, 12:16], stt, stt[:, 12:16], EPS, None, ALU.add)
                            RSTD(stt, stt[:, 12:16])
                            on = on_p.get()
                            for h in range(4):
                                hs = slice(h * P, (h + 1) * P)
                                TS("dve", on, on[:, hs], pO, pO[:, hs], stt[:, h:h + 1], stt[:, 12 + h:13 + h],
                                   ALU.subtract, ALU.mult, reads=[stt])
                            og = og_p.get()
                            TT("pool", og, og[:], on, on[:], gs_, gs_[:, jl, :], ALU.mult)
                            pX = ps[4 + jl % 2]
                            for h in range(4):
                                TR(pX, pX[:, h * P:(h + 1) * P], og, og[:, h * P:(h + 1) * P], *ident)
                            CP("act", oT["ret"], oT["ret"][:, :, j * P:(j + 1) * P], pX, pX[:].rearrange("p (h c) -> p h c", h=4))
                    u1 = r_units(1)
                    mixed = []
                    for i in range(max(8, len(u1))):
                        if i < 8:
                            mixed.append((None, lambda _w, i=i: r_rec(0, i)))
                        if i < len(u1):
                            mixed.append(u1[i])
                    run_units(r_units(0) + mixed + [(None, lambda _w, i=i: r_rec(1, i)) for i in range(8)])
                S.barrier()
                if dbg == "ret":
                    return "stop"
                with ExitStack() as st2:
                    zT = S.sb("c_zT", [P, 4, 30 + SEQ], BF16, st2)
                    acc = S.sb("c_acc", [P, 4, SEQ], F32, st2)
                    sgp = Pool("c_sg", [P, 512], F32, 2, st2)
                    dgp = Pool("c_dg", [P, P], BF16, 6, st2)
                    MSET("dve", zT, zT[:, :, 0:30], 0.0)
                    def c_unit(cc):
                        def ld():
                            return (load_f(C_CU + cc * P), load_f(C_CU + 512 + cc * P))

                        def cp(w):
                            wa, wb = w
                            for q in range(NQ):
                                pa, pb = ps[2 * (q % 2)], ps[2 * (q % 2) + 1]
                                f_mm(wa, q, pa)
                                f_mm(wb, q, pb)
                                sg = sgp.get()
                                ACTF(sg, sg[:], pb, pb[:], ACT.Sigmoid)
                                TT("dve", zT, zT[:, cc, 30 + q * 512:30 + (q + 1) * 512], pa, pa[:], sg, sg[:], ALU.mult)
                            for k in range(31):
                                dg = dgp.get()
                                wk = pp[:, PP_CW + cc * 31 + k:PP_CW + cc * 31 + k + 1]
                                TS("pool" if k % 2 else "dve", dg, dg[:], identb, identb[:], wk, None, ALU.mult, reads=[pp])
                                for q in range(NQ):
                                    pc = ps[4 + q]
                                    MM(pc, pc[:], dg, dg[:], zT, zT[:, cc, q * 512 + k:q * 512 + k + 512], k == 0, k == 30)
                            for q in range(NQ):
                                pc = ps[4 + q]
                                TS("dve", acc, acc[:, cc, q * 512:(q + 1) * 512], pc, pc[:], pp[:, PP_CB + cc:PP_CB + cc + 1], None,
                                   ALU.add, reads=[pp])
                        return (ld, cp)

                    run_units([c_unit(cc) for cc in range(4)])
                    sqp = Pool("c_sq", [P, 512], F32, 2, st2)
                    m2p = Pool("c_m2", [P, 512], F32, 2, st2)
                    rsp = Pool("c_rs", [P, 512], F32, 2, st2)
                    tp = Pool("c_t", [P, 512], F32, 2, st2)
                    for q in range(NQ):
                        qs = slice(q * 512, (q + 1) * 512)
                        pm, pe2 = ps[4 + 2 * (q % 2)], ps[5 + 2 * (q % 2)]
                        for cc in range(4):
                            MM(pm, pm[:], onesln, onesln[:], acc, acc[:, cc, qs], cc == 0, cc == 3)
                        for cc in range(4):
                            sq = sqp.get()
                            ACTF(sq, sq[:], acc, acc[:, cc, qs], ACT.Square)
                            MM(pe2, pe2[:], onesln, onesln[:], sq, sq[:], cc == 0, cc == 3)
                        m2 = m2p.get()
                        ACTF(m2, m2[:], pm, pm[:], ACT.Square)
                        rs = rsp.get()
                        TT("dve", rs, rs[:], pe2, pe2[:], m2, m2[:], ALU.subtract)
                        TS("dve", rs, rs[:], rs, rs[:], EPS, None, ALU.add)
                        RSTD(rs, rs[:])
                        for cc in range(4):
                            t = tp.get()
                            TT("dve", t, t[:], acc, acc[:, cc, qs], pm, pm[:], ALU.subtract)
                            TT("pool", t, t[:], t, t[:], rs, rs[:], ALU.mult)
                            ACTF(oT["conv"], oT["conv"][:, cc, qs], t, t[:], ACT.Silu,
                                 bias=pp[:, PP_LB + cc:PP_LB + cc + 1], scale=pp[:, PP_LG + cc:PP_LG + cc + 1], reads=[pp])
                S.barrier()
                if dbg == "conv":
                    return "stop"
                with ExitStack() as st2:
                    aqM = [S.sb(f"a_qM{i}", [P, 4, SEQ], BF16, st2) for i in range(2)]
                    MSET("dve", aqM[0], aqM[0][:], 0.0)
                    MSET("dve", aqM[1], aqM[1][:], 0.0)
                    akT = S.sb("a_kT", [P, 4, SEQ], BF16, st2)
                    vaug = S.sb("a_v", [P, NT, 8, 66], BF16, st2)
                    biasT = S.sb("a_bias", [P, 5, 8, P], F32, st2)
                    S.dma("sp", biasT, biasT[:], wts_d, biasx_h[l])
                    MSET("dve", biasT, biasT[0:64, 0, :, 64:128], NEG)
                    MSET("dve", biasT, biasT[64:128, 4, :, 0:64], NEG)
                    MSET("dve", vaug, vaug[:], 1.0)
                    with ExitStack() as st3:
                        sqp = Pool("a_sq", [P, 512], BF16, 2, st3)
                        rsp = Pool("a_rs", [P, 512], F32, 2, st3)
                        def a_unit(dst, cbase, gcol, c):
                            def ld():
                                return load_f(cbase + c * P)

                            def cp(wb):
                                for q in range(NQ):
                                    qs = slice(q * 512, (q + 1) * 512)
                                    pa, pb = ps[2 * (q % 2)], ps[2 * (q % 2) + 1]
                                    f_mm(wb, q, pa)
                                    sq = sqp.get()
                                    ACTF(sq, sq[:], pa, pa[:], ACT.Square)
                                    MM(pb, pb[:], blk64, blk64[:], sq, sq[:])
                                    rs = rsp.get()
                                    TS("dve", rs, rs[:], pb, pb[:], EPS, None, ALU.add)
                                    RSTD(rs, rs[:])
                                    if dst is None:
                                        for i in range(2):
                                            hp = slice(64 * i, 64 * i + 64)
                                            STT("dve", aqM[i], aqM[i][hp, c, qs], pa, pa[hp, :], pp[hp, gcol:gcol + 1], rs, rs[hp, :],
                                                ALU.mult, ALU.mult, reads=[pp])
                                    else:
                                        STT("dve", dst, dst[:, c, qs], pa, pa[:], pp[:, gcol:gcol + 1], rs, rs[:],
                                            ALU.mult, ALU.mult, reads=[pp])
                            return (ld, cp)

                        def av_ld():
                            wb = tpool.get()
                            S.dma("pool", wb, wb[:], wts_d, wcols(w_in, C_AV, 512))
                            return wb

                        def av_cp(wb):
                            for j in range(NT if dbg != "att0" else 0):
                                q, jj = divmod(j, 4)
                                pt = ps[4 + j % 2]
                                for kc in range(KC):
                                    MM(pt, pt[:], hT[q], hT[q][:, kc, jj * P:(jj + 1) * P], wb, wb[:, kc, :], kc == 0, kc == KC - 1)
                                CP("act", vaug, vaug[:, j, :, 0:64], pt, pt[:].rearrange("p (h d) -> p h d", h=8))

                        tpool = Pool("tblk", [P, KC, 512], BF16, 1, st3)
                        run_units([a_unit(dst, cbase, gcol, c) for dst, cbase, gcol in ((None, C_AQ, PP_QG), (akT, C_AK, PP_KG))
                                   for c in range(4)] + [(av_ld, av_cp)])
                    S.barrier()
                    ep = Pool("a_e", [P, 512], F32, 2, st2)
                    pTp = Pool("a_pT", [P, 5, 512], BF16, 2, st2)
                    recp = Pool("a_rec", [P, 8], F32, 2, st2)
                    oap = Pool("a_o", [P, 512], F32, 2, st2)
                    for j in range(NT if dbg not in ("att0", "att1") else 0):
                        tsl = slice(j * P, (j + 1) * P)
                        kbs = [kb for kb in range(5) if j - 4 + kb >= 0]
                        oa = oap.get()
                        rec = recp.get()
                        for g in range(2):
                            pTt = pTp.get()
                            for kb in kbs:
                                jk = j - 4 + kb
                                ksl = slice(jk * P, (jk + 1) * P)
                                pq = ps[kb % 2 + 2 * g]
                                for hh in range(4):
                                    h = g * 4 + hh
                                    c, r0 = h // 2, 64 * (h % 2)
                                    MM(pq, pq[:, hh * P:(hh + 1) * P], akT, akT[:, c, ksl], aqM[h % 2], aqM[h % 2][:, c, tsl])
                                e_ = ep.get()
                                STT("dve", e_, e_[:].rearrange("p (h q) -> p h q", h=4), pq,
                                    pq[:].rearrange("p (h q) -> p h q", h=4), 0.125,
                                    biasT, biasT[:, kb, g * 4:(g + 1) * 4, :], ALU.mult, ALU.add)
                                ACTF(pTt, pTt[:, kb, :], e_, e_[:], ACT.Exp)
                            po = ps[4 + g]
                            if dbg == "att2":
                                continue
                            for hh in range(4):
                                h = g * 4 + hh
                                for i, kb in enumerate(kbs):
                                    jk = j - 4 + kb
                                    MM(po, po[:, hh * 66:(hh + 1) * 66], pTt, pTt[:, kb, hh * P:(hh + 1) * P],
                                       vaug, vaug[:, jk, h, :], i == 0, i == len(kbs) - 1)
                            pov = po[:, 0:264].rearrange("p (h d) -> p h d", h=4)
                            S.op("dve", lambda e, rec=rec, pov=pov, g=g: e.reciprocal(
                                out=rec[:, g * 4:(g + 1) * 4].unsqueeze(2), in_=pov[:, :, 64:65]), [po], [rec])
                            for hh in range(4):
                                h = g * 4 + hh
                                TS("dve", oa, oa[:, h * 64:(h + 1) * 64], po, po[:, hh * 66:hh * 66 + 64],
                                   rec[:, h:h + 1], None, ALU.mult, reads=[rec])
                        if dbg in ("att2", "att3"):
                            continue
                        pX = ps[6 + j % 2]
                        for c in range(4):
                            TR(pX, pX[:, c * P:(c + 1) * P], oa, oa[:, c * P:(c + 1) * P], *ident)
                        CP("act", oT["att"], oT["att"][:, :, tsl], pX, pX[:].rearrange("p (c t) -> p c t", c=4))
                for i_, k_ in enumerate(("ret", "conv", "att")):
                    DUMP(1 + i_, oT[k_], oT[k_][:].rearrange("p k t -> p (k t)"), 8192)
                S.barrier()
                if dbg and dbg.startswith("att"):
                    return "stop"
                with ExitStack() as st2:
                    mT = [S.sb(f"mT{q}", [P, KC, 512], BF16, st2) for q in range(NQ)]
                    glp = Pool("s4_gl", [P, KC, 3, P], BF16, 2, st2)
                    wop = Pool("s4_wo", [P, 4, 3, P], BF16, 2, st2)
                    gtp = Pool("s4_g", [P, 512], F32, 3, st2)
                    tmp = Pool("s4_t", [P, 512], F32, 2, st2)
                    macc = Pool("s4_m", [P, 512], F32, 2, st2)
                    outs_w = (w_ro_h[l], w_co_h[l], w_ao_h[l])
                    keys = ("ret", "conv", "att")
                    def s4_unit(c):
                        def ld():
                            gw = glp.get()
                            ow = wop.get()
                            for b in range(3):
                                S.dma("pool", gw, gw[:, :, b, :], wts_d, wcols(w_in, C_GL + b * D + c * P, P))
                                S.dma("pool", ow, ow[:, :, b, :], wts_d, wcols(outs_w[b], c * P, P))
                            return (gw, ow)

                        def cp(w):
                            gw, ow = w
                            for q in range(NQ):
                                qs = slice(q * 512, (q + 1) * 512)
                                m = macc.get()
                                for b in range(3):
                                    pg, py = ps[2 * (b % 2)], ps[2 * (b % 2) + 1]
                                    for kc in range(KC):
                                        MM(pg, pg[:], gw, gw[:, kc, b, :], hT[q], hT[q][:, kc, :], kc == 0, kc == KC - 1)
                                    for kc in range(4):
                                        MM(py, py[:], ow, ow[:, kc, b, :], oT[keys[b]], oT[keys[b]][:, kc, qs], kc == 0, kc == 3)
                                    gt = gtp.get()
                                    ACTF(gt, gt[:], pg, pg[:], ACT.Sigmoid,
                                         bias=pp[:, PP_BG + b * 8 + c:PP_BG + b * 8 + c + 1], reads=[pp])
                                    if b == 0:
                                        TT("dve", m, m[:], py, py[:], gt, gt[:], ALU.mult)
                                    elif b == 1:
                                        t = tmp.get()
                                        TT("dve", t, t[:], py, py[:], gt, gt[:], ALU.mult)
                                        TT("pool", m, m[:], m, m[:], t, t[:], ALU.add)
                                    else:
                                        t = tmp.get()
                                        TT("dve", t, t[:], py, py[:], gt, gt[:], ALU.mult)
                                        TT("pool", mT[q], mT[q][:, c, :], m, m[:], t, t[:], ALU.add)
                        return (ld, cp)

                    wo = S.sb("s4_wout", [P, KC, D], BF16, st2)
                    gate1 = load_mod_bc(st2, l, s, 2, "gate1")
                    xs_pool = Pool("s4_xs", [P, D], F32, 2, st2)

                    def wo_ld():
                        S.dma("pool", wo, wo[:], wts_d, wcols(w_out_h[l], 0, D))
                        return wo

                    def wo_cp(_w):
                        DUMP(4, mT[0], mT[0][:].rearrange("p k t -> p (k t)"), 4096)
                        s4_out()

                    def s4_out():
                        for j in range(NT):
                            q, jj = divmod(j, 4)
                            xs = xs_pool.get()
                            if first:
                                S.dma("sp", xs, xs[:], x_d, x_h[s, j * P:(j + 1) * P, :])
                            else:
                                S.dma("sp", xs, xs[:], xo_d[s][j], out_h[s, j * P:(j + 1) * P, :])
                            for half in range(2):
                                pt = ps[4 + (2 * j + half) % 4]
                                hsl = slice(half * 512, (half + 1) * 512)
                                for kc in range(KC):
                                    MM(pt, pt[:], mT[q], mT[q][:, kc, jj * P:(jj + 1) * P], wo, wo[:, kc, hsl], kc == 0, kc == KC - 1)
                                t = tmp.get()
                                TT("dve", t, t[:], pt, pt[:], gate1, gate1[:, hsl], ALU.mult)
                                TT("pool", xs, xs[:, hsl], xs, xs[:, hsl], t, t[:], ALU.add)
                            S.dma("sp", xo_d[s][j], out_h[s, j * P:(j + 1) * P, :], xs, xs[:], sem_t=xs)
                    run_units([s4_unit(c) for c in range(KC)] + [(wo_ld, wo_cp)])
                S.barrier()
            S.barrier()

        def moe(s, l):
            with ExitStack() as st:
                hT = [S.sb(f"hT{q}", [P, KC, 512], BF16, st) for q in range(NQ)]
                acc = S.sb("m_acc", [P, NT, D], F32, st)
                wge = S.sb("m_wge", [P, NT, 32], F32, st)
                upp = Pool("m_wup", [P, KC, 512], BF16, 2, st)
                dnp = Pool("m_wdn", [P, 2, D], BF16, 2, st)

                def e_ld(ge):
                    g, e_ = divmod(ge, NE)
                    wu = upp.get()
                    wd = dnp.get()
                    S.dma("pool", wu, wu[:], wts_d, wcols(w_up_h[l, g, e_], 0, 512))
                    S.dma("pool", wd, wd[:], wts_d, w_dn_h[l, g, e_].rearrange("(fc p) n -> p fc n", p=P))
                    return (wu, wd)

                e_nxt = e_ld(0) if n_experts > 0 else None
                with ExitStack() as st1:
                    gmod = load_mod_bc(st1, l, s, 4, "gmod2")
                    shift = load_mod_bc(st1, l, s, 3, "shift2")
                    wr = S.sb("m_wr", [P, KC, 36], F32, st1)
                    S.dma("sp", wr, wr[:], wts_d, wr_h[l])
                    brow = S.sb("m_brow", [P, 36], F32, st1)
                    S.dma("sp", brow, brow[:], wts_d, brow_h[l:l + 1, :].partition_broadcast(P))
                    whi = S.sb("m_whi", [P, KC, 36], BF16, st1)
                    wlo = S.sb("m_wlo", [P, KC, 36], BF16, st1)
                    CP("dve", whi, whi[:], wr, wr[:])
                    TT("dve", wlo, wlo[:], wr, wr[:], whi, whi[:], ALU.subtract)
                    h2p = Pool("m_h2lo", [P, KC, P], BF16, 2, st1)
                    rp = Pool("m_r", [P, 96], F32, 2, st1)

                    def router(j, pa, pb):
                        q, jj = divmod(j, 4)
                        tsl = slice(jj * P, (jj + 1) * P)
                        h2 = h2p.get()
                        TT("dve", h2, h2[:, 0:4, :], pa, pa[:].rearrange("p (k t) -> p k t", k=4),
                           hT[q], hT[q][:, 0:4, tsl], ALU.subtract)
                        TT("dve", h2, h2[:, 4:8, :], pb, pb[:].rearrange("p (k t) -> p k t", k=4),
                           hT[q], hT[q][:, 4:8, tsl], ALU.subtract)
                        pl = ps[4 + j % 2]
                        for kc in range(KC):
                            MM(pl, pl[:, 0:36], hT[q], hT[q][:, kc, tsl], whi, whi[:, kc, :], kc == 0, False)
                            MM(pl, pl[:, 0:36], hT[q], hT[q][:, kc, tsl], wlo, wlo[:, kc, :], False, False)
                            MM(pl, pl[:, 0:36], h2, h2[:, kc, :], whi, whi[:, kc, :], False, kc == KC - 1)
                        r = rp.get()
                        MSET("dve", r, r[:], 0.0)
                        LG, GMX, OHG, NGM, GS, CH, M1, OH1, CH2, M2, OH2, DD, W1, W2, WE, EX = (
                            slice(0, 36), slice(36, 37), slice(37, 41), slice(41, 42), slice(42, 43), slice(43, 51),
                            slice(51, 52), slice(52, 60), slice(60, 68), slice(68, 69), slice(69, 77), slice(77, 78),
                            slice(78, 79), slice(79, 80), slice(80, 88), slice(88, 92))
                        R = lambda sl: r[:, sl]
                        TT("dve", r, R(LG), pl, pl[:, 0:36], brow, brow[:], ALU.add)
                        if dbg == "m_r1":
                            return
                        S.op("dve", lambda e: e.tensor_reduce(out=R(GMX), in_=r[:, 0:4], axis=AX.X, op=ALU.max), [r], [r])
                        TS("dve", r, R(OHG), r, r[:, 0:4], R(GMX), None, ALU.is_ge)
                        TS("dve", r, R(NGM), r, R(GMX), -1.0, None, ALU.mult)
                        ACTF(r, R(EX), r, r[:, 0:4], ACT.Exp, bias=R(NGM), accum=(r, R(GS)))
                        S.op("dve", lambda e: e.reciprocal(out=R(GS), in_=R(GS)), [r], [r])
                        if dbg == "m_r2":
                            return
                        TS("dve", r, R(CH), r, r[:, 4:12], r[:, 37:38], None, ALU.mult)
                        for g in range(1, 4):
                            STT("dve", r, R(CH), r, r[:, 4 + g * 8:12 + g * 8], r[:, 37 + g:38 + g], r, R(CH), ALU.mult, ALU.add)
                        S.op("dve", lambda e: e.tensor_reduce(out=R(M1), in_=R(CH), axis=AX.X, op=ALU.max), [r], [r])
                        TS("dve", r, R(OH1), r, R(CH), R(M1), None, ALU.is_ge)
                        STT("dve", r, R(CH2), r, R(OH1), NEG, r, R(CH), ALU.mult, ALU.add)
                        S.op("dve", lambda e: e.tensor_reduce(out=R(M2), in_=R(CH2), axis=AX.X, op=ALU.max), [r], [r])
                        TS("dve", r, R(OH2), r, R(CH2), R(M2), None, ALU.is_ge)
                        TT("dve", r, R(DD), r, R(M2), r, R(M1), ALU.subtract)
                        ACTF(r, R(W2), r, R(DD), ACT.Sigmoid)
                        TS("dve", r, R(W1), r, R(W2), -1.0, 1.0, ALU.mult, ALU.add)
                        TT("dve", r, R(W1), r, R(W1), r, R(GS), ALU.mult)
                        TT("dve", r, R(W2), r, R(W2), r, R(GS), ALU.mult)
                        TS("dve", r, R(WE), r, R(OH1), R(W1), None, ALU.mult)
                        STT("dve", r, R(WE), r, R(OH2), R(W2), r, R(WE), ALU.mult, ALU.add)
                        if dbg == "m_r3":
                            return
                        for g in range(4):
                            TS("dve", wge, wge[:, j, g * 8:(g + 1) * 8], r, R(WE), r[:, 37 + g:38 + g], None, ALU.mult, reads=[r])

                    norm_and_transpose(st1, l, s, False, gmod, shift, hT, h2f_cb=(None if dbg == "m_norouter" else router))
                S.barrier()
                with ExitStack() as st2:
                    sap = Pool("m_sa", [P, 512], F32, 2, st2)
                    actp = Pool("m_act", [P, 2, 512], BF16, 2, st2)
                    dcount = 0
                    for ge in range(n_experts):
                        g, e_ = divmod(ge, NE)
                        wu, wd = e_nxt
                        e_nxt = e_ld(ge + 1) if ge + 1 < n_experts else None
                        for q in range(NQ):
                            for fc in range(4):
                                pu = ps[fc]
                                for kc in range(KC):
                                    MM(pu, pu[:], wu, wu[:, kc, fc * P:(fc + 1) * P], hT[q], hT[q][:, kc, :], kc == 0, kc == KC - 1)
                            at = actp.get()
                            for fc in range(2):
                                sa = sap.get()
                                ACTF(sa, sa[:], ps[fc], ps[fc][:], ACT.Silu)
                                TT("dve", at, at[:, fc, :], sa, sa[:], ps[fc + 2], ps[fc + 2][:], ALU.mult)
                            for jj in range(4):
                                j = q * 4 + jj
                                for half in range(2):
                                    pd = ps[4 + dcount % 4]
                                    dcount += 1
                                    hsl = slice(half * 512, (half + 1) * 512)
                                    for fc in range(2):
                                        MM(pd, pd[:], at, at[:, fc, jj * P:(jj + 1) * P], wd, wd[:, fc, hsl], fc == 0, fc == 1)
                                    if ge == 0:
                                        TS("dve", acc, acc[:, j, hsl], pd, pd[:], wge[:, j, ge:ge + 1], None, ALU.mult, reads=[wge])
                                    else:
                                        STT("dve", acc, acc[:, j, hsl], pd, pd[:], wge[:, j, ge:ge + 1], acc, acc[:, j, hsl],
                                            ALU.mult, ALU.add, reads=[wge])
                    gate2 = load_mod_bc(st2, l, s, 5, "gate2")
                    xs_pool = Pool("m_xs", [P, D], F32, 2, st2)
                    toks = []
                    for j in range(NT):
                        xs = xs_pool.get()
                        S.dma("sp", xs, xs[:], xo_d[s][j], out_h[s, j * P:(j + 1) * P, :])
                        TT("pool", acc, acc[:, j, :], acc, acc[:, j, :], gate2, gate2[:], ALU.mult)
                        TT("pool", xs, xs[:], xs, xs[:], acc, acc[:, j, :], ALU.add)
                        toks.append(S.dma("sp", xo_d[s][j], out_h[s, j * P:(j + 1) * P, :], xs, xs[:], sem_t=xs))
                S.barrier()
                return toks

        identb = S.sb("identb", [P, P], BF16)
        CP("dve", identb, identb[:], cst, cst[:, CS_ID:CS_ID + P])

        final = []
        try:
            if dbg and dbg.startswith("pro"):
                raise _Stop()
            for s in range(n_seq):
                for l in range(n_layers):
                    if mixer(s, l) == "stop":
                        raise _Stop()
                    final = moe(s, l) if n_experts > 0 else []
        except _Stop:
            S.barrier()
        S.emit(final_toks=S.dma_toks + final + dump_toks)
    return nc


def _constants():
    cst = np.zeros((P, NCST), np.float32)
    cst[:, CS_ID:CS_ID + P] = np.eye(P, dtype=np.float32)
    m = np.arange(P)[:, None].astype(np.float64)
    c = np.arange(P)[None, :].astype(np.float64)
    for h in range(4):
        gamma = 1.0 - 2.0 ** (-5 - h)
        mask = np.where(c >= m, gamma ** np.maximum(c - m, 0.0), 0.0) * (128.0 ** -0.5)
        cst[:, CS_MASK + h * P:CS_MASK + (h + 1) * P] = mask
        cst[:, CS_QDEC + h * P:CS_QDEC + (h + 1) * P] = (gamma ** (c + 1.0))
        cst[:, CS_KDEC + h] = (gamma ** (127.0 - m[:, 0])) * (128.0 ** -0.5)
    half = 64
    inv = (np.float32(10000.0) ** (-np.arange(half, dtype=np.float32) / np.float32(half))).astype(np.float32)
    cst[:, CS_INV] = np.tile(inv, 2)
    cst[0:64, CS_SGN] = -1.0
    cst[64:128, CS_SGN] = 1.0
    cst[0:64, CS_MLO] = 1.0
    cst[64:128, CS_MHI] = 1.0
    return cst


def _layout_params(inp):
    f32 = np.float32
    pp = np.zeros((L, P, NPP), f32)
    cw = np.asarray(inp["conv_w"], f32)[:, :, 0, :]
    pp[:, :, PP_CW:PP_CW + 124] = cw.reshape(L, 31, 4, P).transpose(0, 3, 2, 1).reshape(L, P, 124)
    pp[:, :, PP_CB:PP_CB + 4] = np.asarray(inp["conv_b"], f32).reshape(L, 4, P).transpose(0, 2, 1)
    pp[:, :, PP_LG:PP_LG + 4] = np.asarray(inp["conv_ln_g"], f32).reshape(L, 4, P).transpose(0, 2, 1)
    pp[:, :, PP_LB:PP_LB + 4] = np.asarray(inp["conv_ln_b"], f32).reshape(L, 4, P).transpose(0, 2, 1)
    pp[:, :, PP_QG] = np.tile(np.asarray(inp["att_q_gain"], f32), (1, 2))
    pp[:, :, PP_KG] = np.tile(np.asarray(inp["att_k_gain"], f32), (1, 2))
    pp[:, :, PP_BG:PP_BG + 24] = np.asarray(inp["b_gate"], f32).reshape(L, 24, P).transpose(0, 2, 1)
    wr = np.concatenate([np.asarray(inp["w_group"], f32), np.asarray(inp["w_inner"], f32)], axis=-1)
    wr = np.ascontiguousarray(wr.reshape(L, KC, P, 36).transpose(0, 2, 1, 3))
    brow = np.ascontiguousarray(np.concatenate([np.asarray(inp["b_group"], f32), np.asarray(inp["b_inner"], f32)], axis=-1))
    k = np.arange(P)[:, None, None]
    kb = np.arange(5)[None, :, None]
    q = np.arange(P)[None, None, :]
    idx = np.clip(q - k + 128 * (4 - kb), -256, 256) + 256
    tab = np.asarray(inp["att_rel_bias"], f32)
    bx = tab[:, :, idx]
    biasx = np.ascontiguousarray(bx.transpose(0, 2, 3, 1, 4))
    return pp, wr, brow, biasx


_PROG = {}


def kernel(**inp):
    n = 8
    f32 = np.float32
    x = np.asarray(inp["x"], f32)
    c = np.asarray(inp["c"], f32)
    pos = np.asarray(inp["positions"], np.int32)
    pp, wr, brow, biasx = _layout_params(inp)
    cst = _constants()
    shared = dict(
        w_ada=np.asarray(inp["w_ada"], f32), b_ada=np.asarray(inp["b_ada"], f32),
        g_mix=np.asarray(inp["g_mix"], f32), g_ffn=np.asarray(inp["g_ffn"], f32),
        w_in=np.asarray(inp["w_in"], f32), ret_gn=np.asarray(inp["ret_gn"], f32),
        w_ret_out=np.asarray(inp["w_ret_out"], f32), w_conv_out=np.asarray(inp["w_conv_out"], f32),
        w_att_out=np.asarray(inp["w_att_out"], f32), w_out=np.asarray(inp["w_out"], f32),
        w_up=np.asarray(inp["w_up"], f32), w_down=np.asarray(inp["w_down"], f32),
        pp=pp, wr=wr, brow=brow, biasx=biasx, cst=cst)
    if "nc" not in _PROG:
        _PROG["nc"] = build_program()
    nc = _PROG["nc"]
    in_maps = []
    for i in range(n):
        sl = slice(NSEQ * i, NSEQ * (i + 1))
        m = dict(shared)
        m["x"] = np.ascontiguousarray(x[sl])
        m["pos"] = np.ascontiguousarray(pos[sl])
        m["cT"] = np.ascontiguousarray(c[sl].reshape(NSEQ, KC, P).transpose(2, 1, 0))
        in_maps.append(m)
    res = run_bass_kernel_spmd(nc, in_maps, core_ids=list(range(n)))
    return np.concatenate([np.asarray(r["out"], f32) for r in res.results], axis=0)
```

```python
import math
import numpy as np
from concourse.bass_utils import run_bass_kernel_spmd

import numpy as np
import concourse.bass as bass
import concourse.mybir as mybir

F32 = mybir.dt.float32
BF16 = mybir.dt.bfloat16
I32 = mybir.dt.int32
ALU = mybir.AluOpType
ACT = mybir.ActivationFunctionType
AX = mybir.AxisListType

SAME_ENGINE_SYNC = False
SMALL_N = 512


class Op:
    __slots__ = ("eng", "fn", "deps", "signal", "semval", "dma_inc", "idx", "small")

    def __init__(self, eng, fn, deps, dma_inc=None):
        self.eng = eng
        self.fn = fn
        self.deps = deps
        self.signal = False
        self.semval = None
        self.dma_inc = dma_inc
        self.idx = None
        self.small = False


class DmaTok:
    __slots__ = ("sem", "val", "eng")

    def __init__(self, sem, val):
        self.sem = sem
        self.val = val
        self.eng = None


class T:
    def __init__(self, h, name=""):
        self.h = h
        self.name = name
        self.last_w = None
        self.readers = []
        self.dsem = None
        self.dcount = 0

    def __getitem__(self, k):
        return self.h[k]


class TView:
    def __init__(self, base, ap):
        self.__dict__["base"] = base
        self.__dict__["ap"] = ap

    def __getitem__(self, k):
        return self.ap[k]

    def __getattr__(self, k):
        return getattr(self.base, k)

    def __setattr__(self, k, v):
        setattr(self.base, k, v)


class Sched:
    ENGS = ("pe", "act", "dve", "pool", "sp")

    def __init__(self, nc):
        self.nc = nc
        self.ops = {e: [] for e in self.ENGS}
        self.sems = {}
        self.stack = None
        self.dma_sems = []
        self.free_dma_sems = []
        self.dma_toks = []
        self.uid = 0

    def set_stack(self, stack):
        self.stack = stack

    def sem(self, name):
        return self.stack.enter_context(self.nc.semaphore(name))

    def sb(self, name, shape, dtype, stack=None):
        st = stack or self.stack
        self.uid += 1
        name = f"{name}_{self.uid}"
        h = st.enter_context(self.nc.sbuf_tensor(name, list(shape), dtype))
        t = T(h, name)
        if st is not self.stack:
            st.callback(self._retire, t)
        return t

    def _retire(self, t):
        if t.dsem is not None:
            self.free_dma_sems.append((t.dsem, t.dcount))
            t.dsem = None

    def ps(self, name, shape, dtype=F32, stack=None):
        st = stack or self.stack
        h = st.enter_context(self.nc.psum_tensor(name, list(shape), dtype))
        return T(h, name)

    def dram(self, h, name=""):
        return T(h, name)

    def _deps(self, reads, writes):
        deps = []
        for t in reads:
            if t.last_w is not None:
                deps.append(t.last_w)
        for t in writes:
            if t.last_w is not None:
                deps.append(t.last_w)
            deps.extend(t.readers)
        return deps

    def _commit(self, tok, reads, writes):
        for t in reads:
            t.readers.append(tok)
            if len(t.readers) > 64:
                t.readers = self._compact(t.readers)
        for t in writes:
            t.last_w = tok
            t.readers = []

    @staticmethod
    def _compact(toks):
        last = {}
        for tk in toks:
            key = (tk.eng if isinstance(tk, Op) else id(tk.sem))
            last[key] = tk
        return list(last.values())

    def op(self, eng, fn, reads=(), writes=(), small=True):
        o = Op(eng, fn, self._deps(reads, writes))
        o.small = bool(small) and eng != "pe"
        o.idx = len(self.ops[eng])
        self.ops[eng].append(o)
        self._commit(o, reads, writes)
        return o

    def dma(self, q, out_t, out_ap, in_t, in_ap, sem_t=None, **kw):
        st = sem_t or out_t
        if st.dsem is None:
            if self.free_dma_sems:
                st.dsem, st.dcount = self.free_dma_sems.pop()
            else:
                st.dsem = self.sem("d_" + st.name)
                st.dcount = 0
        st.dcount += 16
        tok = DmaTok(st.dsem, st.dcount)
        deps = self._deps([in_t], [out_t])
        sem = st.dsem

        def fn(e, out_ap=out_ap, in_ap=in_ap, kw=kw):
            return e.dma_start(out=out_ap, in_=in_ap, **kw)

        o = Op(q, fn, deps, dma_inc=sem)
        o.idx = len(self.ops[q])
        self.ops[q].append(o)
        self._commit(tok, [in_t], [out_t])
        self.dma_toks.append(tok)
        return tok

    def barrier(self):
        lasts = []
        for e in self.ENGS:
            for o in reversed(self.ops[e]):
                if o.fn is not None and o.dma_inc is None:
                    lasts.append(o)
                    break
        toks = list(self.dma_toks)
        self.dma_toks = []
        for e in self.ENGS:
            deps = [o for o in lasts if o.eng != e] + toks
            if not deps:
                continue
            o = Op(e, None, deps)
            o.idx = len(self.ops[e])
            self.ops[e].append(o)

    def finalize(self):
        for e in self.ENGS:
            for o in self.ops[e]:
                for d in o.deps:
                    if isinstance(d, Op):
                        if d.eng != o.eng or SAME_ENGINE_SYNC or d.small:
                            d.signal = True
        self.counts = {}
        for e in self.ENGS:
            c = 0
            for o in self.ops[e]:
                if o.signal:
                    c += 1
                    o.semval = c
            self.counts[e] = c
            if c > 0 or True:
                self.sems[e] = self.sem("eng_" + e)

    def replay(self, e, handle):
        seen = {}
        nc = self.nc
        for o in self.ops[e]:
            waits = {}
            for d in o.deps:
                if isinstance(d, Op):
                    if d.eng == e and not (SAME_ENGINE_SYNC or d.small):
                        continue
                    key = ("e", d.eng)
                    sem = self.sems[d.eng]
                    val = d.semval
                else:
                    key = ("d", id(d.sem))
                    sem = d.sem
                    val = d.val
                if seen.get(key, 0) >= val:
                    continue
                if key not in waits or waits[key][1] < val:
                    waits[key] = (sem, val)
            for key, (sem, val) in waits.items():
                handle.wait_ge(sem, val)
                seen[key] = val
            if o.fn is None:
                continue
            ins = o.fn(handle)
            if o.dma_inc is not None:
                ins.then_inc(o.dma_inc, 16)
            elif o.signal:
                ins.then_inc(self.sems[e], 1)

    def emit(self, final_toks=()):
        nc = self.nc
        self.finalize()
        with nc.Block() as block:
            @block.tensor
            def _(h):
                self.replay("pe", h)

            @block.scalar
            def _(h):
                self.replay("act", h)

            @block.vector
            def _(h):
                self.replay("dve", h)

            @block.gpsimd
            def _(h):
                self.replay("pool", h)

            @block.sync
            def _(h):
                self.replay("sp", h)
                for tk in final_toks:
                    h.wait_ge(tk.sem, tk.val)

from contextlib import ExitStack

P = 128
SEQ = 2048
D = 1024
KC = 8
NT = 16
NQ = 4
L = 2
NSEQ = 2
IN_COLS = 7680
EPS = 1e-6
NEG = -1e30
C_RQ, C_RK, C_RV, C_RG = 0, 512, 1024, 1536
C_CU = 2048
C_AQ, C_AK, C_AV = 3072, 3584, 4096
C_GL = 4608
NG, NE = 4, 8
PP_CW = 0
PP_CB = 124
PP_LG = 128
PP_LB = 132
PP_QG = 136
PP_KG = 137
PP_BG = 138
NPP = 162
CS_ID = 0
CS_MASK = 128
CS_QDEC = 640
CS_KDEC = 1152
CS_INV = 1156
CS_SGN = 1157
CS_MLO = 1158
CS_MHI = 1159
NCST = 1160
RET_CD = [float((1.0 - 2.0 ** (-5 - h)) ** 128) for h in range(4)]


def build_program(n_layers=L, n_seq=NSEQ, n_experts=NG * NE, dbg=None, dump=False):
    nc = bass.Bass("TRN2", target_bir_lowering=False)
    dump_toks = []

    def din(name, shape, dt=F32):
        return nc.dram_tensor(name, list(shape), dt, kind="ExternalInput")

    x_h = din("x", [NSEQ, SEQ, D])
    pos_h = din("pos", [NSEQ, SEQ], I32)
    cT_h = din("cT", [P, KC, NSEQ])
    w_ada_h = din("w_ada", [L, D, 6 * D])
    b_ada_h = din("b_ada", [L, 6 * D])
    g_mix_h = din("g_mix", [L, D])
    g_ffn_h = din("g_ffn", [L, D])
    w_in_h = din("w_in", [L, D, IN_COLS])
    ret_gn_h = din("ret_gn", [L, 512])
    w_ro_h = din("w_ret_out", [L, 512, D])
    w_co_h = din("w_conv_out", [L, 512, D])
    w_ao_h = din("w_att_out", [L, 512, D])
    w_out_h = din("w_out", [L, D, D])
    w_up_h = din("w_up", [L, NG, NE, D, 512])
    w_dn_h = din("w_down", [L, NG, NE, 256, D])
    pp_h = din("pp", [L, P, NPP])
    wr_h = din("wr", [L, P, KC, 36])
    brow_h = din("brow", [L, 36])
    biasx_h = din("biasx", [L, P, 5, 8, P])
    cst_h = din("cst", [P, NCST])
    out_h = nc.dram_tensor("out", [NSEQ, SEQ, D], F32, kind="ExternalOutput")
    modd_h = nc.dram_tensor("modd", [L, NSEQ, 6 * D], F32)
    dbg_h = nc.dram_tensor("dbg", [P, 6, 8192], F32, kind="ExternalOutput") if dump else None

    top = ExitStack()
    with top:
        S = Sched(nc)
        S.set_stack(top)

        def MM(out_t, out_ap, lt, lap, rt, rap, start=True, stop=True):
            S.op("pe", lambda e: e.matmul(out_ap, lhsT=lap, rhs=rap, start=start, stop=stop),
                 [lt, rt], [out_t])

        def TR(out_t, out_ap, in_t, in_ap, id_t, id_ap):
            S.op("pe", lambda e: e.transpose(out=out_ap, in_=in_ap, identity=id_ap), [in_t, id_t], [out_t])

        def ACTF(out_t, out_ap, in_t, in_ap, func, bias=None, scale=None, accum=None, reads=()):
            kw = {}
            if bias is not None:
                kw["bias"] = bias
            if scale is not None:
                kw["scale"] = scale
            wr = [out_t]
            if accum is not None:
                kw["accum_out"] = accum[1]
                wr.append(accum[0])
            S.op("act", lambda e: e.activation(out=out_ap, in_=in_ap, func=func, **kw),
                 [in_t] + list(reads), wr, small=(accum is not None or out_ap.free_size() < SMALL_N))

        def TT(eng, out_t, out_ap, a_t, a_ap, b_t, b_ap, op):
            S.op(eng, lambda e: e.tensor_tensor(out=out_ap, in0=a_ap, in1=b_ap, op=op), [a_t, b_t], [out_t],
                 small=out_ap.free_size() < SMALL_N)

        def TS(eng, out_t, out_ap, a_t, a_ap, s1, s2, op0, op1=None, reads=()):
            if op1 is None:
                S.op(eng, lambda e: e.tensor_scalar(out=out_ap, in0=a_ap, scalar1=s1, scalar2=None, op0=op0),
                     [a_t] + list(reads), [out_t], small=out_ap.free_size() < SMALL_N)
            else:
                S.op(eng, lambda e: e.tensor_scalar(out=out_ap, in0=a_ap, scalar1=s1, scalar2=s2, op0=op0, op1=op1),
                     [a_t] + list(reads), [out_t], small=out_ap.free_size() < SMALL_N)

        def STT(eng, out_t, out_ap, a_t, a_ap, sc, b_t, b_ap, op0, op1, reads=()):
            S.op(eng, lambda e: e.scalar_tensor_tensor(out=out_ap, in0=a_ap, scalar=sc, in1=b_ap, op0=op0, op1=op1),
                 [a_t, b_t] + list(reads), [out_t], small=out_ap.free_size() < SMALL_N)

        def CP(eng, out_t, out_ap, in_t, in_ap):
            if eng == "act":
                S.op("act", lambda e: e.copy(out=out_ap, in_=in_ap), [in_t], [out_t], small=out_ap.free_size() < SMALL_N)
            else:
                S.op(eng, lambda e: e.tensor_copy(out=out_ap, in_=in_ap), [in_t], [out_t], small=out_ap.free_size() < SMALL_N)

        def MSET(eng, t, ap, val):
            S.op(eng, lambda e: e.memset(ap, val), [], [t], small=ap.free_size() < SMALL_N)

        def RSTD(t, ap, n_is_one_col=True):
            S.op("dve", lambda e: e.reciprocal(out=ap, in_=ap), [t], [t])
            ACTF(t, ap, t, ap, ACT.Sqrt)

        class Pool:
            def __init__(self, name, shape, dt, n, stack):
                self.tiles = [S.sb(f"{name}{i}", shape, dt, stack) for i in range(n)]
                self.i = 0

            def get(self):
                t = self.tiles[self.i % len(self.tiles)]
                self.i += 1
                return t

        def run_units(units):
            nxt = units[0][0]() if units[0][0] is not None else None
            for i, (ld, cp) in enumerate(units):
                cur, nxt = nxt, None
                if i + 1 < len(units) and units[i + 1][0] is not None:
                    nxt = units[i + 1][0]()
                cp(cur)

        def wcols(ap2d, c0, n):
            return ap2d.rearrange("(kc p) n -> p kc n", p=P)[:, :, c0:c0 + n]

        DR = lambda h, name: S.dram(h, name)
        dbg_d = DR(dbg_h, "dbg") if dump else None

        def DUMP(slot, t, ap, ncols):
            if dump:
                dump_toks.append(S.dma("pool", dbg_d, dbg_h[:, slot, 0:ncols], t, ap, sem_t=t))
        x_d = DR(x_h, "x")
        pos_d = DR(pos_h, "pos")
        wts_d = DR(w_in_h, "weights")
        modd_d = DR(modd_h, "modd")
        xo_d = [[DR(out_h, f"xo{s}_{j}") for j in range(NT)] for s in range(NSEQ)]

        cst = S.sb("cst", [P, NCST], F32)
        S.dma("sp", cst, cst[:], wts_d, cst_h[:, :])
        ident = (cst, cst[:, CS_ID:CS_ID + P])
        ps = [S.ps(f"ps{i}", [P, 512]) for i in range(8)]
        blk64 = S.sb("blk64", [P, P], BF16)
        MSET("dve", blk64, blk64[:], 0.0)
        MSET("dve", blk64, blk64[0:64, 0:64], 1.0 / 64)
        MSET("dve", blk64, blk64[64:128, 64:128], 1.0 / 64)
        onesln = S.sb("onesln", [P, P], F32)
        MSET("dve", onesln, onesln[:], 1.0 / 512)

        with ExitStack() as st:
          if dbg != "pro0":
              cT = S.sb("cT", [P, KC, NSEQ], F32, st)
              scT = S.sb("scT", [P, KC, NSEQ], BF16, st)
              S.dma("sp", cT, cT[:], wts_d, cT_h[:, :, :])
              ACTF(scT, scT[:], cT, cT[:], ACT.Silu)
              modrow = S.sb("modrow", [NSEQ, 6 * D], F32, st)
              brow2 = S.sb("brow2", [NSEQ, 6 * D], F32, st)
              grow = S.sb("grow", [NSEQ, 2, D], F32, st)
              wpool = Pool("wada", [P, KC, 512], BF16, 2, st)
              for l in range(n_layers):
                  S.dma("sp", brow2, brow2[:], wts_d, b_ada_h[l:l + 1, :].partition_broadcast(NSEQ))
                  S.dma("sp", grow, grow[:, 0, :], wts_d, g_mix_h[l:l + 1, :].partition_broadcast(NSEQ))
                  S.dma("sp", grow, grow[:, 1, :], wts_d, g_ffn_h[l:l + 1, :].partition_broadcast(NSEQ))
                  if dbg == "pro1":
                      break
                  for cb in range(12):
                      wb = wpool.get()
                      S.dma("pool", wb, wb[:], wts_d, wcols(w_ada_h[l], cb * 512, 512))
                      pt = ps[cb % 2]
                      for kc in range(KC):
                          MM(pt, pt[0:NSEQ, :], scT, scT[:, kc, :], wb, wb[:, kc, :], kc == 0, kc == KC - 1)
                      TT("dve", modrow, modrow[:, cb * 512:(cb + 1) * 512], pt, pt[0:NSEQ, :],
                         brow2, brow2[:, cb * 512:(cb + 1) * 512], ALU.add)
                  if dbg == "pro2":
                      break
                  for i, sl in ((0, 1), (1, 4)):
                      TS("dve", modrow, modrow[:, sl * D:(sl + 1) * D], modrow, modrow[:, sl * D:(sl + 1) * D], 1.0, None, ALU.add)
                      TT("dve", modrow, modrow[:, sl * D:(sl + 1) * D], modrow, modrow[:, sl * D:(sl + 1) * D],
                         grow, grow[:, i, :], ALU.mult)
                  if dbg == "pro3":
                      break
                  S.dma("sp", modd_d, modd_h[l], modrow, modrow[:], sem_t=modrow)
        S.barrier()

        class _Stop(Exception):
            pass

        def load_mod_bc(st, l, s, idx, name):
            t = S.sb(name, [P, D], F32, st)
            S.dma("sp", t, t[:], modd_d, modd_h[l, s:s + 1, idx * D:(idx + 1) * D].partition_broadcast(P))
            return t

        def norm_and_transpose(st, l, s, first_layer_input, gmod, shift, hT, h2f_cb=None):
            xs_pool = Pool("xs", [P, D], F32, 3, st)
            hf_pool = Pool("hf", [P, D], F32, 3, st)
            junk = S.sb("junk", [P, D], BF16, st)
            ssq = Pool("ssq", [P, 8], F32, 3, st)
            def phase_a(j):
                xs = xs_pool.get()
                if first_layer_input:
                    S.dma("sp", xs, xs[:], x_d, x_h[s, j * P:(j + 1) * P, :])
                else:
                    S.dma("sp", xs, xs[:], xo_d[s][j], out_h[s, j * P:(j + 1) * P, :])
                ss = ssq.get()
                MSET("dve", ss, ss[:], 0.0)
                ACTF(junk, junk[:], xs, xs[:], ACT.Square, accum=(ss, ss[:, 0:1]))
                TS("dve", ss, ss[:, 0:1], ss, ss[:, 0:1], 1.0 / D, EPS, ALU.mult, ALU.add)
                S.op("dve", lambda e, ss=ss: e.reciprocal(out=ss[:, 0:1], in_=ss[:, 0:1]), [ss], [ss])
                ACTF(ss, ss[:, 0:1], ss, ss[:, 0:1], ACT.Sqrt)
                hf = hf_pool.get()
                STT("dve", hf, hf[:], xs, xs[:], ss[:, 0:1], gmod, gmod[:], ALU.mult, ALU.mult, reads=[ss])
                TT("pool", hf, hf[:], hf, hf[:], shift, shift[:], ALU.add)
                return hf

            def phase_b(j, hf):
                q, jj = divmod(j, 4)
                pa, pb = ps[(2 * j) % 4], ps[(2 * j) % 4 + 1]
                for kc in range(KC):
                    pt = pa if kc < 4 else pb
                    TR(pt, pt[:, (kc % 4) * P:(kc % 4 + 1) * P], hf, hf[:, kc * P:(kc + 1) * P], *ident)
                CP("act", hT[q], hT[q][:, 0:4, jj * P:(jj + 1) * P], pa, pa[:].rearrange("p (k t) -> p k t", k=4))
                CP("act", hT[q], hT[q][:, 4:8, jj * P:(jj + 1) * P], pb, pb[:].rearrange("p (k t) -> p k t", k=4))
                if h2f_cb is not None:
                    h2f_cb(j, pa, pb)

            hf_n = phase_a(0)
            for j in range(NT):
                hf_c = hf_n
                if j + 1 < NT:
                    hf_n = phase_a(j + 1)
                phase_b(j, hf_c)

        def mixer(s, l):
            first = (l == 0)
            w_in = w_in_h[l]
            with ExitStack() as st:
                hT = [S.sb(f"hT{q}", [P, KC, 512], BF16, st) for q in range(NQ)]
                oT = {k: S.sb(f"oT_{k}", [P, 4, SEQ], BF16, st) for k in ("ret", "conv", "att")}
                pp = S.sb("pp", [P, NPP], F32, st)
                S.dma("sp", pp, pp[:], wts_d, pp_h[l])
                with ExitStack() as st1:
                    gmod = load_mod_bc(st1, l, s, 1, "gmod1")
                    shift = load_mod_bc(st1, l, s, 0, "shift1")
                    norm_and_transpose(st1, l, s, first, gmod, shift, hT)
                DUMP(0, hT[0], hT[0][:].rearrange("p k t -> p (k t)"), 4096)
                S.barrier()
                if dbg == "s1":
                    return "stop"
                fpool = Pool("fblk", [P, KC, P], BF16, 4, st)

                def fproj(c0, q, pt):
                    raise NotImplementedError

                def load_f(c0, swap=False):
                    wb = fpool.get()
                    if not swap:
                        S.dma("pool", wb, wb[:], wts_d, wcols(w_in, c0, P))
                    else:
                        S.dma("pool", wb, wb[:, :, 0:64], wts_d, wcols(w_in, c0 + 64, 64))
                        S.dma("pool", wb, wb[:, :, 64:128], wts_d, wcols(w_in, c0, 64))
                    return wb

                def f_mm(wb, q, pt):
                    for kc in range(KC):
                        MM(pt, pt[:], wb, wb[:, kc, :], hT[q], hT[q][:, kc, :], kc == 0, kc == KC - 1)

                with ExitStack() as st2:
                    cosT = S.sb("cosT", [P, SEQ], BF16, st2)
                    sinT = S.sb("sinT", [P, SEQ], BF16, st2)
                    with ExitStack() as st3:
                        posi = S.sb("posi", [P, SEQ], I32, st3)
                        u = S.sb("rope_u", [P, SEQ], F32, st3)
                        f = S.sb("rope_f", [P, SEQ], F32, st3)
                        ki = S.sb("rope_ki", [P, SEQ], I32, st3)
                        S.dma("sp", posi, posi[:], pos_d, pos_h[s:s + 1, :].partition_broadcast(P))
                        CP("dve", u, u[:], posi, posi[:])
                        TS("dve", u, u[:], u, u[:], cst[:, CS_INV:CS_INV + 1], 1.0 / (2.0 * math.pi), ALU.mult, ALU.mult, reads=[cst])
                        for tab, off, sgn in ((sinT, 0.0, True), (cosT, 0.25, False)):
                            if off != 0.0:
                                TS("dve", f, f[:], u, u[:], off, None, ALU.add)
                                src = f
                            else:
                                src = u
                            CP("dve", ki, ki[:], src, src[:])
                            fk = S.sb("rope_fk" + ("s" if sgn else "c"), [P, SEQ], F32, st3)
                            CP("dve", fk, fk[:], ki, ki[:])
                            TT("dve", fk, fk[:], src, src[:], fk, fk[:], ALU.subtract)
                            TS("dve", fk, fk[:], fk, fk[:], 0.49999, -0.49999, ALU.min, ALU.max)
                            if sgn:
                                ACTF(fk, fk[:], fk, fk[:], ACT.Sin, scale=2.0 * math.pi)
                                TS("dve", tab, tab[:], fk, fk[:], cst[:, CS_SGN:CS_SGN + 1], None, ALU.mult, reads=[cst])
                            else:
                                ACTF(tab, tab[:], fk, fk[:], ACT.Sin, scale=2.0 * math.pi)
                    S.barrier()
                    state_f = S.sb("state_f", [P, 4, P], F32, st2)
                    state_b = S.sb("state_b", [P, 4, P], BF16, st2)
                    gn_bc = S.sb("gn_bc", [P, 512], F32, st2)
                    S.dma("sp", gn_bc, gn_bc[:], wts_d, ret_gn_h[l:l + 1, :].partition_broadcast(P))
                    HS = SEQ // 2
                    qT = [S.sb("r_qT0", [P, 4, HS], BF16, st2), TView(oT["conv"], oT["conv"][:, 0:2, :].rearrange("p a (b t) -> p (a b) t", b=2))]
                    kT = [S.sb("r_kT0", [P, 4, HS], BF16, st2), TView(oT["conv"], oT["conv"][:, 2:4, :].rearrange("p a (b t) -> p (a b) t", b=2))]
                    v_tok = [S.sb("r_v0", [P, 8, 512], BF16, st2), TView(oT["att"], oT["att"][:, 0:2, :].rearrange("p a (b t) -> p (a b) t", b=4))]
                    gs = [S.sb("r_gs0", [P, 8, 512], BF16, st2), TView(oT["att"], oT["att"][:, 2:4, :].rearrange("p a (b t) -> p (a b) t", b=4))]
                    tpool = Pool("tblk", [P, KC, 512], BF16, 2, st2)
                    t1p = Pool("r_t1", [P, 512], F32, 2, st2)
                    t2p = Pool("r_t2", [P, 512], F32, 2, st2)
                    sTm_p = Pool("r_sTm", [P, 512], BF16, 2, st2)
                    qd_p = Pool("r_qd", [P, 4, P], BF16, 2, st2)
                    kt_p = Pool("r_kt", [P, 512], BF16, 2, st2)
                    on_p = Pool("r_on", [P, 512], F32, 2, st2)
                    og_p = Pool("r_og", [P, 512], F32, 2, st2)
                    st_p = Pool("r_stats", [P, 16], F32, 2, st2)
                    sgt_p = Pool("r_sg", [P, 512], F32, 2, st2)
                    junk = S.sb("r_junk", [P, P], BF16, st2)
                    def r_units(seg):
                        us = []
                        for which, cbase, dst in (("q", C_RQ, qT[seg]), ("k", C_RK, kT[seg])):
                            for h in range(4):
                                def ld(cbase=cbase, h=h):
                                    return (load_f(cbase + h * P), load_f(cbase + h * P, swap=True))

                                def cp(w, dst=dst, h=h, seg=seg):
                                    wb, wsw = w
                                    for qq in range(2):
                                        q = seg * 2 + qq
                                        pa, pb = ps[2 * (qq % 2)], ps[2 * (qq % 2) + 1]
                                        f_mm(wb, q, pa)
                                        f_mm(wsw, q, pb)
                                        t1, t2 = t1p.get(), t2p.get()
                                        TT("dve", t1, t1[:], pa, pa[:], cosT, cosT[:, q * 512:(q + 1) * 512], ALU.mult)
                                        TT("dve", t2, t2[:], pb, pb[:], sinT, sinT[:, q * 512:(q + 1) * 512], ALU.mult)
                                        TT("pool", dst, dst[:, h, qq * 512:(qq + 1) * 512], t1, t1[:], t2, t2[:], ALU.add)
                                us.append((ld, cp))
                        for which, cbase in (("v", C_RV), ("g", C_RG)):
                            def ld(cbase=cbase):
                                wb = tpool.get()
                                S.dma("pool", wb, wb[:], wts_d, wcols(w_in, cbase, 512))
                                return wb

                            def cp(wb, which=which, seg=seg):
                                for jl in range(8):
                                    j = seg * 8 + jl
                                    q, jj = divmod(j, 4)
                                    pt = ps[4 + jl % 2]
                                    for kc in range(KC):
                                        MM(pt, pt[:], hT[q], hT[q][:, kc, jj * P:(jj + 1) * P], wb, wb[:, kc, :], kc == 0, kc == KC - 1)
                                    if which == "v":
                                        CP("act", v_tok[seg], v_tok[seg][:, jl, :], pt, pt[:])
                                    else:
                                        sg = sgt_p.get()
                                        ACTF(sg, sg[:], pt, pt[:], ACT.Silu)
                                        TT("pool", gs[seg], gs[seg][:, jl, :], sg, sg[:], gn_bc, gn_bc[:], ALU.mult)
                            us.append((ld, cp))
                        return us

                    def r_rec(seg, jl):
                        qT_, kT_, v_tok_, gs_ = qT[seg], kT[seg], v_tok[seg], gs[seg]
                        if True:
                            j = seg * 8 + jl
                            tsl = slice(jl * P, (jl + 1) * P)
                            pS, pO, pK, pT_ = ps[0 + (jl % 2)], ps[2 + (jl % 2)], ps[6], ps[7]
                            for h in range(4):
                                MM(pS, pS[:, h * P:(h + 1) * P], kT_, kT_[:, h, tsl], qT_, qT_[:, h, tsl])
                            sTm = sTm_p.get()
                            TT("dve", sTm, sTm[:], pS, pS[:], cst, cst[:, CS_MASK:CS_MASK + 512], ALU.mult)
                            qd = qd_p.get()
                            TT("pool", qd, qd[:], qT_, qT_[:, :, tsl], cst,
                               cst[:, CS_QDEC:CS_QDEC + 512].rearrange("p (h c) -> p h c", h=4), ALU.mult)
                            pTb = pT_[:, 0:256].bitcast(BF16)
                            for h in range(4):
                                S.op("pe", lambda e, h=h, tsl=tsl, pTb=pTb: e.transpose(
                                    out=pTb[:, h * P:(h + 1) * P], in_=kT_[:, h, tsl], identity=identb[:]),
                                    [kT_, identb], [pT_])
                            kt = kt_p.get()
                            for h in range(4):
                                TS("dve", kt, kt[:, h * P:(h + 1) * P], pT_, pTb[:, h * P:(h + 1) * P],
                                   cst[:, CS_KDEC + h:CS_KDEC + h + 1], None, ALU.mult, reads=[cst])
                            for h in range(4):
                                hs = slice(h * P, (h + 1) * P)
                                MM(pO, pO[:, hs], sTm, sTm[:, hs], v_tok_, v_tok_[:, jl, hs], True, j == 0)
                                if j > 0:
                                    MM(pO, pO[:, hs], qd, qd[:, h, :], state_b, state_b[:, h, :], False, True)
                            if j < NT - 1:
                                for h in range(4):
                                    hs = slice(h * P, (h + 1) * P)
                                    MM(pK, pK[:, hs], kt, kt[:, hs], v_tok_, v_tok_[:, jl, hs])
                                if j == 0:
                                    CP("dve", state_f, state_f[:], pK, pK[:].rearrange("p (h e) -> p h e", h=4))
                                else:
                                    for h in range(4):
                                        STT("dve", state_f, state_f[:, h, :], state_f, state_f[:, h, :], RET_CD[h],
                                            pK, pK[:, h * P:(h + 1) * P], ALU.mult, ALU.add)
                                CP("act", state_b, state_b[:], state_f, state_f[:])
                            stt = st_p.get()
                            MSET("dve", stt, stt[:], 0.0)
                            for h in range(4):
                                hs = slice(h * P, (h + 1) * P)
                                ACTF(junk, junk[:], pO, pO[:, hs], ACT.Copy, accum=(stt, stt[:, h:h + 1]))
                                ACTF(junk, junk[:], pO, pO[:, hs], ACT.Square, accum=(stt, stt[:, 4 + h:5 + h]))
                            TS("dve", stt, stt[:, 0:8], stt, stt[:, 0:8], 1.0 / P, None, ALU.mult)
                            TT("dve", stt, stt[:, 8:12], stt, stt[:, 0:4], stt, stt[:, 0:4], ALU.mult)
                            TT("dve", stt, stt[:, 12:16], stt, stt[:, 4:8], stt, stt[:, 8:12], ALU.subtract)
                            TS("dve", stt, stt[:, 12:16], stt, stt[:, 12:16], EPS, None, ALU.add)
                            RSTD(stt, stt[:, 12:16])
                            on = on_p.get()
                            for h in range(4):
                                hs = slice(h * P, (h + 1) * P)
                                TS("dve", on, on[:, hs], pO, pO[:, hs], stt[:, h:h + 1], stt[:, 12 + h:13 + h],
                                   ALU.subtract, ALU.mult, reads=[stt])
                            og = og_p.get()
                            TT("pool", og, og[:], on, on[:], gs_, gs_[:, jl, :], ALU.mult)
                            pX = ps[4 + jl % 2]
                            for h in range(4):
                                TR(pX, pX[:, h * P:(h + 1) * P], og, og[:, h * P:(h + 1) * P], *ident)
                            CP("act", oT["ret"], oT["ret"][:, :, j * P:(j + 1) * P], pX, pX[:].rearrange("p (h c) -> p h c", h=4))
                    u1 = r_units(1)
                    mixed = []
                    for i in range(max(8, len(u1))):
                        if i < 8:
                            mixed.append((None, lambda _w, i=i: r_rec(0, i)))
                        if i < len(u1):
                            mixed.append(u1[i])
                    run_units(r_units(0) + mixed + [(None, lambda _w, i=i: r_rec(1, i)) for i in range(8)])
                S.barrier()
                if dbg == "ret":
                    return "stop"
                with ExitStack() as st2:
                    zT = S.sb("c_zT", [P, 4, 30 + SEQ], BF16, st2)
                    acc = S.sb("c_acc", [P, 4, SEQ], F32, st2)
                    sgp = Pool("c_sg", [P, 512], F32, 2, st2)
                    dgp = Pool("c_dg", [P, P], BF16, 6, st2)
                    MSET("dve", zT, zT[:, :, 0:30], 0.0)
                    def c_unit(cc):
                        def ld():
                            return (load_f(C_CU + cc * P), load_f(C_CU + 512 + cc * P))

                        def cp(w):
                            wa, wb = w
                            for q in range(NQ):
                                pa, pb = ps[2 * (q % 2)], ps[2 * (q % 2) + 1]
                                f_mm(wa, q, pa)
                                f_mm(wb, q, pb)
                                sg = sgp.get()
                                ACTF(sg, sg[:], pb, pb[:], ACT.Sigmoid)
                                TT("dve", zT, zT[:, cc, 30 + q * 512:30 + (q + 1) * 512], pa, pa[:], sg, sg[:], ALU.mult)
                            for k in range(31):
                                dg = dgp.get()
                                wk = pp[:, PP_CW + cc * 31 + k:PP_CW + cc * 31 + k + 1]
                                TS("pool" if k % 2 else "dve", dg, dg[:], identb, identb[:], wk, None, ALU.mult, reads=[pp])
                                for q in range(NQ):
                                    pc = ps[4 + q]
                                    MM(pc, pc[:], dg, dg[:], zT, zT[:, cc, q * 512 + k:q * 512 + k + 512], k == 0, k == 30)
                            for q in range(NQ):
                                pc = ps[4 + q]
                                TS("dve", acc, acc[:, cc, q * 512:(q + 1) * 512], pc, pc[:], pp[:, PP_CB + cc:PP_CB + cc + 1], None,
                                   ALU.add, reads=[pp])
                        return (ld, cp)

                    run_units([c_unit(cc) for cc in range(4)])
                    sqp = Pool("c_sq", [P, 512], F32, 2, st2)
                    m2p = Pool("c_m2", [P, 512], F32, 2, st2)
                    rsp = Pool("c_rs", [P, 512], F32, 2, st2)
                    tp = Pool("c_t", [P, 512], F32, 2, st2)
                    for q in range(NQ):
                        qs = slice(q * 512, (q + 1) * 512)
                        pm, pe2 = ps[4 + 2 * (q % 2)], ps[5 + 2 * (q % 2)]
                        for cc in range(4):
                            MM(pm, pm[:], onesln, onesln[:], acc, acc[:, cc, qs], cc == 0, cc == 3)
                        for cc in range(4):
                            sq = sqp.get()
                            ACTF(sq, sq[:], acc, acc[:, cc, qs], ACT.Square)
                            MM(pe2, pe2[:], onesln, onesln[:], sq, sq[:], cc == 0, cc == 3)
                        m2 = m2p.get()
                        ACTF(m2, m2[:], pm, pm[:], ACT.Square)
                        rs = rsp.get()
                        TT("dve", rs, rs[:], pe2, pe2[:], m2, m2[:], ALU.subtract)
                        TS("dve", rs, rs[:], rs, rs[:], EPS, None, ALU.add)
                        RSTD(rs, rs[:])
                        for cc in range(4):
                            t = tp.get()
                            TT("dve", t, t[:], acc, acc[:, cc, qs], pm, pm[:], ALU.subtract)
                            TT("pool", t, t[:], t, t[:], rs, rs[:], ALU.mult)
                            ACTF(oT["conv"], oT["conv"][:, cc, qs], t, t[:], ACT.Silu,
                                 bias=pp[:, PP_LB + cc:PP_LB + cc + 1], scale=pp[:, PP_LG + cc:PP_LG + cc + 1], reads=[pp])
                S.barrier()
                if dbg == "conv":
                    return "stop"
                with ExitStack() as st2:
                    aqM = [S.sb(f"a_qM{i}", [P, 4, SEQ], BF16, st2) for i in range(2)]
                    MSET("dve", aqM[0], aqM[0][:], 0.0)
                    MSET("dve", aqM[1], aqM[1][:], 0.0)
                    akT = S.sb("a_kT", [P, 4, SEQ], BF16, st2)
                    vaug = S.sb("a_v", [P, NT, 8, 66], BF16, st2)
                    biasT = S.sb("a_bias", [P, 5, 8, P], F32, st2)
                    S.dma("sp", biasT, biasT[:], wts_d, biasx_h[l])
                    MSET("dve", biasT, biasT[0:64, 0, :, 64:128], NEG)
                    MSET("dve", biasT, biasT[64:128, 4, :, 0:64], NEG)
                    MSET("dve", vaug, vaug[:], 1.0)
                    with ExitStack() as st3:
                        sqp = Pool("a_sq", [P, 512], BF16, 2, st3)
                        rsp = Pool("a_rs", [P, 512], F32, 2, st3)
                        def a_unit(dst, cbase, gcol, c):
                            def ld():
                                return load_f(cbase + c * P)

                            def cp(wb):
                                for q in range(NQ):
                                    qs = slice(q * 512, (q + 1) * 512)
                                    pa, pb = ps[2 * (q % 2)], ps[2 * (q % 2) + 1]
                                    f_mm(wb, q, pa)
                                    sq = sqp.get()
                                    ACTF(sq, sq[:], pa, pa[:], ACT.Square)
                                    MM(pb, pb[:], blk64, blk64[:], sq, sq[:])
                                    rs = rsp.get()
                                    TS("dve", rs, rs[:], pb, pb[:], EPS, None, ALU.add)
                                    RSTD(rs, rs[:])
                                    if dst is None:
                                        for i in range(2):
                                            hp = slice(64 * i, 64 * i + 64)
                                            STT("dve", aqM[i], aqM[i][hp, c, qs], pa, pa[hp, :], pp[hp, gcol:gcol + 1], rs, rs[hp, :],
                                                ALU.mult, ALU.mult, reads=[pp])
                                    else:
                                        STT("dve", dst, dst[:, c, qs], pa, pa[:], pp[:, gcol:gcol + 1], rs, rs[:],
                                            ALU.mult, ALU.mult, reads=[pp])
                            return (ld, cp)

                        def av_ld():
                            wb = tpool.get()
                            S.dma("pool", wb, wb[:], wts_d, wcols(w_in, C_AV, 512))
                            return wb

                        def av_cp(wb):
                            for j in range(NT if dbg != "att0" else 0):
                                q, jj = divmod(j, 4)
                                pt = ps[4 + j % 2]
                                for kc in range(KC):
                                    MM(pt, pt[:], hT[q], hT[q][:, kc, jj * P:(jj + 1) * P], wb, wb[:, kc, :], kc == 0, kc == KC - 1)
                                CP("act", vaug, vaug[:, j, :, 0:64], pt, pt[:].rearrange("p (h d) -> p h d", h=8))

                        tpool = Pool("tblk", [P, KC, 512], BF16, 1, st3)
                        run_units([a_unit(dst, cbase, gcol, c) for dst, cbase, gcol in ((None, C_AQ, PP_QG), (akT, C_AK, PP_KG))
                                   for c in range(4)] + [(av_ld, av_cp)])
                    S.barrier()
                    ep = Pool("a_e", [P, 512], F32, 3, st2)
                    pTp = Pool("a_pT", [P, 5, 512], BF16, 3, st2)
                    recp = Pool("a_rec", [P, 8], F32, 2, st2)
                    oap = Pool("a_o", [P, 512], F32, 2, st2)
                    n_att = NT if dbg not in ("att0", "att1") else 0

                    def a_scores(j, g):
                        tsl = slice(j * P, (j + 1) * P)
                        kbs = [kb for kb in range(5) if j - 4 + kb >= 0]
                        pTt = pTp.get()
                        for kb in kbs:
                            jk = j - 4 + kb
                            ksl = slice(jk * P, (jk + 1) * P)
                            pq = ps[kb % 2 + 2 * g]
                            for hh in range(4):
                                h = g * 4 + hh
                                c = h // 2
                                MM(pq, pq[:, hh * P:(hh + 1) * P], akT, akT[:, c, ksl], aqM[h % 2], aqM[h % 2][:, c, tsl])
                            e_ = ep.get()
                            STT("dve", e_, e_[:].rearrange("p (h q) -> p h q", h=4), pq,
                                pq[:].rearrange("p (h q) -> p h q", h=4), 0.125,
                                biasT, biasT[:, kb, g * 4:(g + 1) * 4, :], ALU.mult, ALU.add)
                            ACTF(pTt, pTt[:, kb, :], e_, e_[:], ACT.Exp)
                        return pTt, kbs

                    def a_pv(j, g, pTt, kbs, oa, rec):
                        po = ps[4 + g]
                        for hh in range(4):
                            h = g * 4 + hh
                            for i, kb in enumerate(kbs):
                                jk = j - 4 + kb
                                MM(po, po[:, hh * 66:(hh + 1) * 66], pTt, pTt[:, kb, hh * P:(hh + 1) * P],
                                   vaug, vaug[:, jk, h, :], i == 0, i == len(kbs) - 1)
                        pov = po[:, 0:264].rearrange("p (h d) -> p h d", h=4)
                        S.op("dve", lambda e, rec=rec, pov=pov, g=g: e.reciprocal(
                            out=rec[:, g * 4:(g + 1) * 4].unsqueeze(2), in_=pov[:, :, 64:65]), [po], [rec])
                        for hh in range(4):
                            h = g * 4 + hh
                            TS("dve", oa, oa[:, h * 64:(h + 1) * 64], po, po[:, hh * 66:hh * 66 + 64],
                               rec[:, h:h + 1], None, ALU.mult, reads=[rec])

                    def a_fin(j, oa):
                        tsl = slice(j * P, (j + 1) * P)
                        pX = ps[6 + j % 2]
                        for c in range(4):
                            TR(pX, pX[:, c * P:(c + 1) * P], oa, oa[:, c * P:(c + 1) * P], *ident)
                        CP("act", oT["att"], oT["att"][:, :, tsl], pX, pX[:].rearrange("p (c t) -> p c t", c=4))

                    groups = [(j, g) for j in range(n_att) for g in range(2)]
                    nxt = a_scores(*groups[0]) if groups else None
                    oa = rec = None
                    for n, (j, g) in enumerate(groups):
                        cur = nxt
                        if n + 1 < len(groups):
                            nxt = a_scores(*groups[n + 1])
                        if g == 0:
                            oa, rec = oap.get(), recp.get()
                        a_pv(j, g, cur[0], cur[1], oa, rec)
                        if g == 1:
                            a_fin(j, oa)
                for i_, k_ in enumerate(("ret", "conv", "att")):
                    DUMP(1 + i_, oT[k_], oT[k_][:].rearrange("p k t -> p (k t)"), 8192)
                S.barrier()
                if dbg and dbg.startswith("att"):
                    return "stop"
                with ExitStack() as st2:
                    mT = [S.sb(f"mT{q}", [P, KC, 512], BF16, st2) for q in range(NQ)]
                    glp = Pool("s4_gl", [P, KC, 3, P], BF16, 2, st2)
                    wop = Pool("s4_wo", [P, 4, 3, P], BF16, 2, st2)
                    gtp = Pool("s4_g", [P, 512], F32, 3, st2)
                    tmp = Pool("s4_t", [P, 512], F32, 2, st2)
                    macc = Pool("s4_m", [P, 512], F32, 2, st2)
                    outs_w = (w_ro_h[l], w_co_h[l], w_ao_h[l])
                    keys = ("ret", "conv", "att")
                    def s4_unit(c):
                        def ld():
                            gw = glp.get()
                            ow = wop.get()
                            for b in range(3):
                                S.dma("pool", gw, gw[:, :, b, :], wts_d, wcols(w_in, C_GL + b * D + c * P, P))
                                S.dma("pool", ow, ow[:, :, b, :], wts_d, wcols(outs_w[b], c * P, P))
                            return (gw, ow)

                        def cp(w):
                            gw, ow = w
                            for q in range(NQ):
                                qs = slice(q * 512, (q + 1) * 512)
                                m = macc.get()
                                for b in range(3):
                                    pg, py = ps[2 * (b % 2)], ps[2 * (b % 2) + 1]
                                    for kc in range(KC):
                                        MM(pg, pg[:], gw, gw[:, kc, b, :], hT[q], hT[q][:, kc, :], kc == 0, kc == KC - 1)
                                    for kc in range(4):
                                        MM(py, py[:], ow, ow[:, kc, b, :], oT[keys[b]], oT[keys[b]][:, kc, qs], kc == 0, kc == 3)
                                    gt = gtp.get()
                                    ACTF(gt, gt[:], pg, pg[:], ACT.Sigmoid,
                                         bias=pp[:, PP_BG + b * 8 + c:PP_BG + b * 8 + c + 1], reads=[pp])
                                    if b == 0:
                                        TT("dve", m, m[:], py, py[:], gt, gt[:], ALU.mult)
                                    elif b == 1:
                                        t = tmp.get()
                                        TT("dve", t, t[:], py, py[:], gt, gt[:], ALU.mult)
                                        TT("pool", m, m[:], m, m[:], t, t[:], ALU.add)
                                    else:
                                        t = tmp.get()
                                        TT("dve", t, t[:], py, py[:], gt, gt[:], ALU.mult)
                                        TT("pool", mT[q], mT[q][:, c, :], m, m[:], t, t[:], ALU.add)
                        return (ld, cp)

                    wo = S.sb("s4_wout", [P, KC, D], BF16, st2)
                    gate1 = load_mod_bc(st2, l, s, 2, "gate1")
                    xs_pool = Pool("s4_xs", [P, D], F32, 2, st2)

                    def wo_ld():
                        S.dma("pool", wo, wo[:], wts_d, wcols(w_out_h[l], 0, D))
                        return wo

                    def wo_cp(_w):
                        DUMP(4, mT[0], mT[0][:].rearrange("p k t -> p (k t)"), 4096)
                        s4_out()

                    def s4_out():
                        for j in range(NT):
                            q, jj = divmod(j, 4)
                            xs = xs_pool.get()
                            if first:
                                S.dma("sp", xs, xs[:], x_d, x_h[s, j * P:(j + 1) * P, :])
                            else:
                                S.dma("sp", xs, xs[:], xo_d[s][j], out_h[s, j * P:(j + 1) * P, :])
                            for half in range(2):
                                pt = ps[4 + (2 * j + half) % 4]
                                hsl = slice(half * 512, (half + 1) * 512)
                                for kc in range(KC):
                                    MM(pt, pt[:], mT[q], mT[q][:, kc, jj * P:(jj + 1) * P], wo, wo[:, kc, hsl], kc == 0, kc == KC - 1)
                                t = tmp.get()
                                TT("dve", t, t[:], pt, pt[:], gate1, gate1[:, hsl], ALU.mult)
                                TT("pool", xs, xs[:, hsl], xs, xs[:, hsl], t, t[:], ALU.add)
                            S.dma("sp", xo_d[s][j], out_h[s, j * P:(j + 1) * P, :], xs, xs[:], sem_t=xs)
                    run_units([s4_unit(c) for c in range(KC)] + [(wo_ld, wo_cp)])
                S.barrier()
            S.barrier()

        def moe(s, l):
            with ExitStack() as st:
                hT = [S.sb(f"hT{q}", [P, KC, 512], BF16, st) for q in range(NQ)]
                acc = S.sb("m_acc", [P, NT, D], F32, st)
                wge = S.sb("m_wge", [P, NT, 32], F32, st)
                upp = Pool("m_wup", [P, KC, 512], BF16, 2, st)
                dnp = Pool("m_wdn", [P, 2, D], BF16, 2, st)

                def e_ld(ge):
                    g, e_ = divmod(ge, NE)
                    wu = upp.get()
                    wd = dnp.get()
                    S.dma("pool", wu, wu[:], wts_d, wcols(w_up_h[l, g, e_], 0, 512))
                    S.dma("pool", wd, wd[:], wts_d, w_dn_h[l, g, e_].rearrange("(fc p) n -> p fc n", p=P))
                    return (wu, wd)

                e_nxt = e_ld(0) if n_experts > 0 else None
                with ExitStack() as st1:
                    gmod = load_mod_bc(st1, l, s, 4, "gmod2")
                    shift = load_mod_bc(st1, l, s, 3, "shift2")
                    wr = S.sb("m_wr", [P, KC, 36], F32, st1)
                    S.dma("sp", wr, wr[:], wts_d, wr_h[l])
                    brow = S.sb("m_brow", [P, 36], F32, st1)
                    S.dma("sp", brow, brow[:], wts_d, brow_h[l:l + 1, :].partition_broadcast(P))
                    whi = S.sb("m_whi", [P, KC, 36], BF16, st1)
                    wlo = S.sb("m_wlo", [P, KC, 36], BF16, st1)
                    CP("dve", whi, whi[:], wr, wr[:])
                    TT("dve", wlo, wlo[:], wr, wr[:], whi, whi[:], ALU.subtract)
                    h2p = Pool("m_h2lo", [P, KC, P], BF16, 2, st1)
                    rp = Pool("m_r", [P, 96], F32, 2, st1)

                    def router(j, pa, pb):
                        q, jj = divmod(j, 4)
                        tsl = slice(jj * P, (jj + 1) * P)
                        h2 = h2p.get()
                        TT("dve", h2, h2[:, 0:4, :], pa, pa[:].rearrange("p (k t) -> p k t", k=4),
                           hT[q], hT[q][:, 0:4, tsl], ALU.subtract)
                        TT("dve", h2, h2[:, 4:8, :], pb, pb[:].rearrange("p (k t) -> p k t", k=4),
                           hT[q], hT[q][:, 4:8, tsl], ALU.subtract)
                        pl = ps[4 + j % 2]
                        for kc in range(KC):
                            MM(pl, pl[:, 0:36], hT[q], hT[q][:, kc, tsl], whi, whi[:, kc, :], kc == 0, False)
                            MM(pl, pl[:, 0:36], hT[q], hT[q][:, kc, tsl], wlo, wlo[:, kc, :], False, False)
                            MM(pl, pl[:, 0:36], h2, h2[:, kc, :], whi, whi[:, kc, :], False, kc == KC - 1)
                        r = rp.get()
                        MSET("dve", r, r[:], 0.0)
                        LG, GMX, OHG, NGM, GS, CH, M1, OH1, CH2, M2, OH2, DD, W1, W2, WE, EX = (
                            slice(0, 36), slice(36, 37), slice(37, 41), slice(41, 42), slice(42, 43), slice(43, 51),
                            slice(51, 52), slice(52, 60), slice(60, 68), slice(68, 69), slice(69, 77), slice(77, 78),
                            slice(78, 79), slice(79, 80), slice(80, 88), slice(88, 92))
                        R = lambda sl: r[:, sl]
                        TT("dve", r, R(LG), pl, pl[:, 0:36], brow, brow[:], ALU.add)
                        if dbg == "m_r1":
                            return
                        S.op("dve", lambda e: e.tensor_reduce(out=R(GMX), in_=r[:, 0:4], axis=AX.X, op=ALU.max), [r], [r])
                        TS("dve", r, R(OHG), r, r[:, 0:4], R(GMX), None, ALU.is_ge)
                        TS("dve", r, R(NGM), r, R(GMX), -1.0, None, ALU.mult)
                        ACTF(r, R(EX), r, r[:, 0:4], ACT.Exp, bias=R(NGM), accum=(r, R(GS)))
                        S.op("dve", lambda e: e.reciprocal(out=R(GS), in_=R(GS)), [r], [r])
                        if dbg == "m_r2":
                            return
                        TS("dve", r, R(CH), r, r[:, 4:12], r[:, 37:38], None, ALU.mult)
                        for g in range(1, 4):
                            STT("dve", r, R(CH), r, r[:, 4 + g * 8:12 + g * 8], r[:, 37 + g:38 + g], r, R(CH), ALU.mult, ALU.add)
                        S.op("dve", lambda e: e.tensor_reduce(out=R(M1), in_=R(CH), axis=AX.X, op=ALU.max), [r], [r])
                        TS("dve", r, R(OH1), r, R(CH), R(M1), None, ALU.is_ge)
                        STT("dve", r, R(CH2), r, R(OH1), NEG, r, R(CH), ALU.mult, ALU.add)
                        S.op("dve", lambda e: e.tensor_reduce(out=R(M2), in_=R(CH2), axis=AX.X, op=ALU.max), [r], [r])
                        TS("dve", r, R(OH2), r, R(CH2), R(M2), None, ALU.is_ge)
                        TT("dve", r, R(DD), r, R(M2), r, R(M1), ALU.subtract)
                        ACTF(r, R(W2), r, R(DD), ACT.Sigmoid)
                        TS("dve", r, R(W1), r, R(W2), -1.0, 1.0, ALU.mult, ALU.add)
                        TT("dve", r, R(W1), r, R(W1), r, R(GS), ALU.mult)
                        TT("dve", r, R(W2), r, R(W2), r, R(GS), ALU.mult)
                        TS("dve", r, R(WE), r, R(OH1), R(W1), None, ALU.mult)
                        STT("dve", r, R(WE), r, R(OH2), R(W2), r, R(WE), ALU.mult, ALU.add)
                        if dbg == "m_r3":
                            return
                        for g in range(4):
                            TS("dve", wge, wge[:, j, g * 8:(g + 1) * 8], r, R(WE), r[:, 37 + g:38 + g], None, ALU.mult, reads=[r])

                    norm_and_transpose(st1, l, s, False, gmod, shift, hT, h2f_cb=(None if dbg == "m_norouter" else router))
                S.barrier()
                with ExitStack() as st2:
                    sap = Pool("m_sa", [P, 512], F32, 2, st2)
                    actp = Pool("m_act", [P, 2, 512], BF16, 2, st2)
                    dcount = 0
                    for ge in range(n_experts):
                        g, e_ = divmod(ge, NE)
                        wu, wd = e_nxt
                        e_nxt = e_ld(ge + 1) if ge + 1 < n_experts else None
                        for q in range(NQ):
                            for fc in range(4):
                                pu = ps[fc]
                                for kc in range(KC):
                                    MM(pu, pu[:], wu, wu[:, kc, fc * P:(fc + 1) * P], hT[q], hT[q][:, kc, :], kc == 0, kc == KC - 1)
                            at = actp.get()
                            for fc in range(2):
                                sa = sap.get()
                                ACTF(sa, sa[:], ps[fc], ps[fc][:], ACT.Silu)
                                TT("dve", at, at[:, fc, :], sa, sa[:], ps[fc + 2], ps[fc + 2][:], ALU.mult)
                            for jj in range(4):
                                j = q * 4 + jj
                                for half in range(2):
                                    pd = ps[4 + dcount % 4]
                                    dcount += 1
                                    hsl = slice(half * 512, (half + 1) * 512)
                                    for fc in range(2):
                                        MM(pd, pd[:], at, at[:, fc, jj * P:(jj + 1) * P], wd, wd[:, fc, hsl], fc == 0, fc == 1)
                                    if ge == 0:
                                        TS("dve", acc, acc[:, j, hsl], pd, pd[:], wge[:, j, ge:ge + 1], None, ALU.mult, reads=[wge])
                                    else:
                                        STT("dve", acc, acc[:, j, hsl], pd, pd[:], wge[:, j, ge:ge + 1], acc, acc[:, j, hsl],
                                            ALU.mult, ALU.add, reads=[wge])
                    gate2 = load_mod_bc(st2, l, s, 5, "gate2")
                    xs_pool = Pool("m_xs", [P, D], F32, 2, st2)
                    toks = []
                    for j in range(NT):
                        xs = xs_pool.get()
                        S.dma("sp", xs, xs[:], xo_d[s][j], out_h[s, j * P:(j + 1) * P, :])
                        TT("pool", acc, acc[:, j, :], acc, acc[:, j, :], gate2, gate2[:], ALU.mult)
                        TT("pool", xs, xs[:], xs, xs[:], acc, acc[:, j, :], ALU.add)
                        toks.append(S.dma("sp", xo_d[s][j], out_h[s, j * P:(j + 1) * P, :], xs, xs[:], sem_t=xs))
                S.barrier()
                return toks

        identb = S.sb("identb", [P, P], BF16)
        CP("dve", identb, identb[:], cst, cst[:, CS_ID:CS_ID + P])

        final = []
        try:
            if dbg and dbg.startswith("pro"):
                raise _Stop()
            for s in range(n_seq):
                for l in range(n_layers):
                    if mixer(s, l) == "stop":
                        raise _Stop()
                    final = moe(s, l) if n_experts > 0 else []
        except _Stop:
            S.barrier()
        S.emit(final_toks=S.dma_toks + final + dump_toks)
    return nc


def _constants():
    cst = np.zeros((P, NCST), np.float32)
    cst[:, CS_ID:CS_ID + P] = np.eye(P, dtype=np.float32)
    m = np.arange(P)[:, None].astype(np.float64)
    c = np.arange(P)[None, :].astype(np.float64)
    for h in range(4):
        gamma = 1.0 - 2.0 ** (-5 - h)
        mask = np.where(c >= m, gamma ** np.maximum(c - m, 0.0), 0.0) * (128.0 ** -0.5)
        cst[:, CS_MASK + h * P:CS_MASK + (h + 1) * P] = mask
        cst[:, CS_QDEC + h * P:CS_QDEC + (h + 1) * P] = (gamma ** (c + 1.0))
        cst[:, CS_KDEC + h] = (gamma ** (127.0 - m[:, 0])) * (128.0 ** -0.5)
    half = 64
    inv = (np.float32(10000.0) ** (-np.arange(half, dtype=np.float32) / np.float32(half))).astype(np.float32)
    cst[:, CS_INV] = np.tile(inv, 2)
    cst[0:64, CS_SGN] = -1.0
    cst[64:128, CS_SGN] = 1.0
    cst[0:64, CS_MLO] = 1.0
    cst[64:128, CS_MHI] = 1.0
    return cst


def _layout_params(inp):
    f32 = np.float32
    pp = np.zeros((L, P, NPP), f32)
    cw = np.asarray(inp["conv_w"], f32)[:, :, 0, :]
    pp[:, :, PP_CW:PP_CW + 124] = cw.reshape(L, 31, 4, P).transpose(0, 3, 2, 1).reshape(L, P, 124)
    pp[:, :, PP_CB:PP_CB + 4] = np.asarray(inp["conv_b"], f32).reshape(L, 4, P).transpose(0, 2, 1)
    pp[:, :, PP_LG:PP_LG + 4] = np.asarray(inp["conv_ln_g"], f32).reshape(L, 4, P).transpose(0, 2, 1)
    pp[:, :, PP_LB:PP_LB + 4] = np.asarray(inp["conv_ln_b"], f32).reshape(L, 4, P).transpose(0, 2, 1)
    pp[:, :, PP_QG] = np.tile(np.asarray(inp["att_q_gain"], f32), (1, 2))
    pp[:, :, PP_KG] = np.tile(np.asarray(inp["att_k_gain"], f32), (1, 2))
    pp[:, :, PP_BG:PP_BG + 24] = np.asarray(inp["b_gate"], f32).reshape(L, 24, P).transpose(0, 2, 1)
    wr = np.concatenate([np.asarray(inp["w_group"], f32), np.asarray(inp["w_inner"], f32)], axis=-1)
    wr = np.ascontiguousarray(wr.reshape(L, KC, P, 36).transpose(0, 2, 1, 3))
    brow = np.ascontiguousarray(np.concatenate([np.asarray(inp["b_group"], f32), np.asarray(inp["b_inner"], f32)], axis=-1))
    k = np.arange(P)[:, None, None]
    kb = np.arange(5)[None, :, None]
    q = np.arange(P)[None, None, :]
    idx = np.clip(q - k + 128 * (4 - kb), -256, 256) + 256
    tab = np.asarray(inp["att_rel_bias"], f32)
    bx = tab[:, :, idx]
    biasx = np.ascontiguousarray(bx.transpose(0, 2, 3, 1, 4))
    return pp, wr, brow, biasx


_PROG = {}


def kernel(**inp):
    n = 8
    f32 = np.float32
    x = np.asarray(inp["x"], f32)
    c = np.asarray(inp["c"], f32)
    pos = np.asarray(inp["positions"], np.int32)
    pp, wr, brow, biasx = _layout_params(inp)
    cst = _constants()
    shared = dict(
        w_ada=np.asarray(inp["w_ada"], f32), b_ada=np.asarray(inp["b_ada"], f32),
        g_mix=np.asarray(inp["g_mix"], f32), g_ffn=np.asarray(inp["g_ffn"], f32),
        w_in=np.asarray(inp["w_in"], f32), ret_gn=np.asarray(inp["ret_gn"], f32),
        w_ret_out=np.asarray(inp["w_ret_out"], f32), w_conv_out=np.asarray(inp["w_conv_out"], f32),
        w_att_out=np.asarray(inp["w_att_out"], f32), w_out=np.asarray(inp["w_out"], f32),
        w_up=np.asarray(inp["w_up"], f32), w_down=np.asarray(inp["w_down"], f32),
        pp=pp, wr=wr, brow=brow, biasx=biasx, cst=cst)
    if "nc" not in _PROG:
        _PROG["nc"] = build_program()
    nc = _PROG["nc"]
    in_maps = []
    for i in range(n):
        sl = slice(NSEQ * i, NSEQ * (i + 1))
        m = dict(shared)
        m["x"] = np.ascontiguousarray(x[sl])
        m["pos"] = np.ascontiguousarray(pos[sl])
        m["cT"] = np.ascontiguousarray(c[sl].reshape(NSEQ, KC, P).transpose(2, 1, 0))
        in_maps.append(m)
    res = run_bass_kernel_spmd(nc, in_maps, core_ids=list(range(n)))
    return np.concatenate([np.asarray(r["out"], f32) for r in res.results], axis=0)
```

```python
import math
import numpy as np
from concourse.bass_utils import run_bass_kernel_spmd

import numpy as np
import concourse.bass as bass
import concourse.mybir as mybir

F32 = mybir.dt.float32
BF16 = mybir.dt.bfloat16
I32 = mybir.dt.int32
ALU = mybir.AluOpType
ACT = mybir.ActivationFunctionType
AX = mybir.AxisListType

SAME_ENGINE_SYNC = False
SMALL_N = 512


class Op:
    __slots__ = ("eng", "fn", "deps", "signal", "semval", "dma_inc", "idx", "small")

    def __init__(self, eng, fn, deps, dma_inc=None):
        self.eng = eng
        self.fn = fn
        self.deps = deps
        self.signal = False
        self.semval = None
        self.dma_inc = dma_inc
        self.idx = None
        self.small = False


class DmaTok:
    __slots__ = ("sem", "val", "eng")

    def __init__(self, sem, val):
        self.sem = sem
        self.val = val
        self.eng = None


class T:
    def __init__(self, h, name=""):
        self.h = h
        self.name = name
        self.last_w = None
        self.readers = []
        self.dsem = None
        self.dcount = 0

    def __getitem__(self, k):
        return self.h[k]


class TView:
    def __init__(self, base, ap):
        self.__dict__["base"] = base
        self.__dict__["ap"] = ap

    def __getitem__(self, k):
        return self.ap[k]

    def __getattr__(self, k):
        return getattr(self.base, k)

    def __setattr__(self, k, v):
        setattr(self.base, k, v)


class Sched:
    ENGS = ("pe", "act", "dve", "pool", "sp")

    def __init__(self, nc):
        self.nc = nc
        self.ops = {e: [] for e in self.ENGS}
        self.sems = {}
        self.stack = None
        self.dma_sems = []
        self.free_dma_sems = []
        self.dma_toks = []
        self.uid = 0

    def set_stack(self, stack):
        self.stack = stack

    def sem(self, name):
        return self.stack.enter_context(self.nc.semaphore(name))

    def sb(self, name, shape, dtype, stack=None):
        st = stack or self.stack
        self.uid += 1
        name = f"{name}_{self.uid}"
        h = st.enter_context(self.nc.sbuf_tensor(name, list(shape), dtype))
        t = T(h, name)
        if st is not self.stack:
            st.callback(self._retire, t)
        return t

    def _retire(self, t):
        if t.dsem is not None:
            self.free_dma_sems.append((t.dsem, t.dcount))
            t.dsem = None

    def ps(self, name, shape, dtype=F32, stack=None):
        st = stack or self.stack
        h = st.enter_context(self.nc.psum_tensor(name, list(shape), dtype))
        return T(h, name)

    def dram(self, h, name=""):
        return T(h, name)

    def _deps(self, reads, writes):
        deps = []
        for t in reads:
            if t.last_w is not None:
                deps.append(t.last_w)
        for t in writes:
            if t.last_w is not None:
                deps.append(t.last_w)
            deps.extend(t.readers)
        return deps

    def _commit(self, tok, reads, writes):
        for t in reads:
            t.readers.append(tok)
            if len(t.readers) > 64:
                t.readers = self._compact(t.readers)
        for t in writes:
            t.last_w = tok
            t.readers = []

    @staticmethod
    def _compact(toks):
        last = {}
        for tk in toks:
            key = (tk.eng if isinstance(tk, Op) else id(tk.sem))
            last[key] = tk
        return list(last.values())

    def op(self, eng, fn, reads=(), writes=(), small=True):
        o = Op(eng, fn, self._deps(reads, writes))
        o.small = bool(small) and eng != "pe"
        o.idx = len(self.ops[eng])
        self.ops[eng].append(o)
        self._commit(o, reads, writes)
        return o

    def dma(self, q, out_t, out_ap, in_t, in_ap, sem_t=None, **kw):
        st = sem_t or out_t
        if st.dsem is None:
            if self.free_dma_sems:
                st.dsem, st.dcount = self.free_dma_sems.pop()
            else:
                st.dsem = self.sem("d_" + st.name)
                st.dcount = 0
        st.dcount += 16
        tok = DmaTok(st.dsem, st.dcount)
        deps = self._deps([in_t], [out_t])
        sem = st.dsem

        def fn(e, out_ap=out_ap, in_ap=in_ap, kw=kw):
            return e.dma_start(out=out_ap, in_=in_ap, **kw)

        o = Op(q, fn, deps, dma_inc=sem)
        o.idx = len(self.ops[q])
        self.ops[q].append(o)
        self._commit(tok, [in_t], [out_t])
        self.dma_toks.append(tok)
        return tok

    def barrier(self):
        lasts = []
        for e in self.ENGS:
            for o in reversed(self.ops[e]):
                if o.fn is not None and o.dma_inc is None:
                    lasts.append(o)
                    break
        toks = list(self.dma_toks)
        self.dma_toks = []
        for e in self.ENGS:
            deps = [o for o in lasts if o.eng != e] + toks
            if not deps:
                continue
            o = Op(e, None, deps)
            o.idx = len(self.ops[e])
            self.ops[e].append(o)

    def finalize(self):
        for e in self.ENGS:
            for o in self.ops[e]:
                for d in o.deps:
                    if isinstance(d, Op):
                        if d.eng != o.eng or SAME_ENGINE_SYNC or d.small:
                            d.signal = True
        self.counts = {}
        for e in self.ENGS:
            c = 0
            for o in self.ops[e]:
                if o.signal:
                    c += 1
                    o.semval = c
            self.counts[e] = c
            if c > 0 or True:
                self.sems[e] = self.sem("eng_" + e)

    def replay(self, e, handle):
        seen = {}
        nc = self.nc
        for o in self.ops[e]:
            waits = {}
            for d in o.deps:
                if isinstance(d, Op):
                    if d.eng == e and not (SAME_ENGINE_SYNC or d.small):
                        continue
                    key = ("e", d.eng)
                    sem = self.sems[d.eng]
                    val = d.semval
                else:
                    key = ("d", id(d.sem))
                    sem = d.sem
                    val = d.val
                if seen.get(key, 0) >= val:
                    continue
                if key not in waits or waits[key][1] < val:
                    waits[key] = (sem, val)
            for key, (sem, val) in waits.items():
                handle.wait_ge(sem, val)
                seen[key] = val
            if o.fn is None:
                continue
            ins = o.fn(handle)
            if o.dma_inc is not None:
                ins.then_inc(o.dma_inc, 16)
            elif o.signal:
                ins.then_inc(self.sems[e], 1)

    def emit(self, final_toks=()):
        nc = self.nc
        self.finalize()
        with nc.Block() as block:
            @block.tensor
            def _(h):
                self.replay("pe", h)

            @block.scalar
            def _(h):
                self.replay("act", h)

            @block.vector
            def _(h):
                self.replay("dve", h)

            @block.gpsimd
            def _(h):
                self.replay("pool", h)

            @block.sync
            def _(h):
                self.replay("sp", h)
                for tk in final_toks:
                    h.wait_ge(tk.sem, tk.val)

from contextlib import ExitStack

P = 128
SEQ = 2048
D = 1024
KC = 8
NT = 16
NQ = 4
L = 2
NSEQ = 2
IN_COLS = 7680
EPS = 1e-6
NEG = -1e30
C_RQ, C_RK, C_RV, C_RG = 0, 512, 1024, 1536
C_CU = 2048
C_AQ, C_AK, C_AV = 3072, 3584, 4096
C_GL = 4608
NG, NE = 4, 8
PP_CW = 0
PP_CB = 124
PP_LG = 128
PP_LB = 132
PP_QG = 136
PP_KG = 137
PP_BG = 138
NPP = 162
CS_ID = 0
CS_MASK = 128
CS_QDEC = 640
CS_KDEC = 1152
CS_INV = 1156
CS_SGN = 1157
CS_MLO = 1158
CS_MHI = 1159
NCST = 1160
RET_CD = [float((1.0 - 2.0 ** (-5 - h)) ** 128) for h in range(4)]


def build_program(n_layers=L, n_seq=NSEQ, n_experts=NG * NE, dbg=None, dump=False):
    nc = bass.Bass("TRN2", target_bir_lowering=False)
    dump_toks = []

    def din(name, shape, dt=F32):
        return nc.dram_tensor(name, list(shape), dt, kind="ExternalInput")

    x_h = din("x", [NSEQ, SEQ, D])
    pos_h = din("pos", [NSEQ, SEQ], I32)
    cT_h = din("cT", [P, KC, NSEQ])
    w_ada_h = din("w_ada", [L, D, 6 * D])
    b_ada_h = din("b_ada", [L, 6 * D])
    g_mix_h = din("g_mix", [L, D])
    g_ffn_h = din("g_ffn", [L, D])
    w_in_h = din("w_in", [L, D, IN_COLS])
    ret_gn_h = din("ret_gn", [L, 512])
    w_ro_h = din("w_ret_out", [L, 512, D])
    w_co_h = din("w_conv_out", [L, 512, D])
    w_ao_h = din("w_att_out", [L, 512, D])
    w_out_h = din("w_out", [L, D, D])
    w_up_h = din("w_up", [L, NG, NE, D, 512])
    w_dn_h = din("w_down", [L, NG, NE, 256, D])
    pp_h = din("pp", [L, P, NPP])
    wr_h = din("wr", [L, P, KC, 36])
    brow_h = din("brow", [L, 36])
    biasx_h = din("biasx", [L, P, 5, 8, P])
    cst_h = din("cst", [P, NCST])
    out_h = nc.dram_tensor("out", [NSEQ, SEQ, D], F32, kind="ExternalOutput")
    modd_h = nc.dram_tensor("modd", [L, NSEQ, 6 * D], F32)
    dbg_h = nc.dram_tensor("dbg", [P, 6, 8192], F32, kind="ExternalOutput") if dump else None

    top = ExitStack()
    with top:
        S = Sched(nc)
        S.set_stack(top)

        def MM(out_t, out_ap, lt, lap, rt, rap, start=True, stop=True):
            S.op("pe", lambda e: e.matmul(out_ap, lhsT=lap, rhs=rap, start=start, stop=stop),
                 [lt, rt], [out_t])

        def TR(out_t, out_ap, in_t, in_ap, id_t, id_ap):
            S.op("pe", lambda e: e.transpose(out=out_ap, in_=in_ap, identity=id_ap), [in_t, id_t], [out_t])

        def ACTF(out_t, out_ap, in_t, in_ap, func, bias=None, scale=None, accum=None, reads=()):
            kw = {}
            if bias is not None:
                kw["bias"] = bias
            if scale is not None:
                kw["scale"] = scale
            wr = [out_t]
            if accum is not None:
                kw["accum_out"] = accum[1]
                wr.append(accum[0])
            S.op("act", lambda e: e.activation(out=out_ap, in_=in_ap, func=func, **kw),
                 [in_t] + list(reads), wr, small=(accum is not None or out_ap.free_size() < SMALL_N))

        def TT(eng, out_t, out_ap, a_t, a_ap, b_t, b_ap, op):
            S.op(eng, lambda e: e.tensor_tensor(out=out_ap, in0=a_ap, in1=b_ap, op=op), [a_t, b_t], [out_t],
                 small=out_ap.free_size() < SMALL_N)

        def TS(eng, out_t, out_ap, a_t, a_ap, s1, s2, op0, op1=None, reads=()):
            if op1 is None:
                S.op(eng, lambda e: e.tensor_scalar(out=out_ap, in0=a_ap, scalar1=s1, scalar2=None, op0=op0),
                     [a_t] + list(reads), [out_t], small=out_ap.free_size() < SMALL_N)
            else:
                S.op(eng, lambda e: e.tensor_scalar(out=out_ap, in0=a_ap, scalar1=s1, scalar2=s2, op0=op0, op1=op1),
                     [a_t] + list(reads), [out_t], small=out_ap.free_size() < SMALL_N)

        def STT(eng, out_t, out_ap, a_t, a_ap, sc, b_t, b_ap, op0, op1, reads=()):
            S.op(eng, lambda e: e.scalar_tensor_tensor(out=out_ap, in0=a_ap, scalar=sc, in1=b_ap, op0=op0, op1=op1),
                 [a_t, b_t] + list(reads), [out_t], small=out_ap.free_size() < SMALL_N)

        def CP(eng, out_t, out_ap, in_t, in_ap):
            if eng == "act":
                S.op("act", lambda e: e.copy(out=out_ap, in_=in_ap), [in_t], [out_t], small=out_ap.free_size() < SMALL_N)
            else:
                S.op(eng, lambda e: e.tensor_copy(out=out_ap, in_=in_ap), [in_t], [out_t], small=out_ap.free_size() < SMALL_N)

        def MSET(eng, t, ap, val):
            S.op(eng, lambda e: e.memset(ap, val), [], [t], small=ap.free_size() < SMALL_N)

        def RSTD(t, ap, n_is_one_col=True):
            S.op("dve", lambda e: e.reciprocal(out=ap, in_=ap), [t], [t])
            ACTF(t, ap, t, ap, ACT.Sqrt)

        class Pool:
            def __init__(self, name, shape, dt, n, stack):
                self.tiles = [S.sb(f"{name}{i}", shape, dt, stack) for i in range(n)]
                self.i = 0

            def get(self):
                t = self.tiles[self.i % len(self.tiles)]
                self.i += 1
                return t

        def run_units(units):
            nxt = units[0][0]() if units[0][0] is not None else None
            for i, (ld, cp) in enumerate(units):
                cur, nxt = nxt, None
                if i + 1 < len(units) and units[i + 1][0] is not None:
                    nxt = units[i + 1][0]()
                cp(cur)

        def wcols(ap2d, c0, n):
            return ap2d.rearrange("(kc p) n -> p kc n", p=P)[:, :, c0:c0 + n]

        DR = lambda h, name: S.dram(h, name)
        dbg_d = DR(dbg_h, "dbg") if dump else None

        def DUMP(slot, t, ap, ncols):
            if dump:
                dump_toks.append(S.dma("pool", dbg_d, dbg_h[:, slot, 0:ncols], t, ap, sem_t=t))
        x_d = DR(x_h, "x")
        pos_d = DR(pos_h, "pos")
        wts_d = DR(w_in_h, "weights")
        modd_d = DR(modd_h, "modd")
        xo_d = [[DR(out_h, f"xo{s}_{j}") for j in range(NT)] for s in range(NSEQ)]

        cst = S.sb("cst", [P, NCST], F32)
        S.dma("sp", cst, cst[:], wts_d, cst_h[:, :])
        ident = (cst, cst[:, CS_ID:CS_ID + P])
        ps = [S.ps(f"ps{i}", [P, 512]) for i in range(8)]
        blk64 = S.sb("blk64", [P, P], BF16)
        MSET("dve", blk64, blk64[:], 0.0)
        MSET("dve", blk64, blk64[0:64, 0:64], 1.0 / 64)
        MSET("dve", blk64, blk64[64:128, 64:128], 1.0 / 64)
        onesln = S.sb("onesln", [P, P], F32)
        MSET("dve", onesln, onesln[:], 1.0 / 512)

        with ExitStack() as st:
          if dbg != "pro0":
              cT = S.sb("cT", [P, KC, NSEQ], F32, st)
              scT = S.sb("scT", [P, KC, NSEQ], BF16, st)
              S.dma("sp", cT, cT[:], wts_d, cT_h[:, :, :])
              ACTF(scT, scT[:], cT, cT[:], ACT.Silu)
              modrow = S.sb("modrow", [NSEQ, 6 * D], F32, st)
              brow2 = S.sb("brow2", [NSEQ, 6 * D], F32, st)
              grow = S.sb("grow", [NSEQ, 2, D], F32, st)
              wpool = Pool("wada", [P, KC, 512], BF16, 2, st)
              for l in range(n_layers):
                  S.dma("sp", brow2, brow2[:], wts_d, b_ada_h[l:l + 1, :].partition_broadcast(NSEQ))
                  S.dma("sp", grow, grow[:, 0, :], wts_d, g_mix_h[l:l + 1, :].partition_broadcast(NSEQ))
                  S.dma("sp", grow, grow[:, 1, :], wts_d, g_ffn_h[l:l + 1, :].partition_broadcast(NSEQ))
                  if dbg == "pro1":
                      break
                  for cb in range(12):
                      wb = wpool.get()
                      S.dma("pool", wb, wb[:], wts_d, wcols(w_ada_h[l], cb * 512, 512))
                      pt = ps[cb % 2]
                      for kc in range(KC):
                          MM(pt, pt[0:NSEQ, :], scT, scT[:, kc, :], wb, wb[:, kc, :], kc == 0, kc == KC - 1)
                      TT("dve", modrow, modrow[:, cb * 512:(cb + 1) * 512], pt, pt[0:NSEQ, :],
                         brow2, brow2[:, cb * 512:(cb + 1) * 512], ALU.add)
                  if dbg == "pro2":
                      break
                  for i, sl in ((0, 1), (1, 4)):
                      TS("dve", modrow, modrow[:, sl * D:(sl + 1) * D], modrow, modrow[:, sl * D:(sl + 1) * D], 1.0, None, ALU.add)
                      TT("dve", modrow, modrow[:, sl * D:(sl + 1) * D], modrow, modrow[:, sl * D:(sl + 1) * D],
                         grow, grow[:, i, :], ALU.mult)
                  if dbg == "pro3":
                      break
                  S.dma("sp", modd_d, modd_h[l], modrow, modrow[:], sem_t=modrow)
        S.barrier()

        class _Stop(Exception):
            pass

        def load_mod_bc(st, l, s, idx, name):
            t = S.sb(name, [P, D], F32, st)
            S.dma("sp", t, t[:], modd_d, modd_h[l, s:s + 1, idx * D:(idx + 1) * D].partition_broadcast(P))
            return t

        def norm_and_transpose(st, l, s, first_layer_input, gmod, shift, hT, h2f_cb=None):
            xs_pool = Pool("xs", [P, D], F32, 3, st)
            hf_pool = Pool("hf", [P, D], F32, 3, st)
            junk = S.sb("junk", [P, D], BF16, st)
            ssq = Pool("ssq", [P, 8], F32, 3, st)
            def phase_a(j):
                xs = xs_pool.get()
                if first_layer_input:
                    S.dma("sp", xs, xs[:], x_d, x_h[s, j * P:(j + 1) * P, :])
                else:
                    S.dma("sp", xs, xs[:], xo_d[s][j], out_h[s, j * P:(j + 1) * P, :])
                ss = ssq.get()
                MSET("dve", ss, ss[:], 0.0)
                ACTF(junk, junk[:], xs, xs[:], ACT.Square, accum=(ss, ss[:, 0:1]))
                TS("dve", ss, ss[:, 0:1], ss, ss[:, 0:1], 1.0 / D, EPS, ALU.mult, ALU.add)
                S.op("dve", lambda e, ss=ss: e.reciprocal(out=ss[:, 0:1], in_=ss[:, 0:1]), [ss], [ss])
                ACTF(ss, ss[:, 0:1], ss, ss[:, 0:1], ACT.Sqrt)
                hf = hf_pool.get()
                STT("dve", hf, hf[:], xs, xs[:], ss[:, 0:1], gmod, gmod[:], ALU.mult, ALU.mult, reads=[ss])
                TT("pool", hf, hf[:], hf, hf[:], shift, shift[:], ALU.add)
                return hf

            def phase_b(j, hf):
                q, jj = divmod(j, 4)
                pa, pb = ps[(2 * j) % 4], ps[(2 * j) % 4 + 1]
                for kc in range(KC):
                    pt = pa if kc < 4 else pb
                    TR(pt, pt[:, (kc % 4) * P:(kc % 4 + 1) * P], hf, hf[:, kc * P:(kc + 1) * P], *ident)
                CP("act", hT[q], hT[q][:, 0:4, jj * P:(jj + 1) * P], pa, pa[:].rearrange("p (k t) -> p k t", k=4))
                CP("act", hT[q], hT[q][:, 4:8, jj * P:(jj + 1) * P], pb, pb[:].rearrange("p (k t) -> p k t", k=4))
                if h2f_cb is not None:
                    h2f_cb(j, pa, pb)

            hf_n = phase_a(0)
            for j in range(NT):
                hf_c = hf_n
                if j + 1 < NT:
                    hf_n = phase_a(j + 1)
                phase_b(j, hf_c)

        def mixer(s, l):
            first = (l == 0)
            w_in = w_in_h[l]
            with ExitStack() as st:
                hT = [S.sb(f"hT{q}", [P, KC, 512], BF16, st) for q in range(NQ)]
                oT = {k: S.sb(f"oT_{k}", [P, 4, SEQ], BF16, st) for k in ("ret", "conv", "att")}
                pp = S.sb("pp", [P, NPP], F32, st)
                S.dma("sp", pp, pp[:], wts_d, pp_h[l])
                with ExitStack() as st1:
                    gmod = load_mod_bc(st1, l, s, 1, "gmod1")
                    shift = load_mod_bc(st1, l, s, 0, "shift1")
                    norm_and_transpose(st1, l, s, first, gmod, shift, hT)
                DUMP(0, hT[0], hT[0][:].rearrange("p k t -> p (k t)"), 4096)
                S.barrier()
                if dbg == "s1":
                    return "stop"
                fpool = Pool("fblk", [P, KC, P], BF16, 4, st)

                def fproj(c0, q, pt):
                    raise NotImplementedError

                def load_f(c0, swap=False):
                    wb = fpool.get()
                    if not swap:
                        S.dma("pool", wb, wb[:], wts_d, wcols(w_in, c0, P))
                    else:
                        S.dma("pool", wb, wb[:, :, 0:64], wts_d, wcols(w_in, c0 + 64, 64))
                        S.dma("pool", wb, wb[:, :, 64:128], wts_d, wcols(w_in, c0, 64))
                    return wb

                def f_mm(wb, q, pt):
                    for kc in range(KC):
                        MM(pt, pt[:], wb, wb[:, kc, :], hT[q], hT[q][:, kc, :], kc == 0, kc == KC - 1)

                with ExitStack() as st2:
                    cosT = S.sb("cosT", [P, SEQ], BF16, st2)
                    sinT = S.sb("sinT", [P, SEQ], BF16, st2)
                    with ExitStack() as st3:
                        posi = S.sb("posi", [P, SEQ], I32, st3)
                        u = S.sb("rope_u", [P, SEQ], F32, st3)
                        f = S.sb("rope_f", [P, SEQ], F32, st3)
                        ki = S.sb("rope_ki", [P, SEQ], I32, st3)
                        S.dma("sp", posi, posi[:], pos_d, pos_h[s:s + 1, :].partition_broadcast(P))
                        CP("dve", u, u[:], posi, posi[:])
                        TS("dve", u, u[:], u, u[:], cst[:, CS_INV:CS_INV + 1], 1.0 / (2.0 * math.pi), ALU.mult, ALU.mult, reads=[cst])
                        for tab, off, sgn in ((sinT, 0.0, True), (cosT, 0.25, False)):
                            if off != 0.0:
                                TS("dve", f, f[:], u, u[:], off, None, ALU.add)
                                src = f
                            else:
                                src = u
                            CP("dve", ki, ki[:], src, src[:])
                            fk = S.sb("rope_fk" + ("s" if sgn else "c"), [P, SEQ], F32, st3)
                            CP("dve", fk, fk[:], ki, ki[:])
                            TT("dve", fk, fk[:], src, src[:], fk, fk[:], ALU.subtract)
                            TS("dve", fk, fk[:], fk, fk[:], 0.49999, -0.49999, ALU.min, ALU.max)
                            if sgn:
                                ACTF(fk, fk[:], fk, fk[:], ACT.Sin, scale=2.0 * math.pi)
                                TS("dve", tab, tab[:], fk, fk[:], cst[:, CS_SGN:CS_SGN + 1], None, ALU.mult, reads=[cst])
                            else:
                                ACTF(tab, tab[:], fk, fk[:], ACT.Sin, scale=2.0 * math.pi)
                    S.barrier()
                    state_f = S.sb("state_f", [P, 4, P], F32, st2)
                    state_b = S.sb("state_b", [P, 4, P], BF16, st2)
                    gn_bc = S.sb("gn_bc", [P, 512], F32, st2)
                    S.dma("sp", gn_bc, gn_bc[:], wts_d, ret_gn_h[l:l + 1, :].partition_broadcast(P))
                    HS = SEQ // 2
                    qT = [S.sb("r_qT0", [P, 4, HS], BF16, st2), TView(oT["conv"], oT["conv"][:, 0:2, :].rearrange("p a (b t) -> p (a b) t", b=2))]
                    kT = [S.sb("r_kT0", [P, 4, HS], BF16, st2), TView(oT["conv"], oT["conv"][:, 2:4, :].rearrange("p a (b t) -> p (a b) t", b=2))]
                    v_tok = [S.sb("r_v0", [P, 8, 512], BF16, st2), TView(oT["att"], oT["att"][:, 0:2, :].rearrange("p a (b t) -> p (a b) t", b=4))]
                    gs = [S.sb("r_gs0", [P, 8, 512], BF16, st2), TView(oT["att"], oT["att"][:, 2:4, :].rearrange("p a (b t) -> p (a b) t", b=4))]
                    tpool = Pool("tblk", [P, KC, 512], BF16, 2, st2)
                    t1p = Pool("r_t1", [P, 512], F32, 2, st2)
                    t2p = Pool("r_t2", [P, 512], F32, 2, st2)
                    sTm_p = Pool("r_sTm", [P, 512], BF16, 2, st2)
                    qd_p = Pool("r_qd", [P, 4, P], BF16, 2, st2)
                    kt_p = Pool("r_kt", [P, 512], BF16, 2, st2)
                    on_p = Pool("r_on", [P, 512], F32, 2, st2)
                    og_p = Pool("r_og", [P, 512], F32, 3, st2)
                    st_p = Pool("r_stats", [P, 16], F32, 2, st2)
                    sgt_p = Pool("r_sg", [P, 512], F32, 2, st2)
                    junk = S.sb("r_junk", [P, P], BF16, st2)
                    def r_units(seg, mixed_banks=False):
                        qk_banks = [(ps[1], ps[5]), (ps[1], ps[5])] if mixed_banks else [(ps[0], ps[1]), (ps[2], ps[3])]
                        vg_banks = [ps[1], ps[5]] if mixed_banks else [ps[4], ps[5]]
                        us = []
                        for which, cbase, dst in (("q", C_RQ, qT[seg]), ("k", C_RK, kT[seg])):
                            for h in range(4):
                                def ld(cbase=cbase, h=h):
                                    return (load_f(cbase + h * P), load_f(cbase + h * P, swap=True))

                                def cp(w, dst=dst, h=h, seg=seg):
                                    wb, wsw = w
                                    for qq in range(2):
                                        q = seg * 2 + qq
                                        pa, pb = qk_banks[qq]
                                        f_mm(wb, q, pa)
                                        f_mm(wsw, q, pb)
                                        t1, t2 = t1p.get(), t2p.get()
                                        TT("dve", t1, t1[:], pa, pa[:], cosT, cosT[:, q * 512:(q + 1) * 512], ALU.mult)
                                        TT("dve", t2, t2[:], pb, pb[:], sinT, sinT[:, q * 512:(q + 1) * 512], ALU.mult)
                                        TT("pool", dst, dst[:, h, qq * 512:(qq + 1) * 512], t1, t1[:], t2, t2[:], ALU.add)
                                us.append((ld, cp))
                        for which, cbase in (("v", C_RV), ("g", C_RG)):
                            def ld(cbase=cbase):
                                wb = tpool.get()
                                S.dma("pool", wb, wb[:], wts_d, wcols(w_in, cbase, 512))
                                return wb

                            def cp(wb, which=which, seg=seg):
                                for jl in range(8):
                                    j = seg * 8 + jl
                                    q, jj = divmod(j, 4)
                                    pt = vg_banks[jl % 2]
                                    for kc in range(KC):
                                        MM(pt, pt[:], hT[q], hT[q][:, kc, jj * P:(jj + 1) * P], wb, wb[:, kc, :], kc == 0, kc == KC - 1)
                                    if which == "v":
                                        CP("act", v_tok[seg], v_tok[seg][:, jl, :], pt, pt[:])
                                    else:
                                        sg = sgt_p.get()
                                        ACTF(sg, sg[:], pt, pt[:], ACT.Silu)
                                        TT("pool", gs[seg], gs[seg][:, jl, :], sg, sg[:], gn_bc, gn_bc[:], ALU.mult)
                            us.append((ld, cp))
                        return us

                    def r_recA(seg, jl):
                        qT_, kT_, v_tok_, gs_ = qT[seg], kT[seg], v_tok[seg], gs[seg]
                        if True:
                            j = seg * 8 + jl
                            tsl = slice(jl * P, (jl + 1) * P)
                            pS, pO, pK, pT_ = ps[0], ps[2 + (jl % 2)], ps[6], ps[7]
                            for h in range(4):
                                MM(pS, pS[:, h * P:(h + 1) * P], kT_, kT_[:, h, tsl], qT_, qT_[:, h, tsl])
                            sTm = sTm_p.get()
                            TT("dve", sTm, sTm[:], pS, pS[:], cst, cst[:, CS_MASK:CS_MASK + 512], ALU.mult)
                            qd = qd_p.get()
                            TT("pool", qd, qd[:], qT_, qT_[:, :, tsl], cst,
                               cst[:, CS_QDEC:CS_QDEC + 512].rearrange("p (h c) -> p h c", h=4), ALU.mult)
                            pTb = pT_[:, 0:256].bitcast(BF16)
                            for h in range(4):
                                S.op("pe", lambda e, h=h, tsl=tsl, pTb=pTb: e.transpose(
                                    out=pTb[:, h * P:(h + 1) * P], in_=kT_[:, h, tsl], identity=identb[:]),
                                    [kT_, identb], [pT_])
                            kt = kt_p.get()
                            for h in range(4):
                                TS("dve", kt, kt[:, h * P:(h + 1) * P], pT_, pTb[:, h * P:(h + 1) * P],
                                   cst[:, CS_KDEC + h:CS_KDEC + h + 1], None, ALU.mult, reads=[cst])
                            for h in range(4):
                                hs = slice(h * P, (h + 1) * P)
                                MM(pO, pO[:, hs], sTm, sTm[:, hs], v_tok_, v_tok_[:, jl, hs], True, j == 0)
                                if j > 0:
                                    MM(pO, pO[:, hs], qd, qd[:, h, :], state_b, state_b[:, h, :], False, True)
                            if j < NT - 1:
                                for h in range(4):
                                    hs = slice(h * P, (h + 1) * P)
                                    MM(pK, pK[:, hs], kt, kt[:, hs], v_tok_, v_tok_[:, jl, hs])
                                if j == 0:
                                    CP("dve", state_f, state_f[:], pK, pK[:].rearrange("p (h e) -> p h e", h=4))
                                else:
                                    for h in range(4):
                                        STT("dve", state_f, state_f[:, h, :], state_f, state_f[:, h, :], RET_CD[h],
                                            pK, pK[:, h * P:(h + 1) * P], ALU.mult, ALU.add)
                                CP("act", state_b, state_b[:], state_f, state_f[:])
                    def r_recB(seg, jl):
                        qT_, kT_, v_tok_, gs_ = qT[seg], kT[seg], v_tok[seg], gs[seg]
                        if True:
                            j = seg * 8 + jl
                            pO = ps[2 + (jl % 2)]
                            stt = st_p.get()
                            MSET("dve", stt, stt[:], 0.0)
                            for h in range(4):
                                hs = slice(h * P, (h + 1) * P)
                                ACTF(junk, junk[:], pO, pO[:, hs], ACT.Copy, accum=(stt, stt[:, h:h + 1]))
                                ACTF(junk, junk[:], pO, pO[:, hs], ACT.Square, accum=(stt, stt[:, 4 + h:5 + h]))
                            TS("dve", stt, stt[:, 0:8], stt, stt[:, 0:8], 1.0 / P, None, ALU.mult)
                            TT("dve", stt, stt[:, 8:12], stt, stt[:, 0:4], stt, stt[:, 0:4], ALU.mult)
                            TT("dve", stt, stt[:, 12:16], stt, stt[:, 4:8], stt, stt[:, 8:12], ALU.subtract)
                            TS("dve", stt, stt[:, 12:16], stt, stt[:, 12:16], EPS, None, ALU.add)
                            RSTD(stt, stt[:, 12:16])
                            on = on_p.get()
                            for h in range(4):
                                hs = slice(h * P, (h + 1) * P)
                                TS("dve", on, on[:, hs], pO, pO[:, hs], stt[:, h:h + 1], stt[:, 12 + h:13 + h],
                                   ALU.subtract, ALU.mult, reads=[stt])
                            og = og_p.get()
                            TT("pool", og, og[:], on, on[:], gs_, gs_[:, jl, :], ALU.mult)
                            return og

                    def r_recC(seg, jl, og):
                        j = seg * 8 + jl
                        pX = ps[4]
                        for h in range(4):
                            TR(pX, pX[:, h * P:(h + 1) * P], og, og[:, h * P:(h + 1) * P], *ident)
                        CP("act", oT["ret"], oT["ret"][:, :, j * P:(j + 1) * P], pX, pX[:].rearrange("p (h c) -> p h c", h=4))

                    u1 = r_units(1, mixed_banks=True)
                    ogs = {}

                    def stepB(seg, i):
                        ogs[(seg, i)] = r_recB(seg, i)

                    def stepC(seg, i):
                        r_recC(seg, i, ogs.pop((seg, i)))

                    mixed = [(None, lambda _w: r_recA(0, 0))]
                    for i in range(max(8, len(u1))):
                        if i < 8:
                            if i + 1 < 8:
                                mixed.append((None, lambda _w, i=i: r_recA(0, i + 1)))
                            mixed.append((None, lambda _w, i=i: stepB(0, i)))
                            if i >= 1:
                                mixed.append((None, lambda _w, i=i: stepC(0, i - 1)))
                        if i == 8:
                            mixed.append((None, lambda _w: stepC(0, 7)))
                        if i < len(u1):
                            mixed.append(u1[i])
                    tail = [(None, lambda _w: r_recA(1, 0))]
                    for i in range(8):
                        if i + 1 < 8:
                            tail.append((None, lambda _w, i=i: r_recA(1, i + 1)))
                        tail.append((None, lambda _w, i=i: stepB(1, i)))
                        if i >= 1:
                            tail.append((None, lambda _w, i=i: stepC(1, i - 1)))
                    tail.append((None, lambda _w: stepC(1, 7)))
                    run_units(r_units(0) + mixed + tail)
                S.barrier()
                if dbg == "ret":
                    return "stop"
                with ExitStack() as st2:
                    zT = S.sb("c_zT", [P, 4, 30 + SEQ], BF16, st2)
                    acc = S.sb("c_acc", [P, 4, SEQ], F32, st2)
                    sgp = Pool("c_sg", [P, 512], F32, 2, st2)
                    dgp = Pool("c_dg", [P, P], BF16, 6, st2)
                    MSET("dve", zT, zT[:, :, 0:30], 0.0)
                    def c_unit(cc):
                        def ld():
                            return (load_f(C_CU + cc * P), load_f(C_CU + 512 + cc * P))

                        def cp(w):
                            wa, wb = w
                            for q in range(NQ):
                                pa, pb = ps[2 * (q % 2)], ps[2 * (q % 2) + 1]
                                f_mm(wa, q, pa)
                                f_mm(wb, q, pb)
                                sg = sgp.get()
                                ACTF(sg, sg[:], pb, pb[:], ACT.Sigmoid)
                                TT("dve", zT, zT[:, cc, 30 + q * 512:30 + (q + 1) * 512], pa, pa[:], sg, sg[:], ALU.mult)
                            for k in range(31):
                                dg = dgp.get()
                                wk = pp[:, PP_CW + cc * 31 + k:PP_CW + cc * 31 + k + 1]
                                TS("pool" if k % 2 else "dve", dg, dg[:], identb, identb[:], wk, None, ALU.mult, reads=[pp])
                                for q in range(NQ):
                                    pc = ps[4 + q]
                                    MM(pc, pc[:], dg, dg[:], zT, zT[:, cc, q * 512 + k:q * 512 + k + 512], k == 0, k == 30)
                            for q in range(NQ):
                                pc = ps[4 + q]
                                TS("dve", acc, acc[:, cc, q * 512:(q + 1) * 512], pc, pc[:], pp[:, PP_CB + cc:PP_CB + cc + 1], None,
                                   ALU.add, reads=[pp])
                        return (ld, cp)

                    run_units([c_unit(cc) for cc in range(4)])
                    sqp = Pool("c_sq", [P, 512], F32, 2, st2)
                    m2p = Pool("c_m2", [P, 512], F32, 2, st2)
                    rsp = Pool("c_rs", [P, 512], F32, 2, st2)
                    tp = Pool("c_t", [P, 512], F32, 2, st2)
                    for q in range(NQ):
                        qs = slice(q * 512, (q + 1) * 512)
                        pm, pe2 = ps[4 + 2 * (q % 2)], ps[5 + 2 * (q % 2)]
                        for cc in range(4):
                            MM(pm, pm[:], onesln, onesln[:], acc, acc[:, cc, qs], cc == 0, cc == 3)
                        for cc in range(4):
                            sq = sqp.get()
                            ACTF(sq, sq[:], acc, acc[:, cc, qs], ACT.Square)
                            MM(pe2, pe2[:], onesln, onesln[:], sq, sq[:], cc == 0, cc == 3)
                        m2 = m2p.get()
                        ACTF(m2, m2[:], pm, pm[:], ACT.Square)
                        rs = rsp.get()
                        TT("dve", rs, rs[:], pe2, pe2[:], m2, m2[:], ALU.subtract)
                        TS("dve", rs, rs[:], rs, rs[:], EPS, None, ALU.add)
                        RSTD(rs, rs[:])
                        for cc in range(4):
                            t = tp.get()
                            TT("dve", t, t[:], acc, acc[:, cc, qs], pm, pm[:], ALU.subtract)
                            TT("pool", t, t[:], t, t[:], rs, rs[:], ALU.mult)
                            ACTF(oT["conv"], oT["conv"][:, cc, qs], t, t[:], ACT.Silu,
                                 bias=pp[:, PP_LB + cc:PP_LB + cc + 1], scale=pp[:, PP_LG + cc:PP_LG + cc + 1], reads=[pp])
                S.barrier()
                if dbg == "conv":
                    return "stop"
                with ExitStack() as st2:
                    aqM = [S.sb(f"a_qM{i}", [P, 4, SEQ], BF16, st2) for i in range(2)]
                    MSET("dve", aqM[0], aqM[0][:], 0.0)
                    MSET("dve", aqM[1], aqM[1][:], 0.0)
                    akT = S.sb("a_kT", [P, 4, SEQ], BF16, st2)
                    vaug = S.sb("a_v", [P, NT, 8, 66], BF16, st2)
                    biasT = S.sb("a_bias", [P, 5, 8, P], F32, st2)
                    S.dma("sp", biasT, biasT[:], wts_d, biasx_h[l])
                    MSET("dve", biasT, biasT[0:64, 0, :, 64:128], NEG)
                    MSET("dve", biasT, biasT[64:128, 4, :, 0:64], NEG)
                    MSET("dve", vaug, vaug[:], 1.0)
                    with ExitStack() as st3:
                        sqp = Pool("a_sq", [P, 512], BF16, 2, st3)
                        rsp = Pool("a_rs", [P, 512], F32, 2, st3)
                        def a_unit(dst, cbase, gcol, c):
                            def ld():
                                return load_f(cbase + c * P)

                            def cp(wb):
                                for q in range(NQ):
                                    qs = slice(q * 512, (q + 1) * 512)
                                    pa, pb = ps[2 * (q % 2)], ps[2 * (q % 2) + 1]
                                    f_mm(wb, q, pa)
                                    sq = sqp.get()
                                    ACTF(sq, sq[:], pa, pa[:], ACT.Square)
                                    MM(pb, pb[:], blk64, blk64[:], sq, sq[:])
                                    rs = rsp.get()
                                    TS("dve", rs, rs[:], pb, pb[:], EPS, None, ALU.add)
                                    RSTD(rs, rs[:])
                                    if dst is None:
                                        for i in range(2):
                                            hp = slice(64 * i, 64 * i + 64)
                                            STT("dve", aqM[i], aqM[i][hp, c, qs], pa, pa[hp, :], pp[hp, gcol:gcol + 1], rs, rs[hp, :],
                                                ALU.mult, ALU.mult, reads=[pp])
                                    else:
                                        STT("dve", dst, dst[:, c, qs], pa, pa[:], pp[:, gcol:gcol + 1], rs, rs[:],
                                            ALU.mult, ALU.mult, reads=[pp])
                            return (ld, cp)

                        def av_ld():
                            wb = tpool.get()
                            S.dma("pool", wb, wb[:], wts_d, wcols(w_in, C_AV, 512))
                            return wb

                        def av_cp(wb):
                            for j in range(NT if dbg != "att0" else 0):
                                q, jj = divmod(j, 4)
                                pt = ps[4 + j % 2]
                                for kc in range(KC):
                                    MM(pt, pt[:], hT[q], hT[q][:, kc, jj * P:(jj + 1) * P], wb, wb[:, kc, :], kc == 0, kc == KC - 1)
                                CP("act", vaug, vaug[:, j, :, 0:64], pt, pt[:].rearrange("p (h d) -> p h d", h=8))

                        tpool = Pool("tblk", [P, KC, 512], BF16, 1, st3)
                        run_units([a_unit(dst, cbase, gcol, c) for dst, cbase, gcol in ((None, C_AQ, PP_QG), (akT, C_AK, PP_KG))
                                   for c in range(4)] + [(av_ld, av_cp)])
                    S.barrier()
                    ep = Pool("a_e", [P, 512], F32, 3, st2)
                    pTp = Pool("a_pT", [P, 5, 512], BF16, 3, st2)
                    recp = Pool("a_rec", [P, 8], F32, 2, st2)
                    oap = Pool("a_o", [P, 512], F32, 2, st2)
                    n_att = NT if dbg not in ("att0", "att1") else 0

                    def a_scores(j, g):
                        tsl = slice(j * P, (j + 1) * P)
                        kbs = [kb for kb in range(5) if j - 4 + kb >= 0]
                        pTt = pTp.get()
                        for kb in kbs:
                            jk = j - 4 + kb
                            ksl = slice(jk * P, (jk + 1) * P)
                            pq = ps[kb % 2 + 2 * g]
                            for hh in range(4):
                                h = g * 4 + hh
                                c = h // 2
                                MM(pq, pq[:, hh * P:(hh + 1) * P], akT, akT[:, c, ksl], aqM[h % 2], aqM[h % 2][:, c, tsl])
                            e_ = ep.get()
                            STT("dve", e_, e_[:].rearrange("p (h q) -> p h q", h=4), pq,
                                pq[:].rearrange("p (h q) -> p h q", h=4), 0.125,
                                biasT, biasT[:, kb, g * 4:(g + 1) * 4, :], ALU.mult, ALU.add)
                            ACTF(pTt, pTt[:, kb, :], e_, e_[:], ACT.Exp)
                        return pTt, kbs

                    def a_pv(j, g, pTt, kbs, oa, rec):
                        po = ps[4 + g]
                        for hh in range(4):
                            h = g * 4 + hh
                            for i, kb in enumerate(kbs):
                                jk = j - 4 + kb
                                MM(po, po[:, hh * 66:(hh + 1) * 66], pTt, pTt[:, kb, hh * P:(hh + 1) * P],
                                   vaug, vaug[:, jk, h, :], i == 0, i == len(kbs) - 1)
                        pov = po[:, 0:264].rearrange("p (h d) -> p h d", h=4)
                        S.op("dve", lambda e, rec=rec, pov=pov, g=g: e.reciprocal(
                            out=rec[:, g * 4:(g + 1) * 4].unsqueeze(2), in_=pov[:, :, 64:65]), [po], [rec])
                        for hh in range(4):
                            h = g * 4 + hh
                            TS("dve", oa, oa[:, h * 64:(h + 1) * 64], po, po[:, hh * 66:hh * 66 + 64],
                               rec[:, h:h + 1], None, ALU.mult, reads=[rec])

                    def a_fin(j, oa):
                        tsl = slice(j * P, (j + 1) * P)
                        pX = ps[6 + j % 2]
                        for c in range(4):
                            TR(pX, pX[:, c * P:(c + 1) * P], oa, oa[:, c * P:(c + 1) * P], *ident)
                        CP("act", oT["att"], oT["att"][:, :, tsl], pX, pX[:].rearrange("p (c t) -> p c t", c=4))

                    groups = [(j, g) for j in range(n_att) for g in range(2)]
                    nxt = a_scores(*groups[0]) if groups else None
                    oa = rec = None
                    for n, (j, g) in enumerate(groups):
                        cur = nxt
                        if n + 1 < len(groups):
                            nxt = a_scores(*groups[n + 1])
                        if g == 0:
                            oa, rec = oap.get(), recp.get()
                        a_pv(j, g, cur[0], cur[1], oa, rec)
                        if g == 1:
                            a_fin(j, oa)
                for i_, k_ in enumerate(("ret", "conv", "att")):
                    DUMP(1 + i_, oT[k_], oT[k_][:].rearrange("p k t -> p (k t)"), 8192)
                S.barrier()
                if dbg and dbg.startswith("att"):
                    return "stop"
                with ExitStack() as st2:
                    mT = [S.sb(f"mT{q}", [P, KC, 512], BF16, st2) for q in range(NQ)]
                    glp = Pool("s4_gl", [P, KC, 3, P], BF16, 2, st2)
                    wop = Pool("s4_wo", [P, 4, 3, P], BF16, 2, st2)
                    gtp = Pool("s4_g", [P, 512], F32, 3, st2)
                    tmp = Pool("s4_t", [P, 512], F32, 2, st2)
                    macc = Pool("s4_m", [P, 512], F32, 2, st2)
                    outs_w = (w_ro_h[l], w_co_h[l], w_ao_h[l])
                    keys = ("ret", "conv", "att")
                    def s4_unit(c):
                        def ld():
                            gw = glp.get()
                            ow = wop.get()
                            for b in range(3):
                                S.dma("pool", gw, gw[:, :, b, :], wts_d, wcols(w_in, C_GL + b * D + c * P, P))
                                S.dma("pool", ow, ow[:, :, b, :], wts_d, wcols(outs_w[b], c * P, P))
                            return (gw, ow)

                        def cp(w):
                            gw, ow = w
                            for q in range(NQ):
                                qs = slice(q * 512, (q + 1) * 512)
                                m = macc.get()
                                for b in range(3):
                                    pg, py = ps[2 * (b % 2)], ps[2 * (b % 2) + 1]
                                    for kc in range(KC):
                                        MM(pg, pg[:], gw, gw[:, kc, b, :], hT[q], hT[q][:, kc, :], kc == 0, kc == KC - 1)
                                    for kc in range(4):
                                        MM(py, py[:], ow, ow[:, kc, b, :], oT[keys[b]], oT[keys[b]][:, kc, qs], kc == 0, kc == 3)
                                    gt = gtp.get()
                                    ACTF(gt, gt[:], pg, pg[:], ACT.Sigmoid,
                                         bias=pp[:, PP_BG + b * 8 + c:PP_BG + b * 8 + c + 1], reads=[pp])
                                    if b == 0:
                                        TT("dve", m, m[:], py, py[:], gt, gt[:], ALU.mult)
                                    elif b == 1:
                                        t = tmp.get()
                                        TT("dve", t, t[:], py, py[:], gt, gt[:], ALU.mult)
                                        TT("pool", m, m[:], m, m[:], t, t[:], ALU.add)
                                    else:
                                        t = tmp.get()
                                        TT("dve", t, t[:], py, py[:], gt, gt[:], ALU.mult)
                                        TT("pool", mT[q], mT[q][:, c, :], m, m[:], t, t[:], ALU.add)
                        return (ld, cp)

                    wo = S.sb("s4_wout", [P, KC, D], BF16, st2)
                    gate1 = load_mod_bc(st2, l, s, 2, "gate1")
                    xs_pool = Pool("s4_xs", [P, D], F32, 4, st2)

                    def wo_ld():
                        S.dma("pool", wo, wo[:], wts_d, wcols(w_out_h[l], 0, D))
                        return wo

                    def wo_cp(_w):
                        DUMP(4, mT[0], mT[0][:].rearrange("p k t -> p (k t)"), 4096)
                        s4_out()

                    def s4_load(j):
                        xs = xs_pool.get()
                        if first:
                            S.dma("sp", xs, xs[:], x_d, x_h[s, j * P:(j + 1) * P, :])
                        else:
                            S.dma("sp", xs, xs[:], xo_d[s][j], out_h[s, j * P:(j + 1) * P, :])
                        return xs

                    def s4_out():
                        xq = [s4_load(0), s4_load(1)]
                        for j in range(NT):
                            q, jj = divmod(j, 4)
                            xs = xq.pop(0)
                            if j + 2 < NT:
                                xq.append(s4_load(j + 2))
                            for half in range(2):
                                pt = ps[4 + (2 * j + half) % 4]
                                hsl = slice(half * 512, (half + 1) * 512)
                                for kc in range(KC):
                                    MM(pt, pt[:], mT[q], mT[q][:, kc, jj * P:(jj + 1) * P], wo, wo[:, kc, hsl], kc == 0, kc == KC - 1)
                                t = tmp.get()
                                TT("dve", t, t[:], pt, pt[:], gate1, gate1[:, hsl], ALU.mult)
                                TT("pool", xs, xs[:, hsl], xs, xs[:, hsl], t, t[:], ALU.add)
                            S.dma("act", xo_d[s][j], out_h[s, j * P:(j + 1) * P, :], xs, xs[:], sem_t=xs)
                    run_units([s4_unit(c) for c in range(KC)] + [(wo_ld, wo_cp)])
                S.barrier()
            S.barrier()

        def moe(s, l):
            with ExitStack() as st:
                hT = [S.sb(f"hT{q}", [P, KC, 512], BF16, st) for q in range(NQ)]
                acc = S.sb("m_acc", [P, NT, D], F32, st)
                wge = S.sb("m_wge", [P, NT, 32], F32, st)
                upp = Pool("m_wup", [P, KC, 512], BF16, 2, st)
                dnp = Pool("m_wdn", [P, 2, D], BF16, 2, st)

                def e_ld(ge):
                    g, e_ = divmod(ge, NE)
                    wu = upp.get()
                    wd = dnp.get()
                    S.dma("pool", wu, wu[:], wts_d, wcols(w_up_h[l, g, e_], 0, 512))
                    S.dma("pool", wd, wd[:], wts_d, w_dn_h[l, g, e_].rearrange("(fc p) n -> p fc n", p=P))
                    return (wu, wd)

                e_nxt = e_ld(0) if n_experts > 0 else None
                with ExitStack() as st1:
                    gmod = load_mod_bc(st1, l, s, 4, "gmod2")
                    shift = load_mod_bc(st1, l, s, 3, "shift2")
                    wr = S.sb("m_wr", [P, KC, 36], F32, st1)
                    S.dma("sp", wr, wr[:], wts_d, wr_h[l])
                    brow = S.sb("m_brow", [P, 36], F32, st1)
                    S.dma("sp", brow, brow[:], wts_d, brow_h[l:l + 1, :].partition_broadcast(P))
                    whi = S.sb("m_whi", [P, KC, 36], BF16, st1)
                    wlo = S.sb("m_wlo", [P, KC, 36], BF16, st1)
                    CP("dve", whi, whi[:], wr, wr[:])
                    TT("dve", wlo, wlo[:], wr, wr[:], whi, whi[:], ALU.subtract)
                    h2p = Pool("m_h2lo", [P, KC, P], BF16, 2, st1)
                    RW_ = 100
                    r = S.sb("m_r", [P, NT, RW_], F32, st1)
                    MSET("dve", r, r[:], 0.0)

                    def router(j, pa, pb):
                        q, jj = divmod(j, 4)
                        tsl = slice(jj * P, (jj + 1) * P)
                        h2 = h2p.get()
                        TT("dve", h2, h2[:, 0:4, :], pa, pa[:].rearrange("p (k t) -> p k t", k=4),
                           hT[q], hT[q][:, 0:4, tsl], ALU.subtract)
                        TT("dve", h2, h2[:, 4:8, :], pb, pb[:].rearrange("p (k t) -> p k t", k=4),
                           hT[q], hT[q][:, 4:8, tsl], ALU.subtract)
                        pl = ps[4 + j % 2]
                        for kc in range(KC):
                            MM(pl, pl[:, 0:36], hT[q], hT[q][:, kc, tsl], whi, whi[:, kc, :], kc == 0, False)
                            MM(pl, pl[:, 0:36], hT[q], hT[q][:, kc, tsl], wlo, wlo[:, kc, :], False, False)
                            MM(pl, pl[:, 0:36], h2, h2[:, kc, :], whi, whi[:, kc, :], False, kc == KC - 1)
                        TT("dve", r, r[:, j, 0:36], pl, pl[:, 0:36], brow, brow[:], ALU.add)

                    def router_batched():
                        R3 = lambda a_, b_: r[:, :, a_:b_]
                        BC = lambda a_, n_: r[:, :, a_:a_ + 1].broadcast_to([P, NT, n_])
                        C1 = lambda a_: r[:, :, a_:a_ + 1].rearrange("p j o -> p (j o)")
                        RED = lambda dst, a_, b_, op: S.op(
                            "dve", lambda e: e.tensor_reduce(out=C1(dst), in_=R3(a_, b_), axis=AX.X, op=op), [r], [r])
                        T3 = lambda o_, a_, b_, op: TT("dve", r, o_, r, a_, r, b_, op)
                        RED(36, 0, 4, ALU.max)
                        T3(R3(37, 41), R3(0, 4), BC(36, 4), ALU.is_ge)
                        T3(R3(41, 45), R3(0, 4), BC(36, 4), ALU.subtract)
                        ACTF(r, R3(41, 45), r, R3(41, 45), ACT.Exp)
                        RED(45, 41, 45, ALU.add)
                        S.op("dve", lambda e: e.reciprocal(out=C1(45), in_=C1(45)), [r], [r])
                        T3(R3(46, 54), R3(4, 12), BC(37, 8), ALU.mult)
                        for g in range(1, 4):
                            T3(R3(91, 99), R3(4 + 8 * g, 12 + 8 * g), BC(37 + g, 8), ALU.mult)
                            T3(R3(46, 54), R3(46, 54), R3(91, 99), ALU.add)
                        RED(54, 46, 54, ALU.max)
                        T3(R3(55, 63), R3(46, 54), BC(54, 8), ALU.is_ge)
                        STT("dve", r, R3(63, 71), r, R3(55, 63), NEG, r, R3(46, 54), ALU.mult, ALU.add)
                        RED(71, 63, 71, ALU.max)
                        T3(R3(72, 80), R3(63, 71), BC(71, 8), ALU.is_ge)
                        T3(R3(80, 81), R3(71, 72), R3(54, 55), ALU.subtract)
                        ACTF(r, R3(82, 83), r, R3(80, 81), ACT.Sigmoid)
                        TS("dve", r, R3(81, 82), r, R3(82, 83), -1.0, 1.0, ALU.mult, ALU.add)
                        T3(R3(81, 82), R3(81, 82), R3(45, 46), ALU.mult)
                        T3(R3(82, 83), R3(82, 83), R3(45, 46), ALU.mult)
                        T3(R3(83, 91), R3(55, 63), BC(81, 8), ALU.mult)
                        T3(R3(91, 99), R3(72, 80), BC(82, 8), ALU.mult)
                        T3(R3(83, 91), R3(83, 91), R3(91, 99), ALU.add)
                        for g in range(4):
                            TT("dve", wge, wge[:, :, g * 8:(g + 1) * 8], r, R3(83, 91), r, BC(37 + g, 8), ALU.mult)

                    norm_and_transpose(st1, l, s, False, gmod, shift, hT, h2f_cb=(None if dbg == "m_norouter" else router))
                    if dbg != "m_norouter":
                        router_batched()
                S.barrier()
                with ExitStack() as st2:
                    sap = Pool("m_sa", [P, 512], F32, 2, st2)
                    actp = Pool("m_act", [P, 2, 512], BF16, 2, st2)
                    dcount = 0
                    for ge in range(n_experts):
                        g, e_ = divmod(ge, NE)
                        wu, wd = e_nxt
                        e_nxt = e_ld(ge + 1) if ge + 1 < n_experts else None
                        for q in range(NQ):
                            for fc in range(4):
                                pu = ps[fc]
                                for kc in range(KC):
                                    MM(pu, pu[:], wu, wu[:, kc, fc * P:(fc + 1) * P], hT[q], hT[q][:, kc, :], kc == 0, kc == KC - 1)
                            at = actp.get()
                            for fc in range(2):
                                sa = sap.get()
                                ACTF(sa, sa[:], ps[fc], ps[fc][:], ACT.Silu)
                                TT("dve", at, at[:, fc, :], sa, sa[:], ps[fc + 2], ps[fc + 2][:], ALU.mult)
                            for jj in range(4):
                                j = q * 4 + jj
                                for half in range(2):
                                    pd = ps[4 + dcount % 4]
                                    dcount += 1
                                    hsl = slice(half * 512, (half + 1) * 512)
                                    for fc in range(2):
                                        MM(pd, pd[:], at, at[:, fc, jj * P:(jj + 1) * P], wd, wd[:, fc, hsl], fc == 0, fc == 1)
                                    if ge == 0:
                                        TS("dve", acc, acc[:, j, hsl], pd, pd[:], wge[:, j, ge:ge + 1], None, ALU.mult, reads=[wge])
                                    else:
                                        STT("dve", acc, acc[:, j, hsl], pd, pd[:], wge[:, j, ge:ge + 1], acc, acc[:, j, hsl],
                                            ALU.mult, ALU.add, reads=[wge])
                    gate2 = load_mod_bc(st2, l, s, 5, "gate2")
                    xs_pool = Pool("m_xs", [P, D], F32, 4, st2)
                    toks = []

                    def m_load(j):
                        xs = xs_pool.get()
                        S.dma("sp", xs, xs[:], xo_d[s][j], out_h[s, j * P:(j + 1) * P, :])
                        return xs

                    xq = [m_load(0), m_load(1)]
                    for j in range(NT):
                        xs = xq.pop(0)
                        if j + 2 < NT:
                            xq.append(m_load(j + 2))
                        TT("dve", acc, acc[:, j, :], acc, acc[:, j, :], gate2, gate2[:], ALU.mult)
                        TT("pool", xs, xs[:], xs, xs[:], acc, acc[:, j, :], ALU.add)
                        toks.append(S.dma("act", xo_d[s][j], out_h[s, j * P:(j + 1) * P, :], xs, xs[:], sem_t=xs))
                S.barrier()
                return toks

        identb = S.sb("identb", [P, P], BF16)
        CP("dve", identb, identb[:], cst, cst[:, CS_ID:CS_ID + P])

        final = []
        try:
            if dbg and dbg.startswith("pro"):
                raise _Stop()
            for s in range(n_seq):
                for l in range(n_layers):
                    if mixer(s, l) == "stop":
                        raise _Stop()
                    final = moe(s, l) if n_experts > 0 else []
        except _Stop:
            S.barrier()
        S.emit(final_toks=S.dma_toks + final + dump_toks)
    return nc


def _constants():
    cst = np.zeros((P, NCST), np.float32)
    cst[:, CS_ID:CS_ID + P] = np.eye(P, dtype=np.float32)
    m = np.arange(P)[:, None].astype(np.float64)
    c = np.arange(P)[None, :].astype(np.float64)
    for h in range(4):
        gamma = 1.0 - 2.0 ** (-5 - h)
        mask = np.where(c >= m, gamma ** np.maximum(c - m, 0.0), 0.0) * (128.0 ** -0.5)
        cst[:, CS_MASK + h * P:CS_MASK + (h + 1) * P] = mask
        cst[:, CS_QDEC + h * P:CS_QDEC + (h + 1) * P] = (gamma ** (c + 1.0))
        cst[:, CS_KDEC + h] = (gamma ** (127.0 - m[:, 0])) * (128.0 ** -0.5)
    half = 64
    inv = (np.float32(10000.0) ** (-np.arange(half, dtype=np.float32) / np.float32(half))).astype(np.float32)
    cst[:, CS_INV] = np.tile(inv, 2)
    cst[0:64, CS_SGN] = -1.0
    cst[64:128, CS_SGN] = 1.0
    cst[0:64, CS_MLO] = 1.0
    cst[64:128, CS_MHI] = 1.0
    return cst


def _layout_params(inp):
    f32 = np.float32
    pp = np.zeros((L, P, NPP), f32)
    cw = np.asarray(inp["conv_w"], f32)[:, :, 0, :]
    pp[:, :, PP_CW:PP_CW + 124] = cw.reshape(L, 31, 4, P).transpose(0, 3, 2, 1).reshape(L, P, 124)
    pp[:, :, PP_CB:PP_CB + 4] = np.asarray(inp["conv_b"], f32).reshape(L, 4, P).transpose(0, 2, 1)
    pp[:, :, PP_LG:PP_LG + 4] = np.asarray(inp["conv_ln_g"], f32).reshape(L, 4, P).transpose(0, 2, 1)
    pp[:, :, PP_LB:PP_LB + 4] = np.asarray(inp["conv_ln_b"], f32).reshape(L, 4, P).transpose(0, 2, 1)
    pp[:, :, PP_QG] = np.tile(np.asarray(inp["att_q_gain"], f32), (1, 2))
    pp[:, :, PP_KG] = np.tile(np.asarray(inp["att_k_gain"], f32), (1, 2))
    pp[:, :, PP_BG:PP_BG + 24] = np.asarray(inp["b_gate"], f32).reshape(L, 24, P).transpose(0, 2, 1)
    wr = np.concatenate([np.asarray(inp["w_group"], f32), np.asarray(inp["w_inner"], f32)], axis=-1)
    wr = np.ascontiguousarray(wr.reshape(L, KC, P, 36).transpose(0, 2, 1, 3))
    brow = np.ascontiguousarray(np.concatenate([np.asarray(inp["b_group"], f32), np.asarray(inp["b_inner"], f32)], axis=-1))
    k = np.arange(P)[:, None, None]
    kb = np.arange(5)[None, :, None]
    q = np.arange(P)[None, None, :]
    idx = np.clip(q - k + 128 * (4 - kb), -256, 256) + 256
    tab = np.asarray(inp["att_rel_bias"], f32)
    bx = tab[:, :, idx]
    biasx = np.ascontiguousarray(bx.transpose(0, 2, 3, 1, 4))
    return pp, wr, brow, biasx


_PROG = {}


def kernel(**inp):
    n = 8
    f32 = np.float32
    x = np.asarray(inp["x"], f32)
    c = np.asarray(inp["c"], f32)
    pos = np.asarray(inp["positions"], np.int32)
    pp, wr, brow, biasx = _layout_params(inp)
    cst = _constants()
    shared = dict(
        w_ada=np.asarray(inp["w_ada"], f32), b_ada=np.asarray(inp["b_ada"], f32),
        g_mix=np.asarray(inp["g_mix"], f32), g_ffn=np.asarray(inp["g_ffn"], f32),
        w_in=np.asarray(inp["w_in"], f32), ret_gn=np.asarray(inp["ret_gn"], f32),
        w_ret_out=np.asarray(inp["w_ret_out"], f32), w_conv_out=np.asarray(inp["w_conv_out"], f32),
        w_att_out=np.asarray(inp["w_att_out"], f32), w_out=np.asarray(inp["w_out"], f32),
        w_up=np.asarray(inp["w_up"], f32), w_down=np.asarray(inp["w_down"], f32),
        pp=pp, wr=wr, brow=brow, biasx=biasx, cst=cst)
    if "nc" not in _PROG:
        _PROG["nc"] = build_program()
    nc = _PROG["nc"]
    in_maps = []
    for i in range(n):
        sl = slice(NSEQ * i, NSEQ * (i + 1))
        m = dict(shared)
        m["x"] = np.ascontiguousarray(x[sl])
        m["pos"] = np.ascontiguousarray(pos[sl])
        m["cT"] = np.ascontiguousarray(c[sl].reshape(NSEQ, KC, P).transpose(2, 1, 0))
        in_maps.append(m)
    res = run_bass_kernel_spmd(nc, in_maps, core_ids=list(range(n)))
    return np.concatenate([np.asarray(r["out"], f32) for r in res.results], axis=0)
```

```python
import math
import numpy as np
from concourse.bass_utils import run_bass_kernel_spmd

import numpy as np
import concourse.bass as bass
import concourse.mybir as mybir

F32 = mybir.dt.float32
BF16 = mybir.dt.bfloat16
I32 = mybir.dt.int32
ALU = mybir.AluOpType
ACT = mybir.ActivationFunctionType
AX = mybir.AxisListType

SAME_ENGINE_SYNC = False
SMALL_N = 512


class Op:
    __slots__ = ("eng", "fn", "deps", "signal", "semval", "dma_inc", "idx", "small")

    def __init__(self, eng, fn, deps, dma_inc=None):
        self.eng = eng
        self.fn = fn
        self.deps = deps
        self.signal = False
        self.semval = None
        self.dma_inc = dma_inc
        self.idx = None
        self.small = False


class DmaTok:
    __slots__ = ("sem", "val", "eng")

    def __init__(self, sem, val):
        self.sem = sem
        self.val = val
        self.eng = None


class T:
    def __init__(self, h, name=""):
        self.h = h
        self.name = name
        self.last_w = None
        self.readers = []
        self.dsem = None
        self.dcount = 0

    def __getitem__(self, k):
        return self.h[k]


class TView:
    def __init__(self, base, ap):
        self.__dict__["base"] = base
        self.__dict__["ap"] = ap

    def __getitem__(self, k):
        return self.ap[k]

    def __getattr__(self, k):
        return getattr(self.base, k)

    def __setattr__(self, k, v):
        setattr(self.base, k, v)


class Sched:
    ENGS = ("pe", "act", "dve", "pool", "sp")

    def __init__(self, nc):
        self.nc = nc
        self.ops = {e: [] for e in self.ENGS}
        self.sems = {}
        self.stack = None
        self.dma_sems = []
        self.free_dma_sems = []
        self.dma_toks = []
        self.uid = 0

    def set_stack(self, stack):
        self.stack = stack

    def sem(self, name):
        return self.stack.enter_context(self.nc.semaphore(name))

    def sb(self, name, shape, dtype, stack=None):
        st = stack or self.stack
        self.uid += 1
        name = f"{name}_{self.uid}"
        h = st.enter_context(self.nc.sbuf_tensor(name, list(shape), dtype))
        t = T(h, name)
        if st is not self.stack:
            st.callback(self._retire, t)
        return t

    def _retire(self, t):
        if t.dsem is not None:
            self.free_dma_sems.append((t.dsem, t.dcount))
            t.dsem = None

    def ps(self, name, shape, dtype=F32, stack=None):
        st = stack or self.stack
        h = st.enter_context(self.nc.psum_tensor(name, list(shape), dtype))
        return T(h, name)

    def dram(self, h, name=""):
        return T(h, name)

    def _deps(self, reads, writes):
        deps = []
        for t in reads:
            if t.last_w is not None:
                deps.append(t.last_w)
        for t in writes:
            if t.last_w is not None:
                deps.append(t.last_w)
            deps.extend(t.readers)
        return deps

    def _commit(self, tok, reads, writes):
        for t in reads:
            t.readers.append(tok)
            if len(t.readers) > 64:
                t.readers = self._compact(t.readers)
        for t in writes:
            t.last_w = tok
            t.readers = []

    @staticmethod
    def _compact(toks):
        last = {}
        for tk in toks:
            key = (tk.eng if isinstance(tk, Op) else id(tk.sem))
            last[key] = tk
        return list(last.values())

    def op(self, eng, fn, reads=(), writes=(), small=True):
        o = Op(eng, fn, self._deps(reads, writes))
        o.small = bool(small) and eng != "pe"
        o.idx = len(self.ops[eng])
        self.ops[eng].append(o)
        self._commit(o, reads, writes)
        return o

    def dma(self, q, out_t, out_ap, in_t, in_ap, sem_t=None, **kw):
        st = sem_t or out_t
        if st.dsem is None:
            if self.free_dma_sems:
                st.dsem, st.dcount = self.free_dma_sems.pop()
            else:
                st.dsem = self.sem("d_" + st.name)
                st.dcount = 0
        st.dcount += 16
        tok = DmaTok(st.dsem, st.dcount)
        deps = self._deps([in_t], [out_t])
        sem = st.dsem

        def fn(e, out_ap=out_ap, in_ap=in_ap, kw=kw):
            return e.dma_start(out=out_ap, in_=in_ap, **kw)

        o = Op(q, fn, deps, dma_inc=sem)
        o.idx = len(self.ops[q])
        self.ops[q].append(o)
        self._commit(tok, [in_t], [out_t])
        self.dma_toks.append(tok)
        return tok

    def barrier(self):
        lasts = []
        for e in self.ENGS:
            for o in reversed(self.ops[e]):
                if o.fn is not None and o.dma_inc is None:
                    lasts.append(o)
                    break
        toks = list(self.dma_toks)
        self.dma_toks = []
        for e in self.ENGS:
            deps = [o for o in lasts if o.eng != e] + toks
            if not deps:
                continue
            o = Op(e, None, deps)
            o.idx = len(self.ops[e])
            self.ops[e].append(o)

    def finalize(self):
        for e in self.ENGS:
            for o in self.ops[e]:
                for d in o.deps:
                    if isinstance(d, Op):
                        if d.eng != o.eng or SAME_ENGINE_SYNC or d.small:
                            d.signal = True
        self.counts = {}
        for e in self.ENGS:
            c = 0
            for o in self.ops[e]:
                if o.signal:
                    c += 1
                    o.semval = c
            self.counts[e] = c
            if c > 0 or True:
                self.sems[e] = self.sem("eng_" + e)

    def replay(self, e, handle):
        seen = {}
        nc = self.nc
        for o in self.ops[e]:
            waits = {}
            for d in o.deps:
                if isinstance(d, Op):
                    if d.eng == e and not (SAME_ENGINE_SYNC or d.small):
                        continue
                    key = ("e", d.eng)
                    sem = self.sems[d.eng]
                    val = d.semval
                else:
                    key = ("d", id(d.sem))
                    sem = d.sem
                    val = d.val
                if seen.get(key, 0) >= val:
                    continue
                if key not in waits or waits[key][1] < val:
                    waits[key] = (sem, val)
            for key, (sem, val) in waits.items():
                handle.wait_ge(sem, val)
                seen[key] = val
            if o.fn is None:
                continue
            ins = o.fn(handle)
            if o.dma_inc is not None:
                ins.then_inc(o.dma_inc, 16)
            elif o.signal:
                ins.then_inc(self.sems[e], 1)

    def emit(self, final_toks=()):
        nc = self.nc
        self.finalize()
        with nc.Block() as block:
            @block.tensor
            def _(h):
                self.replay("pe", h)

            @block.scalar
            def _(h):
                self.replay("act", h)

            @block.vector
            def _(h):
                self.replay("dve", h)

            @block.gpsimd
            def _(h):
                self.replay("pool", h)

            @block.sync
            def _(h):
                self.replay("sp", h)
                for tk in final_toks:
                    h.wait_ge(tk.sem, tk.val)

from contextlib import ExitStack

P = 128
SEQ = 2048
D = 1024
KC = 8
NT = 16
NQ = 4
L = 2
NSEQ = 2
IN_COLS = 7680
EPS = 1e-6
NEG = -1e30
C_RQ, C_RK, C_RV, C_RG = 0, 512, 1024, 1536
C_CU = 2048
C_AQ, C_AK, C_AV = 3072, 3584, 4096
C_GL = 4608
NG, NE = 4, 8
PP_CW = 0
PP_CB = 124
PP_LG = 128
PP_LB = 132
PP_QG = 136
PP_KG = 137
PP_BG = 138
NPP = 162
CS_ID = 0
CS_MASK = 128
CS_QDEC = 640
CS_KDEC = 1152
CS_INV = 1156
CS_SGN = 1157
CS_MLO = 1158
CS_MHI = 1159
NCST = 1160
RET_CD = [float((1.0 - 2.0 ** (-5 - h)) ** 128) for h in range(4)]


def build_program(n_layers=L, n_seq=NSEQ, n_experts=NG * NE, dbg=None, dump=False):
    nc = bass.Bass("TRN2", target_bir_lowering=False)
    dump_toks = []

    def din(name, shape, dt=F32):
        return nc.dram_tensor(name, list(shape), dt, kind="ExternalInput")

    x_h = din("x", [NSEQ, SEQ, D])
    pos_h = din("pos", [NSEQ, SEQ], I32)
    cT_h = din("cT", [P, KC, NSEQ])
    w_ada_h = din("w_ada", [L, D, 6 * D])
    b_ada_h = din("b_ada", [L, 6 * D])
    g_mix_h = din("g_mix", [L, D])
    g_ffn_h = din("g_ffn", [L, D])
    w_in_h = din("w_in", [L, D, IN_COLS])
    ret_gn_h = din("ret_gn", [L, 512])
    w_ro_h = din("w_ret_out", [L, 512, D])
    w_co_h = din("w_conv_out", [L, 512, D])
    w_ao_h = din("w_att_out", [L, 512, D])
    w_out_h = din("w_out", [L, D, D])
    w_up_h = din("w_up", [L, NG, NE, D, 512])
    w_dn_h = din("w_down", [L, NG, NE, 256, D])
    pp_h = din("pp", [L, P, NPP])
    wr_h = din("wr", [L, P, KC, 36])
    brow_h = din("brow", [L, 36])
    biasx_h = din("biasx", [L, P, 5, 8, P])
    cst_h = din("cst", [P, NCST])
    out_h = nc.dram_tensor("out", [NSEQ, SEQ, D], F32, kind="ExternalOutput")
    modd_h = nc.dram_tensor("modd", [L, NSEQ, 6 * D], F32)
    dbg_h = nc.dram_tensor("dbg", [P, 6, 8192], F32, kind="ExternalOutput") if dump else None

    top = ExitStack()
    with top:
        S = Sched(nc)
        S.set_stack(top)

        def MM(out_t, out_ap, lt, lap, rt, rap, start=True, stop=True):
            S.op("pe", lambda e: e.matmul(out_ap, lhsT=lap, rhs=rap, start=start, stop=stop),
                 [lt, rt], [out_t])

        def TR(out_t, out_ap, in_t, in_ap, id_t, id_ap):
            S.op("pe", lambda e: e.transpose(out=out_ap, in_=in_ap, identity=id_ap), [in_t, id_t], [out_t])

        def ACTF(out_t, out_ap, in_t, in_ap, func, bias=None, scale=None, accum=None, reads=()):
            kw = {}
            if bias is not None:
                kw["bias"] = bias
            if scale is not None:
                kw["scale"] = scale
            wr = [out_t]
            if accum is not None:
                kw["accum_out"] = accum[1]
                wr.append(accum[0])
            S.op("act", lambda e: e.activation(out=out_ap, in_=in_ap, func=func, **kw),
                 [in_t] + list(reads), wr, small=(accum is not None or out_ap.free_size() < SMALL_N))

        def TT(eng, out_t, out_ap, a_t, a_ap, b_t, b_ap, op):
            S.op(eng, lambda e: e.tensor_tensor(out=out_ap, in0=a_ap, in1=b_ap, op=op), [a_t, b_t], [out_t],
                 small=out_ap.free_size() < SMALL_N)

        def TS(eng, out_t, out_ap, a_t, a_ap, s1, s2, op0, op1=None, reads=()):
            if op1 is None:
                S.op(eng, lambda e: e.tensor_scalar(out=out_ap, in0=a_ap, scalar1=s1, scalar2=None, op0=op0),
                     [a_t] + list(reads), [out_t], small=out_ap.free_size() < SMALL_N)
            else:
                S.op(eng, lambda e: e.tensor_scalar(out=out_ap, in0=a_ap, scalar1=s1, scalar2=s2, op0=op0, op1=op1),
                     [a_t] + list(reads), [out_t], small=out_ap.free_size() < SMALL_N)

        def STT(eng, out_t, out_ap, a_t, a_ap, sc, b_t, b_ap, op0, op1, reads=()):
            S.op(eng, lambda e: e.scalar_tensor_tensor(out=out_ap, in0=a_ap, scalar=sc, in1=b_ap, op0=op0, op1=op1),
                 [a_t, b_t] + list(reads), [out_t], small=out_ap.free_size() < SMALL_N)

        def CP(eng, out_t, out_ap, in_t, in_ap):
            if eng == "act":
                S.op("act", lambda e: e.copy(out=out_ap, in_=in_ap), [in_t], [out_t], small=out_ap.free_size() < SMALL_N)
            else:
                S.op(eng, lambda e: e.tensor_copy(out=out_ap, in_=in_ap), [in_t], [out_t], small=out_ap.free_size() < SMALL_N)

        def MSET(eng, t, ap, val):
            S.op(eng, lambda e: e.memset(ap, val), [], [t], small=ap.free_size() < SMALL_N)

        def RSTD(t, ap, n_is_one_col=True):
            S.op("dve", lambda e: e.reciprocal(out=ap, in_=ap), [t], [t])
            ACTF(t, ap, t, ap, ACT.Sqrt)

        class Pool:
            def __init__(self, name, shape, dt, n, stack):
                self.tiles = [S.sb(f"{name}{i}", shape, dt, stack) for i in range(n)]
                self.i = 0

            def get(self):
                t = self.tiles[self.i % len(self.tiles)]
                self.i += 1
                return t

        def run_units(units):
            nxt = units[0][0]() if units[0][0] is not None else None
            for i, (ld, cp) in enumerate(units):
                cur, nxt = nxt, None
                if i + 1 < len(units) and units[i + 1][0] is not None:
                    nxt = units[i + 1][0]()
                cp(cur)

        def wcols(ap2d, c0, n):
            return ap2d.rearrange("(kc p) n -> p kc n", p=P)[:, :, c0:c0 + n]

        DR = lambda h, name: S.dram(h, name)
        dbg_d = DR(dbg_h, "dbg") if dump else None

        def DUMP(slot, t, ap, ncols):
            if dump:
                dump_toks.append(S.dma("pool", dbg_d, dbg_h[:, slot, 0:ncols], t, ap, sem_t=t))
        x_d = DR(x_h, "x")
        pos_d = DR(pos_h, "pos")
        wts_d = DR(w_in_h, "weights")
        modd_d = DR(modd_h, "modd")
        xo_d = [[DR(out_h, f"xo{s}_{j}") for j in range(NT)] for s in range(NSEQ)]

        cst = S.sb("cst", [P, NCST], F32)
        S.dma("sp", cst, cst[:], wts_d, cst_h[:, :])
        ident = (cst, cst[:, CS_ID:CS_ID + P])
        ps = [S.ps(f"ps{i}", [P, 512]) for i in range(8)]
        blk64 = S.sb("blk64", [P, P], BF16)
        MSET("dve", blk64, blk64[:], 0.0)
        MSET("dve", blk64, blk64[0:64, 0:64], 1.0 / 64)
        MSET("dve", blk64, blk64[64:128, 64:128], 1.0 / 64)
        onesln = S.sb("onesln", [P, P], F32)
        MSET("dve", onesln, onesln[:], 1.0 / 512)

        with ExitStack() as st:
          if dbg != "pro0":
              cT = S.sb("cT", [P, KC, NSEQ], F32, st)
              scT = S.sb("scT", [P, KC, NSEQ], BF16, st)
              S.dma("sp", cT, cT[:], wts_d, cT_h[:, :, :])
              ACTF(scT, scT[:], cT, cT[:], ACT.Silu)
              modrow = S.sb("modrow", [NSEQ, 6 * D], F32, st)
              brow2 = S.sb("brow2", [NSEQ, 6 * D], F32, st)
              grow = S.sb("grow", [NSEQ, 2, D], F32, st)
              wpool = Pool("wada", [P, KC, 512], BF16, 2, st)
              for l in range(n_layers):
                  S.dma("sp", brow2, brow2[:], wts_d, b_ada_h[l:l + 1, :].partition_broadcast(NSEQ))
                  S.dma("sp", grow, grow[:, 0, :], wts_d, g_mix_h[l:l + 1, :].partition_broadcast(NSEQ))
                  S.dma("sp", grow, grow[:, 1, :], wts_d, g_ffn_h[l:l + 1, :].partition_broadcast(NSEQ))
                  if dbg == "pro1":
                      break
                  for cb in range(12):
                      wb = wpool.get()
                      S.dma("pool", wb, wb[:], wts_d, wcols(w_ada_h[l], cb * 512, 512))
                      pt = ps[cb % 2]
                      for kc in range(KC):
                          MM(pt, pt[0:NSEQ, :], scT, scT[:, kc, :], wb, wb[:, kc, :], kc == 0, kc == KC - 1)
                      TT("dve", modrow, modrow[:, cb * 512:(cb + 1) * 512], pt, pt[0:NSEQ, :],
                         brow2, brow2[:, cb * 512:(cb + 1) * 512], ALU.add)
                  if dbg == "pro2":
                      break
                  for i, sl in ((0, 1), (1, 4)):
                      TS("dve", modrow, modrow[:, sl * D:(sl + 1) * D], modrow, modrow[:, sl * D:(sl + 1) * D], 1.0, None, ALU.add)
                      TT("dve", modrow, modrow[:, sl * D:(sl + 1) * D], modrow, modrow[:, sl * D:(sl + 1) * D],
                         grow, grow[:, i, :], ALU.mult)
                  if dbg == "pro3":
                      break
                  S.dma("sp", modd_d, modd_h[l], modrow, modrow[:], sem_t=modrow)
        S.barrier()

        class _Stop(Exception):
            pass

        def load_mod_bc(st, l, s, idx, name):
            t = S.sb(name, [P, D], F32, st)
            S.dma("sp", t, t[:], modd_d, modd_h[l, s:s + 1, idx * D:(idx + 1) * D].partition_broadcast(P))
            return t

        def norm_and_transpose(st, l, s, first_layer_input, gmod, shift, hT, h2f_cb=None):
            xs_pool = Pool("xs", [P, D], F32, 3, st)
            hf_pool = Pool("hf", [P, D], F32, 3, st)
            junk = S.sb("junk", [P, D], BF16, st)
            ssq = Pool("ssq", [P, 8], F32, 3, st)
            def phase_a(j):
                xs = xs_pool.get()
                if first_layer_input:
                    S.dma("sp", xs, xs[:], x_d, x_h[s, j * P:(j + 1) * P, :])
                else:
                    S.dma("sp", xs, xs[:], xo_d[s][j], out_h[s, j * P:(j + 1) * P, :])
                ss = ssq.get()
                MSET("dve", ss, ss[:], 0.0)
                ACTF(junk, junk[:], xs, xs[:], ACT.Square, accum=(ss, ss[:, 0:1]))
                TS("dve", ss, ss[:, 0:1], ss, ss[:, 0:1], 1.0 / D, EPS, ALU.mult, ALU.add)
                S.op("dve", lambda e, ss=ss: e.reciprocal(out=ss[:, 0:1], in_=ss[:, 0:1]), [ss], [ss])
                ACTF(ss, ss[:, 0:1], ss, ss[:, 0:1], ACT.Sqrt)
                hf = hf_pool.get()
                STT("dve", hf, hf[:], xs, xs[:], ss[:, 0:1], gmod, gmod[:], ALU.mult, ALU.mult, reads=[ss])
                TT("pool", hf, hf[:], hf, hf[:], shift, shift[:], ALU.add)
                return hf

            def phase_b(j, hf):
                q, jj = divmod(j, 4)
                pa, pb = ps[(2 * j) % 4], ps[(2 * j) % 4 + 1]
                for kc in range(KC):
                    pt = pa if kc < 4 else pb
                    TR(pt, pt[:, (kc % 4) * P:(kc % 4 + 1) * P], hf, hf[:, kc * P:(kc + 1) * P], *ident)
                CP("act", hT[q], hT[q][:, 0:4, jj * P:(jj + 1) * P], pa, pa[:].rearrange("p (k t) -> p k t", k=4))
                CP("act", hT[q], hT[q][:, 4:8, jj * P:(jj + 1) * P], pb, pb[:].rearrange("p (k t) -> p k t", k=4))
                if h2f_cb is not None:
                    h2f_cb(j, pa, pb)

            hf_n = phase_a(0)
            for j in range(NT):
                hf_c = hf_n
                if j + 1 < NT:
                    hf_n = phase_a(j + 1)
                phase_b(j, hf_c)

        def mixer(s, l):
            first = (l == 0)
            w_in = w_in_h[l]
            with ExitStack() as st:
                hT = [S.sb(f"hT{q}", [P, KC, 512], BF16, st) for q in range(NQ)]
                oT = {k: S.sb(f"oT_{k}", [P, 4, SEQ], BF16, st) for k in ("ret", "conv", "att")}
                pp = S.sb("pp", [P, NPP], F32, st)
                S.dma("sp", pp, pp[:], wts_d, pp_h[l])
                with ExitStack() as st1:
                    gmod = load_mod_bc(st1, l, s, 1, "gmod1")
                    shift = load_mod_bc(st1, l, s, 0, "shift1")
                    norm_and_transpose(st1, l, s, first, gmod, shift, hT)
                DUMP(0, hT[0], hT[0][:].rearrange("p k t -> p (k t)"), 4096)
                S.barrier()
                if dbg == "s1":
                    return "stop"
                fpool = Pool("fblk", [P, KC, P], BF16, 4, st)

                def fproj(c0, q, pt):
                    raise NotImplementedError

                def load_f(c0, swap=False):
                    wb = fpool.get()
                    if not swap:
                        S.dma("pool", wb, wb[:], wts_d, wcols(w_in, c0, P))
                    else:
                        S.dma("pool", wb, wb[:, :, 0:64], wts_d, wcols(w_in, c0 + 64, 64))
                        S.dma("pool", wb, wb[:, :, 64:128], wts_d, wcols(w_in, c0, 64))
                    return wb

                def f_mm(wb, q, pt):
                    for kc in range(KC):
                        MM(pt, pt[:], wb, wb[:, kc, :], hT[q], hT[q][:, kc, :], kc == 0, kc == KC - 1)

                with ExitStack() as st2:
                    cosT = S.sb("cosT", [P, SEQ], BF16, st2)
                    sinT = S.sb("sinT", [P, SEQ], BF16, st2)
                    with ExitStack() as st3:
                        posi = S.sb("posi", [P, SEQ], I32, st3)
                        u = S.sb("rope_u", [P, SEQ], F32, st3)
                        f = S.sb("rope_f", [P, SEQ], F32, st3)
                        ki = S.sb("rope_ki", [P, SEQ], I32, st3)
                        S.dma("sp", posi, posi[:], pos_d, pos_h[s:s + 1, :].partition_broadcast(P))
                        CP("dve", u, u[:], posi, posi[:])
                        TS("dve", u, u[:], u, u[:], cst[:, CS_INV:CS_INV + 1], 1.0 / (2.0 * math.pi), ALU.mult, ALU.mult, reads=[cst])
                        for tab, off, sgn in ((sinT, 0.0, True), (cosT, 0.25, False)):
                            if off != 0.0:
                                TS("dve", f, f[:], u, u[:], off, None, ALU.add)
                                src = f
                            else:
                                src = u
                            CP("dve", ki, ki[:], src, src[:])
                            fk = S.sb("rope_fk" + ("s" if sgn else "c"), [P, SEQ], F32, st3)
                            CP("dve", fk, fk[:], ki, ki[:])
                            TT("dve", fk, fk[:], src, src[:], fk, fk[:], ALU.subtract)
                            TS("dve", fk, fk[:], fk, fk[:], 0.49999, -0.49999, ALU.min, ALU.max)
                            if sgn:
                                ACTF(fk, fk[:], fk, fk[:], ACT.Sin, scale=2.0 * math.pi)
                                TS("dve", tab, tab[:], fk, fk[:], cst[:, CS_SGN:CS_SGN + 1], None, ALU.mult, reads=[cst])
                            else:
                                ACTF(tab, tab[:], fk, fk[:], ACT.Sin, scale=2.0 * math.pi)
                    S.barrier()
                    state_f = S.sb("state_f", [P, 4, P], F32, st2)
                    state_b = S.sb("state_b", [P, 4, P], BF16, st2)
                    gn_bc = S.sb("gn_bc", [P, 512], F32, st2)
                    S.dma("sp", gn_bc, gn_bc[:], wts_d, ret_gn_h[l:l + 1, :].partition_broadcast(P))
                    HS = SEQ // 2
                    qT = [S.sb("r_qT0", [P, 4, HS], BF16, st2), TView(oT["conv"], oT["conv"][:, 0:2, :].rearrange("p a (b t) -> p (a b) t", b=2))]
                    kT = [S.sb("r_kT0", [P, 4, HS], BF16, st2), TView(oT["conv"], oT["conv"][:, 2:4, :].rearrange("p a (b t) -> p (a b) t", b=2))]
                    v_tok = [S.sb("r_v0", [P, 8, 512], BF16, st2), TView(oT["att"], oT["att"][:, 0:2, :].rearrange("p a (b t) -> p (a b) t", b=4))]
                    gs = [S.sb("r_gs0", [P, 8, 512], BF16, st2), TView(oT["att"], oT["att"][:, 2:4, :].rearrange("p a (b t) -> p (a b) t", b=4))]
                    tpool = Pool("tblk", [P, KC, 512], BF16, 2, st2)
                    t1p = Pool("r_t1", [P, 512], F32, 2, st2)
                    t2p = Pool("r_t2", [P, 512], F32, 2, st2)
                    sTm_p = Pool("r_sTm", [P, 512], BF16, 2, st2)
                    qd_p = Pool("r_qd", [P, 4, P], BF16, 2, st2)
                    kt_p = Pool("r_kt", [P, 512], BF16, 2, st2)
                    on_p = Pool("r_on", [P, 512], F32, 2, st2)
                    og_p = Pool("r_og", [P, 512], F32, 3, st2)
                    st_p = Pool("r_stats", [P, 16], F32, 2, st2)
                    sgt_p = Pool("r_sg", [P, 512], F32, 2, st2)
                    junk = S.sb("r_junk", [P, P], BF16, st2)
                    def r_units(seg, mixed_banks=False):
                        qk_banks = [(ps[1], ps[5]), (ps[1], ps[5])] if mixed_banks else [(ps[0], ps[1]), (ps[2], ps[3])]
                        vg_banks = [ps[1], ps[5]] if mixed_banks else [ps[4], ps[5]]
                        us = []
                        for which, cbase, dst in (("q", C_RQ, qT[seg]), ("k", C_RK, kT[seg])):
                            for h in range(4):
                                def ld(cbase=cbase, h=h):
                                    return (load_f(cbase + h * P), load_f(cbase + h * P, swap=True))

                                def cp(w, dst=dst, h=h, seg=seg):
                                    wb, wsw = w
                                    for qq in range(2):
                                        q = seg * 2 + qq
                                        pa, pb = qk_banks[qq]
                                        f_mm(wb, q, pa)
                                        f_mm(wsw, q, pb)
                                        t1, t2 = t1p.get(), t2p.get()
                                        TT("dve", t1, t1[:], pa, pa[:], cosT, cosT[:, q * 512:(q + 1) * 512], ALU.mult)
                                        TT("dve", t2, t2[:], pb, pb[:], sinT, sinT[:, q * 512:(q + 1) * 512], ALU.mult)
                                        TT("pool", dst, dst[:, h, qq * 512:(qq + 1) * 512], t1, t1[:], t2, t2[:], ALU.add)
                                us.append((ld, cp))
                        for which, cbase in (("v", C_RV), ("g", C_RG)):
                            def ld(cbase=cbase):
                                wb = tpool.get()
                                S.dma("pool", wb, wb[:], wts_d, wcols(w_in, cbase, 512))
                                return wb

                            def cp(wb, which=which, seg=seg):
                                for jl in range(8):
                                    j = seg * 8 + jl
                                    q, jj = divmod(j, 4)
                                    pt = vg_banks[jl % 2]
                                    for kc in range(KC):
                                        MM(pt, pt[:], hT[q], hT[q][:, kc, jj * P:(jj + 1) * P], wb, wb[:, kc, :], kc == 0, kc == KC - 1)
                                    if which == "v":
                                        CP("act", v_tok[seg], v_tok[seg][:, jl, :], pt, pt[:])
                                    else:
                                        sg = sgt_p.get()
                                        ACTF(sg, sg[:], pt, pt[:], ACT.Silu)
                                        TT("pool", gs[seg], gs[seg][:, jl, :], sg, sg[:], gn_bc, gn_bc[:], ALU.mult)
                            us.append((ld, cp))
                        return us

                    def r_recA(seg, jl):
                        qT_, kT_, v_tok_, gs_ = qT[seg], kT[seg], v_tok[seg], gs[seg]
                        if True:
                            j = seg * 8 + jl
                            tsl = slice(jl * P, (jl + 1) * P)
                            pS, pO, pK, pT_ = ps[0], ps[2 + (jl % 2)], ps[6], ps[7]
                            for h in range(4):
                                MM(pS, pS[:, h * P:(h + 1) * P], kT_, kT_[:, h, tsl], qT_, qT_[:, h, tsl])
                            sTm = sTm_p.get()
                            TT("dve", sTm, sTm[:], pS, pS[:], cst, cst[:, CS_MASK:CS_MASK + 512], ALU.mult)
                            qd = qd_p.get()
                            TT("pool", qd, qd[:], qT_, qT_[:, :, tsl], cst,
                               cst[:, CS_QDEC:CS_QDEC + 512].rearrange("p (h c) -> p h c", h=4), ALU.mult)
                            pTb = pT_[:, 0:256].bitcast(BF16)
                            for h in range(4):
                                S.op("pe", lambda e, h=h, tsl=tsl, pTb=pTb: e.transpose(
                                    out=pTb[:, h * P:(h + 1) * P], in_=kT_[:, h, tsl], identity=identb[:]),
                                    [kT_, identb], [pT_])
                            kt = kt_p.get()
                            for h in range(4):
                                TS("dve", kt, kt[:, h * P:(h + 1) * P], pT_, pTb[:, h * P:(h + 1) * P],
                                   cst[:, CS_KDEC + h:CS_KDEC + h + 1], None, ALU.mult, reads=[cst])
                            for h in range(4):
                                hs = slice(h * P, (h + 1) * P)
                                MM(pO, pO[:, hs], sTm, sTm[:, hs], v_tok_, v_tok_[:, jl, hs], True, j == 0)
                                if j > 0:
                                    MM(pO, pO[:, hs], qd, qd[:, h, :], state_b, state_b[:, h, :], False, True)
                            if j < NT - 1:
                                for h in range(4):
                                    hs = slice(h * P, (h + 1) * P)
                                    MM(pK, pK[:, hs], kt, kt[:, hs], v_tok_, v_tok_[:, jl, hs])
                                if j == 0:
                                    CP("dve", state_f, state_f[:], pK, pK[:].rearrange("p (h e) -> p h e", h=4))
                                else:
                                    for h in range(4):
                                        STT("dve", state_f, state_f[:, h, :], state_f, state_f[:, h, :], RET_CD[h],
                                            pK, pK[:, h * P:(h + 1) * P], ALU.mult, ALU.add)
                                CP("act", state_b, state_b[:], state_f, state_f[:])
                    def r_recB(seg, jl):
                        qT_, kT_, v_tok_, gs_ = qT[seg], kT[seg], v_tok[seg], gs[seg]
                        if True:
                            j = seg * 8 + jl
                            pO = ps[2 + (jl % 2)]
                            stt = st_p.get()
                            MSET("dve", stt, stt[:], 0.0)
                            for h in range(4):
                                hs = slice(h * P, (h + 1) * P)
                                ACTF(junk, junk[:], pO, pO[:, hs], ACT.Copy, accum=(stt, stt[:, h:h + 1]))
                                ACTF(junk, junk[:], pO, pO[:, hs], ACT.Square, accum=(stt, stt[:, 4 + h:5 + h]))
                            TS("dve", stt, stt[:, 0:8], stt, stt[:, 0:8], 1.0 / P, None, ALU.mult)
                            TT("dve", stt, stt[:, 8:12], stt, stt[:, 0:4], stt, stt[:, 0:4], ALU.mult)
                            TT("dve", stt, stt[:, 12:16], stt, stt[:, 4:8], stt, stt[:, 8:12], ALU.subtract)
                            TS("dve", stt, stt[:, 12:16], stt, stt[:, 12:16], EPS, None, ALU.add)
                            RSTD(stt, stt[:, 12:16])
                            on = on_p.get()
                            for h in range(4):
                                hs = slice(h * P, (h + 1) * P)
                                TS("dve", on, on[:, hs], pO, pO[:, hs], stt[:, h:h + 1], stt[:, 12 + h:13 + h],
                                   ALU.subtract, ALU.mult, reads=[stt])
                            og = og_p.get()
                            TT("dve", og, og[:], on, on[:], gs_, gs_[:, jl, :], ALU.mult)
                            return og

                    def r_recC(seg, jl, og):
                        j = seg * 8 + jl
                        pX = ps[4]
                        for h in range(4):
                            TR(pX, pX[:, h * P:(h + 1) * P], og, og[:, h * P:(h + 1) * P], *ident)
                        CP("act", oT["ret"], oT["ret"][:, :, j * P:(j + 1) * P], pX, pX[:].rearrange("p (h c) -> p h c", h=4))

                    u1 = r_units(1, mixed_banks=True)
                    ogs = {}

                    def stepB(seg, i):
                        ogs[(seg, i)] = r_recB(seg, i)

                    def stepC(seg, i):
                        r_recC(seg, i, ogs.pop((seg, i)))

                    mixed = [(None, lambda _w: r_recA(0, 0))]
                    for i in range(max(8, len(u1))):
                        if i < 8:
                            if i + 1 < 8:
                                mixed.append((None, lambda _w, i=i: r_recA(0, i + 1)))
                            mixed.append((None, lambda _w, i=i: stepB(0, i)))
                            if i >= 1:
                                mixed.append((None, lambda _w, i=i: stepC(0, i - 1)))
                        if i == 8:
                            mixed.append((None, lambda _w: stepC(0, 7)))
                        if i < len(u1):
                            mixed.append(u1[i])
                    tail = [(None, lambda _w: r_recA(1, 0))]
                    for i in range(8):
                        if i + 1 < 8:
                            tail.append((None, lambda _w, i=i: r_recA(1, i + 1)))
                        tail.append((None, lambda _w, i=i: stepB(1, i)))
                        if i >= 1:
                            tail.append((None, lambda _w, i=i: stepC(1, i - 1)))
                    tail.append((None, lambda _w: stepC(1, 7)))
                    run_units(r_units(0) + mixed + tail)
                S.barrier()
                if dbg == "ret":
                    return "stop"
                with ExitStack() as st2:
                    zT = S.sb("c_zT", [P, 4, 30 + SEQ], BF16, st2)
                    acc = S.sb("c_acc", [P, 4, SEQ], F32, st2)
                    sgp = Pool("c_sg", [P, 512], F32, 2, st2)
                    dgp = Pool("c_dg", [P, P], BF16, 6, st2)
                    MSET("dve", zT, zT[:, :, 0:30], 0.0)
                    def c_unit(cc):
                        def ld():
                            return (load_f(C_CU + cc * P), load_f(C_CU + 512 + cc * P))

                        def cp(w):
                            wa, wb = w
                            for q in range(NQ):
                                pa, pb = ps[2 * (q % 2)], ps[2 * (q % 2) + 1]
                                f_mm(wa, q, pa)
                                f_mm(wb, q, pb)
                                sg = sgp.get()
                                ACTF(sg, sg[:], pb, pb[:], ACT.Sigmoid)
                                TT("dve", zT, zT[:, cc, 30 + q * 512:30 + (q + 1) * 512], pa, pa[:], sg, sg[:], ALU.mult)
                            for k in range(31):
                                dg = dgp.get()
                                wk = pp[:, PP_CW + cc * 31 + k:PP_CW + cc * 31 + k + 1]
                                TS("pool" if k % 2 else "dve", dg, dg[:], identb, identb[:], wk, None, ALU.mult, reads=[pp])
                                for q in range(NQ):
                                    pc = ps[4 + q]
                                    MM(pc, pc[:], dg, dg[:], zT, zT[:, cc, q * 512 + k:q * 512 + k + 512], k == 0, k == 30)
                            for q in range(NQ):
                                pc = ps[4 + q]
                                TS("dve", acc, acc[:, cc, q * 512:(q + 1) * 512], pc, pc[:], pp[:, PP_CB + cc:PP_CB + cc + 1], None,
                                   ALU.add, reads=[pp])
                        return (ld, cp)

                    run_units([c_unit(cc) for cc in range(4)])
                    sqp = Pool("c_sq", [P, 512], F32, 2, st2)
                    m2p = Pool("c_m2", [P, 512], F32, 2, st2)
                    rsp = Pool("c_rs", [P, 512], F32, 2, st2)
                    tp = Pool("c_t", [P, 512], F32, 2, st2)
                    for q in range(NQ):
                        qs = slice(q * 512, (q + 1) * 512)
                        pm, pe2 = ps[4 + 2 * (q % 2)], ps[5 + 2 * (q % 2)]
                        for cc in range(4):
                            MM(pm, pm[:], onesln, onesln[:], acc, acc[:, cc, qs], cc == 0, cc == 3)
                        for cc in range(4):
                            sq = sqp.get()
                            ACTF(sq, sq[:], acc, acc[:, cc, qs], ACT.Square)
                            MM(pe2, pe2[:], onesln, onesln[:], sq, sq[:], cc == 0, cc == 3)
                        m2 = m2p.get()
                        ACTF(m2, m2[:], pm, pm[:], ACT.Square)
                        rs = rsp.get()
                        TT("dve", rs, rs[:], pe2, pe2[:], m2, m2[:], ALU.subtract)
                        TS("dve", rs, rs[:], rs, rs[:], EPS, None, ALU.add)
                        RSTD(rs, rs[:])
                        for cc in range(4):
                            t = tp.get()
                            TT("dve", t, t[:], acc, acc[:, cc, qs], pm, pm[:], ALU.subtract)
                            TT("pool", t, t[:], t, t[:], rs, rs[:], ALU.mult)
                            ACTF(oT["conv"], oT["conv"][:, cc, qs], t, t[:], ACT.Silu,
                                 bias=pp[:, PP_LB + cc:PP_LB + cc + 1], scale=pp[:, PP_LG + cc:PP_LG + cc + 1], reads=[pp])
                S.barrier()
                if dbg == "conv":
                    return "stop"
                with ExitStack() as st2:
                    aqM = [S.sb(f"a_qM{i}", [P, 4, SEQ], BF16, st2) for i in range(2)]
                    MSET("dve", aqM[0], aqM[0][:], 0.0)
                    MSET("dve", aqM[1], aqM[1][:], 0.0)
                    akT = S.sb("a_kT", [P, 4, SEQ], BF16, st2)
                    vaug = S.sb("a_v", [P, NT, 8, 66], BF16, st2)
                    biasT = S.sb("a_bias", [P, 5, 8, P], F32, st2)
                    S.dma("sp", biasT, biasT[:], wts_d, biasx_h[l])
                    MSET("dve", biasT, biasT[0:64, 0, :, 64:128], NEG)
                    MSET("dve", biasT, biasT[64:128, 4, :, 0:64], NEG)
                    MSET("dve", vaug, vaug[:], 1.0)
                    with ExitStack() as st3:
                        sqp = Pool("a_sq", [P, 512], BF16, 2, st3)
                        rsp = Pool("a_rs", [P, 512], F32, 2, st3)
                        def a_unit(dst, cbase, gcol, c):
                            def ld():
                                return load_f(cbase + c * P)

                            def cp(wb):
                                for q in range(NQ):
                                    qs = slice(q * 512, (q + 1) * 512)
                                    pa, pb = ps[2 * (q % 2)], ps[2 * (q % 2) + 1]
                                    f_mm(wb, q, pa)
                                    sq = sqp.get()
                                    ACTF(sq, sq[:], pa, pa[:], ACT.Square)
                                    MM(pb, pb[:], blk64, blk64[:], sq, sq[:])
                                    rs = rsp.get()
                                    ACTF(rs, rs[:], pb, pb[:], ACT.Sqrt, bias=epsc[:, 0:1], reads=[epsc])
                                    S.op("dve", lambda e, rs=rs: e.reciprocal(out=rs[:], in_=rs[:]), [rs], [rs], small=False)
                                    if dst is None:
                                        for i in range(2):
                                            hp = slice(64 * i, 64 * i + 64)
                                            STT("dve", aqM[i], aqM[i][hp, c, qs], pa, pa[hp, :], pp[hp, gcol:gcol + 1], rs, rs[hp, :],
                                                ALU.mult, ALU.mult, reads=[pp])
                                    else:
                                        STT("dve", dst, dst[:, c, qs], pa, pa[:], pp[:, gcol:gcol + 1], rs, rs[:],
                                            ALU.mult, ALU.mult, reads=[pp])
                            return (ld, cp)

                        def av_ld():
                            wb = tpool.get()
                            S.dma("pool", wb, wb[:], wts_d, wcols(w_in, C_AV, 512))
                            return wb

                        def av_cp(wb):
                            for j in range(NT if dbg != "att0" else 0):
                                q, jj = divmod(j, 4)
                                pt = ps[4 + j % 2]
                                for kc in range(KC):
                                    MM(pt, pt[:], hT[q], hT[q][:, kc, jj * P:(jj + 1) * P], wb, wb[:, kc, :], kc == 0, kc == KC - 1)
                                CP("act", vaug, vaug[:, j, :, 0:64], pt, pt[:].rearrange("p (h d) -> p h d", h=8))

                        tpool = Pool("tblk", [P, KC, 512], BF16, 1, st3)
                        run_units([a_unit(dst, cbase, gcol, c) for dst, cbase, gcol in ((None, C_AQ, PP_QG), (akT, C_AK, PP_KG))
                                   for c in range(4)] + [(av_ld, av_cp)])
                    S.barrier()
                    ep = Pool("a_e", [P, 512], F32, 3, st2)
                    pTp = Pool("a_pT", [P, 5, 512], BF16, 3, st2)
                    recp = Pool("a_rec", [P, 8], F32, 2, st2)
                    oap = Pool("a_o", [P, 512], F32, 2, st2)
                    n_att = NT if dbg not in ("att0", "att1") else 0

                    def a_scores(j, g):
                        tsl = slice(j * P, (j + 1) * P)
                        kbs = [kb for kb in range(5) if j - 4 + kb >= 0]
                        pTt = pTp.get()
                        for kb in kbs:
                            jk = j - 4 + kb
                            ksl = slice(jk * P, (jk + 1) * P)
                            pq = ps[kb % 2 + 2 * g]
                            for hh in range(4):
                                h = g * 4 + hh
                                c = h // 2
                                MM(pq, pq[:, hh * P:(hh + 1) * P], akT, akT[:, c, ksl], aqM[h % 2], aqM[h % 2][:, c, tsl])
                            e_ = ep.get()
                            STT("dve", e_, e_[:].rearrange("p (h q) -> p h q", h=4), pq,
                                pq[:].rearrange("p (h q) -> p h q", h=4), 0.125,
                                biasT, biasT[:, kb, g * 4:(g + 1) * 4, :], ALU.mult, ALU.add)
                            ACTF(pTt, pTt[:, kb, :], e_, e_[:], ACT.Exp)
                        return pTt, kbs

                    def a_pv(j, g, pTt, kbs, oa, rec):
                        po = ps[4 + g]
                        for hh in range(4):
                            h = g * 4 + hh
                            for i, kb in enumerate(kbs):
                                jk = j - 4 + kb
                                MM(po, po[:, hh * 66:(hh + 1) * 66], pTt, pTt[:, kb, hh * P:(hh + 1) * P],
                                   vaug, vaug[:, jk, h, :], i == 0, i == len(kbs) - 1)
                        pov = po[:, 0:264].rearrange("p (h d) -> p h d", h=4)
                        S.op("dve", lambda e, rec=rec, pov=pov, g=g: e.reciprocal(
                            out=rec[:, g * 4:(g + 1) * 4].unsqueeze(2), in_=pov[:, :, 64:65]), [po], [rec])
                        TT("dve", oa, oa[:, g * 256:(g + 1) * 256].rearrange("p (h d) -> p h d", h=4), po, pov[:, :, 0:64],
                           rec, rec[:, g * 4:(g + 1) * 4].unsqueeze(2).broadcast_to([P, 4, 64]), ALU.mult)

                    def a_fin(j, oa):
                        tsl = slice(j * P, (j + 1) * P)
                        pX = ps[6 + j % 2]
                        for c in range(4):
                            TR(pX, pX[:, c * P:(c + 1) * P], oa, oa[:, c * P:(c + 1) * P], *ident)
                        CP("act", oT["att"], oT["att"][:, :, tsl], pX, pX[:].rearrange("p (c t) -> p c t", c=4))

                    groups = [(j, g) for j in range(n_att) for g in range(2)]
                    nxt = a_scores(*groups[0]) if groups else None
                    oa = rec = None
                    for n, (j, g) in enumerate(groups):
                        cur = nxt
                        if n + 1 < len(groups):
                            nxt = a_scores(*groups[n + 1])
                        if g == 0:
                            oa, rec = oap.get(), recp.get()
                        a_pv(j, g, cur[0], cur[1], oa, rec)
                        if g == 1:
                            a_fin(j, oa)
                for i_, k_ in enumerate(("ret", "conv", "att")):
                    DUMP(1 + i_, oT[k_], oT[k_][:].rearrange("p k t -> p (k t)"), 8192)
                S.barrier()
                if dbg and dbg.startswith("att"):
                    return "stop"
                with ExitStack() as st2:
                    mT = [S.sb(f"mT{q}", [P, KC, 512], BF16, st2) for q in range(NQ)]
                    glp = Pool("s4_gl", [P, KC, 3, P], BF16, 2, st2)
                    wop = Pool("s4_wo", [P, 4, 3, P], BF16, 2, st2)
                    gtp = Pool("s4_g", [P, 512], F32, 3, st2)
                    tmp = Pool("s4_t", [P, 512], F32, 2, st2)
                    macc = Pool("s4_m", [P, 512], F32, 2, st2)
                    outs_w = (w_ro_h[l], w_co_h[l], w_ao_h[l])
                    keys = ("ret", "conv", "att")
                    def s4_unit(c):
                        def ld():
                            gw = glp.get()
                            ow = wop.get()
                            for b in range(3):
                                S.dma("pool", gw, gw[:, :, b, :], wts_d, wcols(w_in, C_GL + b * D + c * P, P))
                                S.dma("pool", ow, ow[:, :, b, :], wts_d, wcols(outs_w[b], c * P, P))
                            return (gw, ow)

                        def cp(w):
                            gw, ow = w
                            for q in range(NQ):
                                qs = slice(q * 512, (q + 1) * 512)
                                m = macc.get()
                                for b in range(3):
                                    pg, py = ps[2 * (b % 2)], ps[2 * (b % 2) + 1]
                                    for kc in range(KC):
                                        MM(pg, pg[:], gw, gw[:, kc, b, :], hT[q], hT[q][:, kc, :], kc == 0, kc == KC - 1)
                                    for kc in range(4):
                                        MM(py, py[:], ow, ow[:, kc, b, :], oT[keys[b]], oT[keys[b]][:, kc, qs], kc == 0, kc == 3)
                                    gt = gtp.get()
                                    ACTF(gt, gt[:], pg, pg[:], ACT.Sigmoid,
                                         bias=pp[:, PP_BG + b * 8 + c:PP_BG + b * 8 + c + 1], reads=[pp])
                                    if b == 0:
                                        TT("dve", m, m[:], py, py[:], gt, gt[:], ALU.mult)
                                    elif b == 1:
                                        t = tmp.get()
                                        TT("dve", t, t[:], py, py[:], gt, gt[:], ALU.mult)
                                        TT("pool", m, m[:], m, m[:], t, t[:], ALU.add)
                                    else:
                                        t = tmp.get()
                                        TT("dve", t, t[:], py, py[:], gt, gt[:], ALU.mult)
                                        TT("pool", mT[q], mT[q][:, c, :], m, m[:], t, t[:], ALU.add)
                        return (ld, cp)

                    wo = S.sb("s4_wout", [P, KC, D], BF16, st2)
                    gate1 = load_mod_bc(st2, l, s, 2, "gate1")
                    xs_pool = Pool("s4_xs", [P, D], F32, 4, st2)

                    def wo_ld():
                        S.dma("pool", wo, wo[:], wts_d, wcols(w_out_h[l], 0, D))
                        return wo

                    def wo_cp(_w):
                        DUMP(4, mT[0], mT[0][:].rearrange("p k t -> p (k t)"), 4096)
                        s4_out()

                    def s4_load(j):
                        xs = xs_pool.get()
                        if first:
                            S.dma("sp", xs, xs[:], x_d, x_h[s, j * P:(j + 1) * P, :])
                        else:
                            S.dma("sp", xs, xs[:], xo_d[s][j], out_h[s, j * P:(j + 1) * P, :])
                        return xs

                    def s4_out():
                        xq = [s4_load(0), s4_load(1)]
                        for j in range(NT):
                            q, jj = divmod(j, 4)
                            xs = xq.pop(0)
                            if j + 2 < NT:
                                xq.append(s4_load(j + 2))
                            for half in range(2):
                                pt = ps[4 + (2 * j + half) % 4]
                                hsl = slice(half * 512, (half + 1) * 512)
                                for kc in range(KC):
                                    MM(pt, pt[:], mT[q], mT[q][:, kc, jj * P:(jj + 1) * P], wo, wo[:, kc, hsl], kc == 0, kc == KC - 1)
                                t = tmp.get()
                                TT("dve", t, t[:], pt, pt[:], gate1, gate1[:, hsl], ALU.mult)
                                TT("pool", xs, xs[:, hsl], xs, xs[:, hsl], t, t[:], ALU.add)
                            S.dma("act", xo_d[s][j], out_h[s, j * P:(j + 1) * P, :], xs, xs[:], sem_t=xs)
                    run_units([s4_unit(c) for c in range(KC)] + [(wo_ld, wo_cp)])
                S.barrier()
            S.barrier()

        def moe(s, l):
            with ExitStack() as st:
                hT = [S.sb(f"hT{q}", [P, KC, 512], BF16, st) for q in range(NQ)]
                acc = S.sb("m_acc", [P, NT, D], F32, st)
                wge = S.sb("m_wge", [P, NT, 32], F32, st)
                upp = Pool("m_wup", [P, KC, 512], BF16, 2, st)
                dnp = Pool("m_wdn", [P, 2, D], BF16, 2, st)

                def e_ld(ge):
                    g, e_ = divmod(ge, NE)
                    wu = upp.get()
                    wd = dnp.get()
                    S.dma("pool", wu, wu[:], wts_d, wcols(w_up_h[l, g, e_], 0, 512))
                    S.dma("pool", wd, wd[:], wts_d, w_dn_h[l, g, e_].rearrange("(fc p) n -> p fc n", p=P))
                    return (wu, wd)

                e_nxt = e_ld(0) if n_experts > 0 else None
                with ExitStack() as st1:
                    gmod = load_mod_bc(st1, l, s, 4, "gmod2")
                    shift = load_mod_bc(st1, l, s, 3, "shift2")
                    wr = S.sb("m_wr", [P, KC, 36], F32, st1)
                    S.dma("sp", wr, wr[:], wts_d, wr_h[l])
                    brow = S.sb("m_brow", [P, 36], F32, st1)
                    S.dma("sp", brow, brow[:], wts_d, brow_h[l:l + 1, :].partition_broadcast(P))
                    whi = S.sb("m_whi", [P, KC, 36], BF16, st1)
                    wlo = S.sb("m_wlo", [P, KC, 36], BF16, st1)
                    CP("dve", whi, whi[:], wr, wr[:])
                    TT("dve", wlo, wlo[:], wr, wr[:], whi, whi[:], ALU.subtract)
                    h2p = Pool("m_h2lo", [P, KC, P], BF16, 2, st1)
                    RW_ = 100
                    r = S.sb("m_r", [P, NT, RW_], F32, st1)
                    MSET("dve", r, r[:], 0.0)

                    def router(j, pa, pb):
                        q, jj = divmod(j, 4)
                        tsl = slice(jj * P, (jj + 1) * P)
                        h2 = h2p.get()
                        TT("dve", h2, h2[:, 0:4, :], pa, pa[:].rearrange("p (k t) -> p k t", k=4),
                           hT[q], hT[q][:, 0:4, tsl], ALU.subtract)
                        TT("dve", h2, h2[:, 4:8, :], pb, pb[:].rearrange("p (k t) -> p k t", k=4),
                           hT[q], hT[q][:, 4:8, tsl], ALU.subtract)
                        pl = ps[4 + j % 2]
                        for kc in range(KC):
                            MM(pl, pl[:, 0:36], hT[q], hT[q][:, kc, tsl], whi, whi[:, kc, :], kc == 0, False)
                            MM(pl, pl[:, 0:36], hT[q], hT[q][:, kc, tsl], wlo, wlo[:, kc, :], False, False)
                            MM(pl, pl[:, 0:36], h2, h2[:, kc, :], whi, whi[:, kc, :], False, kc == KC - 1)
                        TT("dve", r, r[:, j, 0:36], pl, pl[:, 0:36], brow, brow[:], ALU.add)

                    def router_batched():
                        R3 = lambda a_, b_: r[:, :, a_:b_]
                        BC = lambda a_, n_: r[:, :, a_:a_ + 1].broadcast_to([P, NT, n_])
                        C1 = lambda a_: r[:, :, a_:a_ + 1].rearrange("p j o -> p (j o)")
                        RED = lambda dst, a_, b_, op: S.op(
                            "dve", lambda e: e.tensor_reduce(out=C1(dst), in_=R3(a_, b_), axis=AX.X, op=op), [r], [r])
                        T3 = lambda o_, a_, b_, op: TT("dve", r, o_, r, a_, r, b_, op)
                        RED(36, 0, 4, ALU.max)
                        T3(R3(37, 41), R3(0, 4), BC(36, 4), ALU.is_ge)
                        T3(R3(41, 45), R3(0, 4), BC(36, 4), ALU.subtract)
                        ACTF(r, R3(41, 45), r, R3(41, 45), ACT.Exp)
                        RED(45, 41, 45, ALU.add)
                        S.op("dve", lambda e: e.reciprocal(out=C1(45), in_=C1(45)), [r], [r])
                        T3(R3(46, 54), R3(4, 12), BC(37, 8), ALU.mult)
                        for g in range(1, 4):
                            T3(R3(91, 99), R3(4 + 8 * g, 12 + 8 * g), BC(37 + g, 8), ALU.mult)
                            T3(R3(46, 54), R3(46, 54), R3(91, 99), ALU.add)
                        RED(54, 46, 54, ALU.max)
                        T3(R3(55, 63), R3(46, 54), BC(54, 8), ALU.is_ge)
                        STT("dve", r, R3(63, 71), r, R3(55, 63), NEG, r, R3(46, 54), ALU.mult, ALU.add)
                        RED(71, 63, 71, ALU.max)
                        T3(R3(72, 80), R3(63, 71), BC(71, 8), ALU.is_ge)
                        T3(R3(80, 81), R3(71, 72), R3(54, 55), ALU.subtract)
                        ACTF(r, R3(82, 83), r, R3(80, 81), ACT.Sigmoid)
                        TS("dve", r, R3(81, 82), r, R3(82, 83), -1.0, 1.0, ALU.mult, ALU.add)
                        T3(R3(81, 82), R3(81, 82), R3(45, 46), ALU.mult)
                        T3(R3(82, 83), R3(82, 83), R3(45, 46), ALU.mult)
                        T3(R3(83, 91), R3(55, 63), BC(81, 8), ALU.mult)
                        T3(R3(91, 99), R3(72, 80), BC(82, 8), ALU.mult)
                        T3(R3(83, 91), R3(83, 91), R3(91, 99), ALU.add)
                        for g in range(4):
                            TT("dve", wge, wge[:, :, g * 8:(g + 1) * 8], r, R3(83, 91), r, BC(37 + g, 8), ALU.mult)

                    norm_and_transpose(st1, l, s, False, gmod, shift, hT, h2f_cb=(None if dbg == "m_norouter" else router))
                    if dbg != "m_norouter":
                        router_batched()
                S.barrier()
                with ExitStack() as st2:
                    sap = Pool("m_sa", [P, 512], F32, 2, st2)
                    actp = Pool("m_act", [P, 2, 512], BF16, 2, st2)
                    dcount = 0
                    for ge in range(n_experts):
                        g, e_ = divmod(ge, NE)
                        wu, wd = e_nxt
                        e_nxt = e_ld(ge + 1) if ge + 1 < n_experts else None
                        for q in range(NQ):
                            for fc in range(4):
                                pu = ps[fc]
                                for kc in range(KC):
                                    MM(pu, pu[:], wu, wu[:, kc, fc * P:(fc + 1) * P], hT[q], hT[q][:, kc, :], kc == 0, kc == KC - 1)
                            at = actp.get()
                            for fc in range(2):
                                sa = sap.get()
                                ACTF(sa, sa[:], ps[fc], ps[fc][:], ACT.Silu)
                                TT("dve", at, at[:, fc, :], sa, sa[:], ps[fc + 2], ps[fc + 2][:], ALU.mult)
                            for jj in range(4):
                                j = q * 4 + jj
                                for half in range(2):
                                    pd = ps[4 + dcount % 4]
                                    dcount += 1
                                    hsl = slice(half * 512, (half + 1) * 512)
                                    for fc in range(2):
                                        MM(pd, pd[:], at, at[:, fc, jj * P:(jj + 1) * P], wd, wd[:, fc, hsl], fc == 0, fc == 1)
                                    if ge == 0:
                                        TS("dve", acc, acc[:, j, hsl], pd, pd[:], wge[:, j, ge:ge + 1], None, ALU.mult, reads=[wge])
                                    else:
                                        STT("dve", acc, acc[:, j, hsl], pd, pd[:], wge[:, j, ge:ge + 1], acc, acc[:, j, hsl],
                                            ALU.mult, ALU.add, reads=[wge])
                    gate2 = load_mod_bc(st2, l, s, 5, "gate2")
                    xs_pool = Pool("m_xs", [P, D], F32, 4, st2)
                    toks = []

                    def m_load(j):
                        xs = xs_pool.get()
                        S.dma("sp", xs, xs[:], xo_d[s][j], out_h[s, j * P:(j + 1) * P, :])
                        return xs

                    xq = [m_load(0), m_load(1)]
                    for j in range(NT):
                        xs = xq.pop(0)
                        if j + 2 < NT:
                            xq.append(m_load(j + 2))
                        TT("dve", acc, acc[:, j, :], acc, acc[:, j, :], gate2, gate2[:], ALU.mult)
                        TT("pool", xs, xs[:], xs, xs[:], acc, acc[:, j, :], ALU.add)
                        toks.append(S.dma("act", xo_d[s][j], out_h[s, j * P:(j + 1) * P, :], xs, xs[:], sem_t=xs))
                S.barrier()
                return toks

        epsc = S.sb("epsc", [P, 1], F32)
        MSET("dve", epsc, epsc[:], EPS)
        identb = S.sb("identb", [P, P], BF16)
        CP("dve", identb, identb[:], cst, cst[:, CS_ID:CS_ID + P])

        final = []
        try:
            if dbg and dbg.startswith("pro"):
                raise _Stop()
            for s in range(n_seq):
                for l in range(n_layers):
                    if mixer(s, l) == "stop":
                        raise _Stop()
                    final = moe(s, l) if n_experts > 0 else []
        except _Stop:
            S.barrier()
        S.emit(final_toks=S.dma_toks + final + dump_toks)
    return nc


def _constants():
    cst = np.zeros((P, NCST), np.float32)
    cst[:, CS_ID:CS_ID + P] = np.eye(P, dtype=np.float32)
    m = np.arange(P)[:, None].astype(np.float64)
    c = np.arange(P)[None, :].astype(np.float64)
    for h in range(4):
        gamma = 1.0 - 2.0 ** (-5 - h)
        mask = np.where(c >= m, gamma ** np.maximum(c - m, 0.0), 0.0) * (128.0 ** -0.5)
        cst[:, CS_MASK + h * P:CS_MASK + (h + 1) * P] = mask
        cst[:, CS_QDEC + h * P:CS_QDEC + (h + 1) * P] = (gamma ** (c + 1.0))
        cst[:, CS_KDEC + h] = (gamma ** (127.0 - m[:, 0])) * (128.0 ** -0.5)
    half = 64
    inv = (np.float32(10000.0) ** (-np.arange(half, dtype=np.float32) / np.float32(half))).astype(np.float32)
    cst[:, CS_INV] = np.tile(inv, 2)
    cst[0:64, CS_SGN] = -1.0
    cst[64:128, CS_SGN] = 1.0
    cst[0:64, CS_MLO] = 1.0
    cst[64:128, CS_MHI] = 1.0
    return cst


def _layout_params(inp):
    f32 = np.float32
    pp = np.zeros((L, P, NPP), f32)
    cw = np.asarray(inp["conv_w"], f32)[:, :, 0, :]
    pp[:, :, PP_CW:PP_CW + 124] = cw.reshape(L, 31, 4, P).transpose(0, 3, 2, 1).reshape(L, P, 124)
    pp[:, :, PP_CB:PP_CB + 4] = np.asarray(inp["conv_b"], f32).reshape(L, 4, P).transpose(0, 2, 1)
    pp[:, :, PP_LG:PP_LG + 4] = np.asarray(inp["conv_ln_g"], f32).reshape(L, 4, P).transpose(0, 2, 1)
    pp[:, :, PP_LB:PP_LB + 4] = np.asarray(inp["conv_ln_b"], f32).reshape(L, 4, P).transpose(0, 2, 1)
    pp[:, :, PP_QG] = np.tile(np.asarray(inp["att_q_gain"], f32), (1, 2))
    pp[:, :, PP_KG] = np.tile(np.asarray(inp["att_k_gain"], f32), (1, 2))
    pp[:, :, PP_BG:PP_BG + 24] = np.asarray(inp["b_gate"], f32).reshape(L, 24, P).transpose(0, 2, 1)
    wr = np.concatenate([np.asarray(inp["w_group"], f32), np.asarray(inp["w_inner"], f32)], axis=-1)
    wr = np.ascontiguousarray(wr.reshape(L, KC, P, 36).transpose(0, 2, 1, 3))
    brow = np.ascontiguousarray(np.concatenate([np.asarray(inp["b_group"], f32), np.asarray(inp["b_inner"], f32)], axis=-1))
    k = np.arange(P)[:, None, None]
    kb = np.arange(5)[None, :, None]
    q = np.arange(P)[None, None, :]
    idx = np.clip(q - k + 128 * (4 - kb), -256, 256) + 256
    tab = np.asarray(inp["att_rel_bias"], f32)
    bx = tab[:, :, idx]
    biasx = np.ascontiguousarray(bx.transpose(0, 2, 3, 1, 4))
    return pp, wr, brow, biasx


_PROG = {}


def kernel(**inp):
    n = 8
    f32 = np.float32
    x = np.asarray(inp["x"], f32)
    c = np.asarray(inp["c"], f32)
    pos = np.asarray(inp["positions"], np.int32)
    pp, wr, brow, biasx = _layout_params(inp)
    cst = _constants()
    shared = dict(
        w_ada=np.asarray(inp["w_ada"], f32), b_ada=np.asarray(inp["b_ada"], f32),
        g_mix=np.asarray(inp["g_mix"], f32), g_ffn=np.asarray(inp["g_ffn"], f32),
        w_in=np.asarray(inp["w_in"], f32), ret_gn=np.asarray(inp["ret_gn"], f32),
        w_ret_out=np.asarray(inp["w_ret_out"], f32), w_conv_out=np.asarray(inp["w_conv_out"], f32),
        w_att_out=np.asarray(inp["w_att_out"], f32), w_out=np.asarray(inp["w_out"], f32),
        w_up=np.asarray(inp["w_up"], f32), w_down=np.asarray(inp["w_down"], f32),
        pp=pp, wr=wr, brow=brow, biasx=biasx, cst=cst)
    if "nc" not in _PROG:
        _PROG["nc"] = build_program()
    nc = _PROG["nc"]
    in_maps = []
    for i in range(n):
        sl = slice(NSEQ * i, NSEQ * (i + 1))
        m = dict(shared)
        m["x"] = np.ascontiguousarray(x[sl])
        m["pos"] = np.ascontiguousarray(pos[sl])
        m["cT"] = np.ascontiguousarray(c[sl].reshape(NSEQ, KC, P).transpose(2, 1, 0))
        in_maps.append(m)
    res = run_bass_kernel_spmd(nc, in_maps, core_ids=list(range(n)))
    return np.concatenate([np.asarray(r["out"], f32) for r in res.results], axis=0)
```
